# Optimizing a Trainium2 kernel written in Bass

```python
import math
import jax, jax.numpy as jnp
from jax import lax
import numpy as np

D_MODEL = 1024
BATCH = 16
SEQ = 4096
DEPTH = 2

MOBA_HEADS = 8
MOBA_HEAD_DIM = D_MODEL // 16
MOBA_WIDTH = MOBA_HEADS * MOBA_HEAD_DIM
MOBA_BLOCK = 256
MOBA_TOP_BLOCKS = 3
MOBA_QUERY_BLOCK = 128
ROPE_THETA = 500000.0
ROPE_DIM = MOBA_HEAD_DIM // 4
GLA_HEADS = 4
GLA_KEY_DIM = D_MODEL // 8
GLA_VAL_DIM = D_MODEL // 8
GLA_K_WIDTH = GLA_HEADS * GLA_KEY_DIM
GLA_V_WIDTH = GLA_HEADS * GLA_VAL_DIM
GLA_GATE_RANK = 16
GLA_GATE_TAU = 16.0
GLA_CHUNK = 64
N_BRANCHES = 2
IN_SPLITS = (MOBA_WIDTH, MOBA_WIDTH, MOBA_WIDTH, GLA_K_WIDTH, GLA_K_WIDTH, GLA_V_WIDTH, GLA_V_WIDTH, GLA_GATE_RANK, D_MODEL, D_MODEL)
IN_COLS = 3 * MOBA_WIDTH + 2 * GLA_K_WIDTH + 2 * GLA_V_WIDTH + GLA_GATE_RANK + N_BRANCHES * D_MODEL
FFN_DIM = 7 * D_MODEL // 2
N_EXPERTS = 8
TOP_K = 2
EXPERT_DIM = 7 * D_MODEL // 2
MOE_TOKEN_BLOCK = 1024
N_DENSE_LAYERS = (DEPTH + 1) // 2
N_MOE_LAYERS = DEPTH // 2
DEEPNORM_ALPHA = (2 * DEPTH) ** 0.25
DEEPNORM_BETA = (8 * DEPTH) ** -0.25
LN_EPS = 1e-5
MASK_VALUE = -1e30

kernel_name = "hybrid_moba_gla_deepnorm_moe"


def layer_norm(x, g, b):
    xf = x.astype(jnp.float32)
    mu = jnp.mean(xf, -1, keepdims=True)
    var = jnp.mean(jnp.square(xf - mu), -1, keepdims=True)
    return ((xf - mu) * lax.rsqrt(var + LN_EPS) * g.astype(jnp.float32) + b.astype(jnp.float32)).astype(x.dtype)


def rope_tables(seq):
    inv_freq = jnp.power(ROPE_THETA, -jnp.arange(0, ROPE_DIM, 2, dtype=jnp.float32) / ROPE_DIM)
    ang = jnp.arange(seq, dtype=jnp.float32)[:, None] * inv_freq[None, :]
    return jnp.cos(ang), jnp.sin(ang)


def partial_rotary(t, cos, sin):
    half = ROPE_DIM // 2
    tf = t[..., :ROPE_DIM].astype(jnp.float32)
    t1, t2 = tf[..., :half], tf[..., half:]
    c, s = cos[None, :, None, :], sin[None, :, None, :]
    rot = jnp.concatenate([t1 * c - t2 * s, t2 * c + t1 * s], -1).astype(t.dtype)
    return jnp.concatenate([rot, t[..., ROPE_DIM:]], -1)


def moba_attention(q, k, v):
    B, S, H, dh = q.shape
    nb = -(-S // MOBA_BLOCK)
    s_pad = nb * MOBA_BLOCK
    padw = ((0, 0), (0, s_pad - S), (0, 0), (0, 0))
    q, k, v = jnp.pad(q, padw), jnp.pad(k, padw), jnp.pad(v, padw)
    kb = k.reshape(B, nb, MOBA_BLOCK, H, dh)
    vb = v.reshape(B, nb, MOBA_BLOCK, H, dh)
    k_mean = jnp.mean(kb.astype(jnp.float32), axis=2)
    gate = jnp.einsum('bshd,bnhd->bhsn', q.astype(jnp.float32), k_mean)
    q_blk_id = jnp.arange(s_pad, dtype=jnp.int32) // MOBA_BLOCK
    past = jnp.arange(nb, dtype=jnp.int32)[None, :] < q_blk_id[:, None]
    gate = jnp.where(past[None, None], gate, -jnp.inf)
    k_sel = min(MOBA_TOP_BLOCKS, nb)
    _, sel = lax.top_k(gate, k_sel)
    valid = sel < q_blk_id[None, None, :, None]

    n_qc = s_pad // MOBA_QUERY_BLOCK
    qc = q.reshape(B, n_qc, MOBA_QUERY_BLOCK, H, dh).transpose(0, 1, 3, 2, 4).reshape(B * n_qc, H, MOBA_QUERY_BLOCK, dh)
    sel_c = sel.reshape(B, H, n_qc, MOBA_QUERY_BLOCK, k_sel).transpose(0, 2, 1, 3, 4).reshape(B * n_qc, H, MOBA_QUERY_BLOCK, k_sel)
    valid_c = valid.reshape(B, H, n_qc, MOBA_QUERY_BLOCK, k_sel).transpose(0, 2, 1, 3, 4).reshape(B * n_qc, H, MOBA_QUERY_BLOCK, k_sel)
    kbh = kb.transpose(0, 3, 1, 2, 4)
    vbh = vb.transpose(0, 3, 1, 2, 4)
    b_idx = jnp.repeat(jnp.arange(B, dtype=jnp.int32), n_qc)
    c_idx = jnp.tile(jnp.arange(n_qc, dtype=jnp.int32), B)
    h_idx = jnp.arange(H, dtype=jnp.int32)[:, None, None]
    scale = dh ** -0.5

    def query_block(args):
        b, c, q_b, s_idx, s_valid = args
        k_b, v_b = kbh[b], vbh[b]
        own = (c * MOBA_QUERY_BLOCK) // MOBA_BLOCK
        k_own = lax.dynamic_index_in_dim(k_b, own, axis=1, keepdims=False)
        v_own = lax.dynamic_index_in_dim(v_b, own, axis=1, keepdims=False)
        k_g = k_b[h_idx, s_idx]
        v_g = v_b[h_idx, s_idx]
        q_local = (c * MOBA_QUERY_BLOCK) % MOBA_BLOCK + jnp.arange(MOBA_QUERY_BLOCK, dtype=jnp.int32)
        causal = jnp.arange(MOBA_BLOCK, dtype=jnp.int32)[None, :] <= q_local[:, None]
        s_own = jnp.einsum('hqd,hkd->hqk', q_b, k_own).astype(jnp.float32) * scale
        s_own = jnp.where(causal[None], s_own, MASK_VALUE)
        s_g = jnp.einsum('hqd,hqnkd->hqnk', q_b, k_g).astype(jnp.float32) * scale
        s_g = jnp.where(s_valid[..., None], s_g, MASK_VALUE).reshape(H, MOBA_QUERY_BLOCK, k_sel * MOBA_BLOCK)
        p = jax.nn.softmax(jnp.concatenate([s_own, s_g], -1), axis=-1).astype(v.dtype)
        p_own = p[..., :MOBA_BLOCK]
        p_g = p[..., MOBA_BLOCK:].reshape(H, MOBA_QUERY_BLOCK, k_sel, MOBA_BLOCK)
        return jnp.einsum('hqk,hkd->hqd', p_own, v_own) + jnp.einsum('hqnk,hqnkd->hqd', p_g, v_g)

    out = lax.map(query_block, (b_idx, c_idx, qc, sel_c, valid_c))
    out = out.reshape(B, n_qc, H, MOBA_QUERY_BLOCK, dh).transpose(0, 1, 3, 2, 4).reshape(B, s_pad, H, dh)
    return out[:, :S]


def gla_attention(q, k, v, log_a):
    B, S, H, dk = q.shape
    dv = v.shape[-1]
    nc = S // GLA_CHUNK

    def chunks(t):
        return t.reshape(B, nc, GLA_CHUNK, H, t.shape[-1]).transpose(1, 0, 3, 2, 4)

    q, k, v, g = chunks(q), chunks(k), chunks(v), chunks(log_a)
    cum = jnp.cumsum(g, axis=3)
    cum_last = cum[:, :, :, -1:, :]
    q_dec = q * jnp.exp(cum)
    k_inv = k * jnp.exp(-cum)
    k_to_end = k * jnp.exp(cum_last - cum)
    causal = jnp.tril(jnp.ones((GLA_CHUNK, GLA_CHUNK), dtype=bool))
    att = jnp.where(causal, jnp.einsum('nbhcd,nbhkd->nbhck', q_dec, k_inv), 0.0)
    intra = jnp.einsum('nbhck,nbhkv->nbhcv', att, v)
    d_state = jnp.einsum('nbhcd,nbhcv->nbhdv', k_to_end, v)
    decay = jnp.exp(cum_last[:, :, :, 0, :])

    def step(state, xs):
        q_c, dec_c, ds_c = xs
        inter = jnp.einsum('bhcd,bhdv->bhcv', q_c, state)
        return dec_c[..., None] * state + ds_c, inter

    state0 = jnp.zeros((B, H, dk, dv), jnp.float32)
    _, inter = lax.scan(step, state0, (q_dec, decay, d_state))
    out = intra + inter
    return out.transpose(1, 0, 3, 2, 4).reshape(B, S, H, dv)


def head_norm(o, g):
    mu = jnp.mean(o, -1, keepdims=True)
    var = jnp.mean(jnp.square(o - mu), -1, keepdims=True)
    on = (o - mu) * lax.rsqrt(var + LN_EPS)
    B, S = o.shape[0], o.shape[1]
    return on.reshape(B, S, -1) * g.astype(jnp.float32)


def token_mixer(h, cos, sin, w_in, w_gla_gate_up, b_gla_gate, gla_norm_g, w_branch_a, w_branch_b, w_out):
    B, S, _ = h.shape
    proj = h @ w_in
    offsets = [int(o) for o in np.cumsum(IN_SPLITS)[:-1]]
    qa, ka, va, qg, kg, vg, rg, lg, gate_a, gate_b = jnp.split(proj, offsets, axis=-1)

    qa = partial_rotary(qa.reshape(B, S, MOBA_HEADS, MOBA_HEAD_DIM), cos, sin)
    ka = partial_rotary(ka.reshape(B, S, MOBA_HEADS, MOBA_HEAD_DIM), cos, sin)
    y_a = moba_attention(qa, ka, va.reshape(B, S, MOBA_HEADS, MOBA_HEAD_DIM)).reshape(B, S, MOBA_WIDTH)

    log_a = jax.nn.log_sigmoid((lg @ w_gla_gate_up + b_gla_gate).astype(jnp.float32)) / GLA_GATE_TAU
    o = gla_attention(
        qg.reshape(B, S, GLA_HEADS, GLA_KEY_DIM).astype(jnp.float32) * (GLA_KEY_DIM ** -0.5),
        kg.reshape(B, S, GLA_HEADS, GLA_KEY_DIM).astype(jnp.float32),
        vg.reshape(B, S, GLA_HEADS, GLA_VAL_DIM).astype(jnp.float32),
        log_a.reshape(B, S, GLA_HEADS, GLA_KEY_DIM))
    y_b = head_norm(o, gla_norm_g).astype(h.dtype) * jax.nn.silu(rg)

    merged = jax.nn.sigmoid(gate_a) * (y_a @ w_branch_a) + jax.nn.sigmoid(gate_b) * (y_b @ w_branch_b)
    return merged @ w_out


def swiglu(h, w_gate, w_up, w_down):
    return (jax.nn.silu(h @ w_gate) * (h @ w_up)) @ w_down


def moe_swiglu(h, w_router, w_gate, w_up, w_down):
    B, S, D = h.shape
    T = B * S
    t = h.reshape(T, D)
    logits = (t @ w_router).astype(jnp.float32)
    top_val, top_idx = lax.top_k(logits, TOP_K)
    top_w = jax.nn.softmax(top_val, axis=-1)
    combine = jnp.sum(jax.nn.one_hot(top_idx, N_EXPERTS, dtype=jnp.float32) * top_w[..., None], axis=1).astype(h.dtype)
    tb = math.gcd(T, MOE_TOKEN_BLOCK)
    nblk = T // tb

    def token_block(args):
        xb, cb = args
        hid = jax.nn.silu(jnp.einsum('td,edf->tef', xb, w_gate)) * jnp.einsum('td,edf->tef', xb, w_up)
        return jnp.einsum('tef,efd->td', hid * cb[:, :, None], w_down)

    y = lax.map(token_block, (t.reshape(nblk, tb, D), combine.reshape(nblk, tb, N_EXPERTS)))
    return y.reshape(B, S, D)


def setup_inputs(seed: int = 0) -> dict:
    key = jax.random.key(seed)
    ks = jax.random.split(key, 20)

    def nrm(k, shape, scale):
        return jax.random.normal(k, shape, jnp.float32) * scale

    return {
        "x": nrm(ks[0], (BATCH, SEQ, D_MODEL), 1.0),
        "w_in": nrm(ks[1], (DEPTH, D_MODEL, IN_COLS), D_MODEL ** -0.5),
        "w_gla_gate_up": nrm(ks[2], (DEPTH, GLA_GATE_RANK, GLA_K_WIDTH), GLA_GATE_RANK ** -0.5),
        "b_gla_gate": nrm(ks[3], (DEPTH, GLA_K_WIDTH), 0.1),
        "gla_norm_g": 1.0 + nrm(ks[4], (DEPTH, GLA_V_WIDTH), 0.02),
        "w_branch_a": nrm(ks[5], (DEPTH, MOBA_WIDTH, D_MODEL), MOBA_WIDTH ** -0.5 * DEEPNORM_BETA),
        "w_branch_b": nrm(ks[6], (DEPTH, GLA_V_WIDTH, D_MODEL), GLA_V_WIDTH ** -0.5 * DEEPNORM_BETA),
        "w_out": nrm(ks[7], (DEPTH, D_MODEL, D_MODEL), D_MODEL ** -0.5 * DEEPNORM_BETA),
        "ln_mix_g": 1.0 + nrm(ks[8], (DEPTH, D_MODEL), 0.02),
        "ln_mix_b": nrm(ks[9], (DEPTH, D_MODEL), 0.02),
        "ffn_w_gate": nrm(ks[10], (N_DENSE_LAYERS, D_MODEL, FFN_DIM), D_MODEL ** -0.5),
        "ffn_w_up": nrm(ks[11], (N_DENSE_LAYERS, D_MODEL, FFN_DIM), D_MODEL ** -0.5),
        "ffn_w_down": nrm(ks[12], (N_DENSE_LAYERS, FFN_DIM, D_MODEL), FFN_DIM ** -0.5 * DEEPNORM_BETA),
        "moe_w_router": nrm(ks[13], (N_MOE_LAYERS, D_MODEL, N_EXPERTS), D_MODEL ** -0.5),
        "moe_w_gate": nrm(ks[14], (N_MOE_LAYERS, N_EXPERTS, D_MODEL, EXPERT_DIM), D_MODEL ** -0.5),
        "moe_w_up": nrm(ks[15], (N_MOE_LAYERS, N_EXPERTS, D_MODEL, EXPERT_DIM), D_MODEL ** -0.5),
        "moe_w_down": nrm(ks[16], (N_MOE_LAYERS, N_EXPERTS, EXPERT_DIM, D_MODEL), EXPERT_DIM ** -0.5 * DEEPNORM_BETA),
        "ln_ffn_g": 1.0 + nrm(ks[17], (DEPTH, D_MODEL), 0.02),
        "ln_ffn_b": nrm(ks[18], (DEPTH, D_MODEL), 0.02),
    }


def reference(x, w_in, w_gla_gate_up, b_gla_gate, gla_norm_g, w_branch_a, w_branch_b, w_out,
              ln_mix_g, ln_mix_b, ffn_w_gate, ffn_w_up, ffn_w_down, moe_w_router, moe_w_gate,
              moe_w_up, moe_w_down, ln_ffn_g, ln_ffn_b):
    cos, sin = rope_tables(x.shape[1])
    for layer in range(DEPTH):
        mix = token_mixer(x, cos, sin, w_in[layer], w_gla_gate_up[layer], b_gla_gate[layer],
                          gla_norm_g[layer], w_branch_a[layer], w_branch_b[layer], w_out[layer])
        x = layer_norm(DEEPNORM_ALPHA * x + mix, ln_mix_g[layer], ln_mix_b[layer])
        j = layer // 2
        if layer % 2 == 0:
            f = swiglu(x, ffn_w_gate[j], ffn_w_up[j], ffn_w_down[j])
        else:
            f = moe_swiglu(x, moe_w_router[j], moe_w_gate[j], moe_w_up[j], moe_w_down[j])
        x = layer_norm(DEEPNORM_ALPHA * x + f, ln_ffn_g[layer], ln_ffn_b[layer])
    return x
```

```python
import math
from contextlib import ExitStack

import numpy as np
import concourse.bass as bass
import concourse.mybir as mybir
from concourse.bass_utils import run_bass_kernel_spmd

F32 = mybir.dt.float32
BF16 = mybir.dt.bfloat16
I32 = mybir.dt.int32
AF = mybir.ActivationFunctionType
ALU = mybir.AluOpType
AX = mybir.AxisListType

D = 1024
DEPTH = 2
MH, MD = 8, 64
MBLK = 256
GH, GK = 4, 128
FF = 3584
NE = 8
IN_COLS = 5648
ALPHA = (2 * DEPTH) ** 0.25
LN_EPS = 1e-5
G = 512
NEG = -240000.0
BIG = 1.0e30

ENGS = ("pe", "act", "dve", "pool", "sp")
EPOCH = 12000
DMA_EPOCH = 1500


class Res:
    __slots__ = ("name", "writer", "readers", "stream_w", "stream_r")

    def __init__(self, name):
        self.name = name
        self.writer = None
        self.readers = []
        self.stream_w = None
        self.stream_r = None


class Stream:
    __slots__ = ("sem", "count", "name", "q")

    def __init__(self, name):
        self.name = name
        self.sem = None
        self.count = 0
        self.q = None


class Op:
    __slots__ = ("eng", "fn", "deps", "needs_inc", "epoch", "val", "is_dma", "stream", "dval")

    def __init__(self, eng, fn):
        self.eng = eng
        self.fn = fn
        self.deps = []
        self.needs_inc = False
        self.epoch = 0
        self.val = 0
        self.is_dma = False
        self.stream = None
        self.dval = 0


class Sched:
    def __init__(self, nc, stack):
        self.nc = nc
        self.stack = stack
        self.ops = {e: [] for e in ENGS}
        self.nsem = 0
        self.all_res = []
        self.live_streams = []
        self.free_streams = {"pool": [], "sp": []}

    def res(self, name):
        r = Res(name)
        self.all_res.append(r)
        return r

    def _newsem(self, name):
        self.nsem += 1
        return self.stack.enter_context(self.nc.semaphore(f"{name[:40]}_{self.nsem}"))

    def _collect(self, op, reads, writes):
        deps = []
        for r in reads:
            if r.writer is not None:
                deps.append((r.writer, "raw"))
        for w in writes:
            if w.writer is not None:
                deps.append((w.writer, "waw"))
            for t in w.readers:
                deps.append((t, "war"))
        seen = set()
        for t, kind in deps:
            if t is op or id(t) in seen:
                continue
            if (not t.is_dma) and (not op.is_dma) and t.eng == op.eng:
                if op.eng == "pe":
                    continue
            seen.add(id(t))
            op.deps.append(t)
            if not t.is_dma:
                t.needs_inc = True
        for r in reads:
            if not op.is_dma:
                r.readers = [t for t in r.readers if t.is_dma or t.eng != op.eng]
            r.readers.append(op)
        for w in writes:
            w.writer = op
            w.readers = []

    def op(self, eng, fn, reads=(), writes=()):
        o = Op(eng, fn)
        self._collect(o, reads, writes)
        self.ops[eng].append(o)
        return o

    def new_stream(self, name):
        st = Stream(name)
        st.sem = self._newsem(name[:48])
        return st

    def dma(self, q, out, in_, reads=(), writes=(), sres=None, load=True, stream=None, fn=None):
        o = Op(q, fn if fn is not None else (lambda e: e.dma_start(out=out, in_=in_)))
        o.is_dma = True
        st = stream if stream is not None else (sres.stream_w if load else sres.stream_r)
        if stream is None and (st is None or st.count >= DMA_EPOCH or st.q != q):
            fl = self.free_streams[q]
            while fl and fl[-1].count >= DMA_EPOCH - 64:
                fl.pop()
            if fl:
                st = fl.pop()
            else:
                st = Stream(sres.name + ("_w" if load else "_r"))
                st.sem = self._newsem(st.name[:48])
                st.q = q
            self.live_streams.append(st)
            if load:
                sres.stream_w = st
            else:
                sres.stream_r = st
        st.count += 1
        o.stream = st
        o.dval = st.count * 16
        self._collect(o, reads, writes)
        self.ops[q].append(o)
        return o

    def barrier(self):
        allr = list(self.all_res)
        for e in ENGS:
            if e == "sp":
                continue
            self.op(e, lambda e_: e_.drain(), reads=(), writes=allr)
        for r in allr:
            r.stream_w = None
            r.stream_r = None
        for st in self.live_streams:
            self.free_streams[st.q].append(st)
        self.live_streams = []

    def emit(self, final_tokens=()):
        nc = self.nc
        esems = {}
        for e in ENGS:
            cnt = 0
            ep = 0
            for o in self.ops[e]:
                if o.is_dma or not o.needs_inc:
                    continue
                if cnt >= EPOCH:
                    ep += 1
                    cnt = 0
                cnt += 1
                o.epoch = ep
                o.val = cnt
                if (e, ep) not in esems:
                    esems[(e, ep)] = self._newsem(f"s_{e}_{ep}")
        stats = {e: [0, 0] for e in ENGS}

        def run(e):
            def body(eng):
                known = {}
                for o in self.ops[e]:
                    need = {}
                    for t in o.deps:
                        if t.is_dma:
                            sem, val = t.stream.sem, t.dval
                        else:
                            sem, val = esems[(t.eng, t.epoch)], t.val
                        k = id(sem)
                        if k not in need or need[k][1] < val:
                            need[k] = (sem, val)
                    for k, (sem, val) in need.items():
                        if known.get(k, 0) >= val:
                            continue
                        known[k] = val
                        eng.wait_ge(sem, val)
                        stats[e][1] += 1
                    ins = o.fn(eng)
                    stats[e][0] += 1
                    if o.is_dma:
                        ins.then_inc(o.stream.sem, 16)
                    elif o.needs_inc:
                        ins.then_inc(esems[(e, o.epoch)], 1)
                if e == "pool":
                    for t in final_tokens:
                        eng.wait_ge(t.stream.sem, t.dval)
            return body

        with nc.Block() as block:
            block.tensor(run("pe"))
            block.scalar(run("act"))
            block.vector(run("dve"))
            block.gpsimd(run("pool"))
            block.sync(run("sp"))
        return stats


INP = [("qm", 0), ("km", 512), ("vm", 1024), ("qg", 1536), ("kg", 2048), ("vg", 2560), ("rg", 3072),
       ("ga0", 3600), ("ga1", 4112), ("gb0", 4624), ("gb1", 5136)]
LG_OFF = 3584


def block_table():
    tab = {}
    n = 0

    def add(key):
        nonlocal n
        tab[key] = n
        n += 1

    for l in range(DEPTH):
        for name, _ in INP:
            add((l, name))
        add((l, "lg"))
        add((l, "wa"))
        add((l, "wb"))
        add((l, "wo0"))
        add((l, "wo1"))
    for i in range(7):
        add(("f", "g", i))
    for i in range(7):
        add(("f", "u", i))
    for i in range(7):
        add(("f", "d", i))
    for e in range(NE):
        for i in range(7):
            add(("m", e, "g", i))
        for i in range(7):
            add(("m", e, "u", i))
        for i in range(7):
            add(("m", e, "d", i))
    return tab, n


class Cfg:
    def __init__(self, S=4096, NSEQ=2, layers=(0, 1), do_mix=True, do_ffn=True, dbg=False, parts="AB", skip=()):
        self.S = S
        self.NSEQ = NSEQ
        self.T = S * NSEQ
        self.layers = layers
        self.do_mix = do_mix
        self.do_ffn = do_ffn
        self.dbg = dbg
        self.parts = parts
        self.sparse = True
        self.skip = set(skip)


def build_program(cfg):
    S_, NSEQ, T = cfg.S, cfg.NSEQ, cfg.T
    NG = S_ // G
    NTS = S_ // 128
    NBK = S_ // MBLK
    nc = bass.Bass("TRN2", target_bir_lowering=False)

    def din(name, shape, dt=F32):
        return nc.dram_tensor(name, list(shape), dt, kind="ExternalInput").ap()

    x_in = din("x", [T, D])
    w_in = din("w_in", [DEPTH, D, IN_COLS])
    w_up = din("w_gla_gate_up", [DEPTH, 16, 512])
    b_gg = din("b_gla_gate", [DEPTH, 512])
    gn_g = din("gla_norm_g", [DEPTH, 512])
    w_ba = din("w_branch_a", [DEPTH, 512, D])
    w_bb = din("w_branch_b", [DEPTH, 512, D])
    w_o = din("w_out", [DEPTH, D, D])
    ln_mg = din("ln_mix_g", [DEPTH, D])
    ln_mb = din("ln_mix_b", [DEPTH, D])
    f_g = din("ffn_w_gate", [1, D, FF])
    f_u = din("ffn_w_up", [1, D, FF])
    f_d = din("ffn_w_down", [1, FF, D])
    m_r = din("moe_w_router", [1, D, NE])
    m_g = din("moe_w_gate", [1, NE, D, FF])
    m_u = din("moe_w_up", [1, NE, D, FF])
    m_d = din("moe_w_down", [1, NE, FF, D])
    ln_fg = din("ln_ffn_g", [DEPTH, D])
    ln_fb = din("ln_ffn_b", [DEPTH, D])
    c_ident = din("c_ident", [128, 128])
    c_cc = din("c_cc", [S_, 16])
    c_ss = din("c_ss", [S_, 16])
    c_cmb = din("c_cmb", [4, 128, 512])
    c_oh = din("c_oh", [16, S_])
    c_pm = din("c_pm", [16, 128])
    c_pa = din("c_pa", [16, 128])
    c_own = din("c_own", [16, 128])
    c_u = din("c_u", [128, 128])
    c_tri = din("c_tri", [128, 128])
    m_rT = din("moe_w_router_T", [NE, D])
    NTT = T // 128
    NGRP = (2 * T) // 512 + NE
    NSLOT = NGRP * 512
    c_tok = din("c_tok", [128, NTT], I32)
    c_wcst = din("c_wcst", [128, 21])
    c_jj = din("c_jj", [128, NGRP])
    c_thr = din("c_thr", [128, 16])
    c_tris = din("c_tris", [128, 128])

    tab, NBLKS = block_table()
    BASE_M = tab[("m", 0, "g", 0)]
    wsc_d = nc.dram_tensor("wsc", [BASE_M, 128, 4096], BF16, kind="Internal").ap()
    wsm = nc.dram_tensor("wsm", [NE * 21, 128, 4096], BF16, kind="Internal").ap()

    class _W:
        def __getitem__(self, b):
            return wsc_d[b] if b < BASE_M else wsm[b - BASE_M]
    wsc = _W()
    x1s = nc.dram_tensor("x1s", [T, D], F32, kind="Internal").ap()
    xs1 = nc.dram_tensor("xs1", [T, D], F32, kind="Internal").ap()
    yas = nc.dram_tensor("yas", [T // G, 128, 4, G], BF16, kind="Internal").ap()
    s2t = nc.dram_tensor("s2t", [NSLOT, 1], I32, kind="Internal").ap()
    ygs = nc.dram_tensor("ygs", [NSLOT, D], F32, kind="Internal").ap()
    y_out = nc.dram_tensor("y", [T, D], F32, kind="ExternalOutput").ap()
    dbg_t = {}
    if cfg.dbg:
        NGT = T // G
        for nm, shp in (("ya", [NGT, 128, 4, 512]), ("ybT", [NGT, 128, 4, G]), ("yaT", [NGT, 128, 4, G]), ("QT", [NGT, 128, MH, G]),
                        ("mT", [NGT, 128, 8, G]), ("BT", [NGT, 16, MH, G])):
            dbg_t[nm] = nc.dram_tensor("dbg_" + nm, shp, BF16, kind="ExternalOutput").ap()

    with ExitStack() as top:
        S = Sched(nc, top)
        r_wsc = [S.res(f"wsc{i}") for i in range(NBLKS)]
        r_x1s = [S.res(f"x1s{i}") for i in range(T // G)]
        r_xs1 = [S.res(f"xs1{i}") for i in range(T // G)]
        r_yas = [S.res(f"yas{i}") for i in range(T // G)]
        final_tokens = []
        r_dbg = S.res("dbg")

        pre = {"st": None, "ops": []}

        def pre_begin(name):
            pre["st"] = S.new_stream(name)
            pre["ops"] = []

        def pre_end():
            for o in pre["ops"]:
                o.dval = pre["st"].count * 16

        def conv(key, src, view):
            b = tab[key]
            if view == "c8":
                dst = wsc[b].rearrange("p (c n) -> p c n", c=8)
                s = src.rearrange("(c p) n -> p c n", p=128)
            elif view == "c4":
                dst = wsc[b].rearrange("p (c n) -> p c n", c=4)
                s = src.rearrange("(c p) n -> p c n", p=128)
            elif view == "lg":
                dst = wsc[b][:, 0:128].rearrange("p (c n) -> p c n", c=8)
                s = src.rearrange("(c p) n -> p c n", p=128)
            pre["ops"].append(S.dma("pool", dst, s, writes=[r_wsc[b]], sres=r_wsc[b], load=True, stream=pre["st"]))

        def prepass_layer(l):
            pre_begin(f"pre_l{l}a")
            for name, off in INP[:3]:
                conv((l, name), w_in[l][:, off:off + 512], "c8")
            pre_end()
            pre_begin(f"pre_l{l}")
            for name, off in INP[3:]:
                conv((l, name), w_in[l][:, off:off + 512], "c8")
            conv((l, "lg"), w_in[l][:, LG_OFF:LG_OFF + 16], "lg")
            conv((l, "wa"), w_ba[l], "c4")
            conv((l, "wb"), w_bb[l], "c4")
            conv((l, "wo0"), w_o[l][:, 0:512], "c8")
            conv((l, "wo1"), w_o[l][:, 512:1024], "c8")
            pre_end()

        def prepass_ffn():
            pre_begin("pre_ffn")
            for i in range(7):
                conv(("f", "g", i), f_g[0][:, i * 512:(i + 1) * 512], "c8")
                conv(("f", "u", i), f_u[0][:, i * 512:(i + 1) * 512], "c8")
                conv(("f", "d", i), f_d[0][i * 512:(i + 1) * 512, :], "c4")
            pre_end()

        def prepass_moe(experts=range(NE)):
            for e in experts:
                pre_begin(f"pre_moe{e}")
                for i in range(7):
                    conv(("m", e, "g", i), m_g[0][e][:, i * 512:(i + 1) * 512], "c8")
                    conv(("m", e, "u", i), m_u[0][e][:, i * 512:(i + 1) * 512], "c8")
                    conv(("m", e, "d", i), m_d[0][e][i * 512:(i + 1) * 512, :], "c4")
                pre_end()

        sbn = {"n": 0}

        def sb(st, name, shape, dt):
            sbn["n"] += 1
            return st.enter_context(nc.sbuf_tensor(f"{name}_{sbn['n']}", list(shape), dt))

        psb = [top.enter_context(nc.psum_tensor(f"ps{i}", [128, 512], F32)) for i in range(8)]
        r_ps = [S.res(f"ps{i}") for i in range(8)]

        identb = sb(top, "identb", [128, 128], BF16)
        r_ident = S.res("ident")
        S.dma("pool", identb[:], c_ident, writes=[r_ident], sres=r_ident)
        identf = sb(top, "identf", [128, 128], F32)
        r_identf = S.res("identf")
        S.dma("pool", identf[:], c_ident, writes=[r_identf], sres=r_identf)

        wring = []
        r_wring = []
        wstate = {"i": 0}

        def set_ring(st, n):
            wring[:] = [sb(st, f"wring{i}", [128, 4096], BF16) for i in range(n)]
            r_wring[:] = [S.res(f"wring{i}") for i in range(n)]
            wstate["i"] = 0

        def wload(key, view):
            i = wstate["i"] % len(wring)
            wstate["i"] += 1
            b = tab[key]
            if view == "lg":
                S.dma("sp", wring[i][:, 0:128], wsc[b][:, 0:128], reads=[r_wsc[b]], writes=[r_wring[i]], sres=r_wring[i])
            else:
                S.dma("sp", wring[i][:], wsc[b], reads=[r_wsc[b]], writes=[r_wring[i]], sres=r_wring[i])
            t = wring[i]
            if view == "c8":
                v = t[:].rearrange("p (c n) -> p c n", c=8)
            elif view == "c4":
                v = t[:].rearrange("p (c n) -> p c n", c=4)
            else:
                v = t[:, 0:128].rearrange("p (c n) -> p c n", c=8)
            return v, r_wring[i]

        def MM(out, lhsT, rhs, start, stop, R, W, **kw):
            S.op("pe", lambda e: e.matmul(out, lhsT=lhsT, rhs=rhs, start=start, stop=stop, **kw), reads=R, writes=W)

        def TR(out, in_, ident, R, W):
            S.op("pe", lambda e: e.transpose(out=out, in_=in_, identity=ident), reads=R, writes=W)

        def ACT(out, in_, func, R, W, **kw):
            S.op("act", lambda e: e.activation(out=out, in_=in_, func=func, **kw), reads=R, writes=W)

        def V(eng, meth, R, W, **kw):
            S.op(eng, lambda e: getattr(e, meth)(**kw), reads=R, writes=W)

        def layer_norm_tile(st_tiles, tin, r_tin, gb, bb, r_gbl, out, r_out, eng="dve"):
            stt, r_st = st_tiles
            V("dve", "bn_stats", [r_tin], [r_st], out=stt[:, 0:6], in_=tin[:, 0:512])
            V("dve", "bn_stats", [r_tin], [r_st], out=stt[:, 6:12], in_=tin[:, 512:1024])
            V("dve", "bn_aggr", [r_st], [r_st], out=stt[:, 12:14], in_=stt[:, 0:12])
            V("dve", "tensor_scalar_add", [r_st], [r_st], out=stt[:, 13:14], in0=stt[:, 13:14], scalar1=LN_EPS)
            ACT(stt[:, 13:14], stt[:, 13:14], AF.Sqrt, [r_st], [r_st])
            V("dve", "reciprocal", [r_st], [r_st], out=stt[:, 13:14], in_=stt[:, 13:14])
            V("dve", "tensor_scalar", [r_tin, r_st], [r_tin], out=tin[:], in0=tin[:], scalar1=stt[:, 12:13],
              scalar2=stt[:, 13:14], op0=ALU.subtract, op1=ALU.mult)
            V(eng, "tensor_tensor", [r_tin] + r_gbl, [r_tin], out=tin[:], in0=tin[:], in1=gb[:], op=ALU.mult)
            V(eng, "tensor_tensor", [r_tin] + r_gbl, [r_out], out=out, in0=tin[:], in1=bb[:], op=ALU.add)

        def phase_mixer(l, x_src, r_xsrc, hooks=()):
            hooks = list(hooks)
            pbig = [0, 1]
            pst = [3, 4]
            PACCS, PTR, PMISC = (2, 5), 6, 7

            def cload(st, name, shape, dt, src, q="pool"):
                t = sb(st, name, shape, dt)
                r = S.res(name)
                S.dma(q, t[:], src, writes=[r], sres=r)
                return t, r

            with ExitStack() as st:
              if "A" in cfg.parts:
                  set_ring(st, 4)
                  cc, r_cc = cload(st, "cc", [128, NTS, 16], F32, c_cc.rearrange("(t p) k -> p t k", p=128))
                  ss, r_ss = cload(st, "ss", [128, NTS, 16], F32, c_ss.rearrange("(t p) k -> p t k", p=128))
                  cmb, r_cmb = cload(st, "cmb", [128, 4, 512], BF16, c_cmb.rearrange("c p n -> p c n"))
                  oh, r_oh = cload(st, "oh", [16, S_], BF16, c_oh)
                  onesb = sb(st, "onesb", [128, 1], BF16)
                  r_ones = S.res("onesb")
                  V("pool", "memset", [], [r_ones], ap=onesb[:], constant=1.0)
                  zerob = sb(st, "zerob", [128, 512], BF16)
                  r_zero = S.res("zerob")
                  V("pool", "memset", [], [r_zero], ap=zerob[:], constant=0.0)

                  KT = sb(st, "KT", [128, 4, S_], BF16)
                  r_KT = [S.res(f"KT{g}") for g in range(NG)]
                  VA = sb(st, "VA", [128, NTS, MH, MD + 1], BF16)
                  r_VA = [S.res(f"VA{g}") for g in range(NG)]
                  V("pool", "memset", [], r_VA, ap=VA[:, :, :, MD:MD + 1], constant=1.0)
                  kmT = sb(st, "kmT", [128, 4, 16], BF16)
                  r_kmT = S.res("kmT")

                  NXB = 2
                  xf = [sb(st, f"xf{i}", [128, D], F32) for i in range(NXB)]
                  r_xf = [S.res(f"xf{i}") for i in range(NXB)]
                  xb = [sb(st, f"xb{i}", [128, D], BF16) for i in range(NXB)]
                  r_xb = [S.res(f"xb{i}") for i in range(NXB)]
                  xT = sb(st, "xT", [128, 8, G], BF16)
                  r_xT = S.res("xT")
                  QA = sb(st, "QA", [128, 4, 512], BF16)
                  r_QA = [S.res(f"QA{t}") for t in range(4)]
                  KA = sb(st, "KA", [128, 4, 512], BF16)
                  r_KA = [S.res(f"KA{t}") for t in range(4)]
                  QTs = [sb(st, f"QT{i}", [128, MH, G], BF16) for i in range(2)]
                  r_QTs = [S.res(f"QT{i}") for i in range(2)]
                  for i in range(2):
                      V("pool", "memset", [], [r_QTs[i]], ap=QTs[i][:], constant=0.0)
                  BTs = [sb(st, f"BT{i}", [16, MH, G], BF16) for i in range(2)]
                  r_BTs = [S.res(f"BT{i}") for i in range(2)]
                  gsb = sb(st, "gsb", [128, 6, 128], F32)
                  r_gsb = S.res("gsb")
                  gm = sb(st, "gm", [128, 3, 8], F32)
                  ksum = sb(st, "ksum", [128, 16], F32)
                  r_gm = S.res("gm")
                  bias_tm = sb(st, "bias_tm", [128, 128], BF16)
                  r_btm = S.res("bias_tm")
                  pmt = sb(st, "pmt", [128, 2, 3, 128], F32)
                  r_pmt = S.res("pmt")
                  rp = sb(st, "rp", [128, 2, 8, 16], F32)
                  r_rp = S.res("rp")
                  NPT = 4
                  PT = [sb(st, f"PT{i}", [128, G], BF16) for i in range(NPT)]
                  r_PT = [S.res(f"PT{i}") for i in range(NPT)]
                  rcp = sb(st, "rcp", [128, 2, 4], F32)
                  r_rcp = [S.res("rcp0"), S.res("rcp1")]
                  ya = sb(st, "ya", [128, 4, 512], BF16)
                  r_ya = S.res("ya")
                  yaT = [sb(st, f"yaT{i}", [128, 4, G], BF16) for i in range(2)]
                  r_yaT = [S.res(f"yaT{i}") for i in range(2)]
                  ring = {"big": 0, "st": 0, "pt": 0, "x": 0}

                  def nbig():
                      i = pbig[ring["big"] % len(pbig)]
                      ring["big"] += 1
                      return i

                  def prelude(s, g):
                      QTb, r_QTb, BTb, r_BTb = QTs[g % 2], r_QTs[g % 2], BTs[g % 2], r_BTs[g % 2]
                      tok0 = s * S_ + g * G
                      gi = tok0 // G
                      for t in range(4):
                          i = ring["x"] % NXB
                          ring["x"] += 1
                          r0 = tok0 + t * 128
                          S.dma("sp", xf[i][:], x_src[r0:r0 + 128, :], reads=[r_xsrc[gi]], writes=[r_xf[i]], sres=r_xf[i])
                          ACT(xb[i][:], xf[i][:], AF.Copy, [r_xf[i]], [r_xb[i]])
                          ptr = psb[PTR][:].bitcast(BF16).rearrange("p (c n) -> p c n", c=8)
                          for c in range(8):
                              TR(ptr[:, c, :], xb[i][:, c * 128:(c + 1) * 128], identb[:], [r_xb[i], r_ident], [r_ps[PTR]])
                          V("dve", "tensor_copy", [r_ps[PTR]], [r_xT], out=xT[:, :, t * 128:(t + 1) * 128], in_=ptr)

                      def proj_tm(key, consume):
                          wv, rw = wload((l, key), "c8")
                          for t in range(4):
                              pi = nbig()
                              for c in range(8):
                                  MM(psb[pi][:], xT[:, c, t * 128:(t + 1) * 128], wv[:, c, :], c == 0, c == 7,
                                     [r_xT, rw], [r_ps[pi]])
                              consume(t, psb[pi], r_ps[pi])

                      def rope_to(dst, r_dst):
                          def consume(t, ps, rps):
                              tt = g * 4 + t
                              p3 = ps[:].rearrange("p (h d) -> p h d", h=MH)
                              d3 = dst[:, t, :].rearrange("p (h d) -> p h d", h=MH)
                              ccb = cc[:, tt, :].unsqueeze(1).broadcast_to([128, MH, 16])
                              ssb = ss[:, tt, :].unsqueeze(1).broadcast_to([128, MH, 16])
                              V("dve", "tensor_tensor", [rps, r_cc], [r_rp], out=rp[:, 0, :, :], in0=p3[:, :, 0:16], in1=ccb, op=ALU.mult)
                              V("dve", "tensor_tensor", [rps, r_ss], [r_rp], out=rp[:, 1, :, 0:8], in0=p3[:, :, 8:16], in1=ssb[:, :, 0:8], op=ALU.mult)
                              V("dve", "tensor_tensor", [rps, r_ss], [r_rp], out=rp[:, 1, :, 8:16], in0=p3[:, :, 0:8], in1=ssb[:, :, 8:16], op=ALU.mult)
                              V("dve", "tensor_tensor", [r_rp], [r_dst[t]], out=d3[:, :, 0:16], in0=rp[:, 0, :, :], in1=rp[:, 1, :, :], op=ALU.add)
                              ACT(d3[:, :, 16:64], p3[:, :, 16:64], AF.Copy, [rps], [r_dst[t]])
                          return consume

                      proj_tm("qm", rope_to(QA, r_QA))
                      proj_tm("km", rope_to(KA, r_KA))

                      def cons_v(t, ps, rps):
                          tt = g * 4 + t
                          ACT(VA[:, tt, :, 0:MD], ps[:].rearrange("p (h d) -> p h d", h=MH), AF.Copy, [rps], [r_VA[g]])
                      proj_tm("vm", cons_v)

                      yield
                      ptr4 = psb[PTR][:].bitcast(BF16).rearrange("p (c n) -> p c n", c=8)
                      for t in range(4):
                          tt = g * 4 + t
                          for pr in range(4):
                              TR(ptr4[:, pr, :], KA[:, t, pr * 128:(pr + 1) * 128], identb[:], [r_KA[t], r_ident], [r_ps[PTR]])
                          V("dve", "tensor_copy", [r_ps[PTR]], [r_KT[g]], out=KT[:, :, tt * 128:(tt + 1) * 128], in_=ptr4[:, 0:4, :])
                      pm3 = psb[PMISC][:, 0:16].rearrange("p (a b) -> p a b", a=4)
                      for t in range(4):
                          for pr in range(4):
                              if "ksum" in cfg.skip:
                                  continue
                              MM(pm3[:, pr, t:t + 1], KA[:, t, pr * 128:(pr + 1) * 128], onesb[:, 0:1], True, True,
                                 [r_KA[t], r_ones], [r_ps[PMISC]])
                      ks3 = ksum[:].rearrange("p (a b) -> p a b", a=4)
                      if "ksum" in cfg.skip:
                          V("dve", "memset", [], [r_gm], ap=ksum[:], constant=0.0)
                      else:
                          V("dve", "tensor_copy", [r_ps[PMISC]], [r_gm], out=ks3, in_=pm3)
                      for bb_ in range(2):
                          blk = g * 2 + bb_
                          V("dve", "tensor_tensor", [r_gm], [r_gm], out=gm[:, 0, 0:4], in0=ks3[:, :, 2 * bb_],
                            in1=ks3[:, :, 2 * bb_ + 1], op=ALU.add)
                          V("dve", "tensor_scalar_mul", [r_gm], [r_kmT], out=kmT[:, :, blk], in0=gm[:, 0, 0:4], scalar1=1.0 / MBLK)

                      for t in range(4):
                          for pr in range(4):
                              TR(ptr4[:, pr, :], QA[:, t, pr * 128:(pr + 1) * 128], identb[:], [r_QA[t], r_ident], [r_ps[PTR]])
                          QT4 = QTb[:].rearrange("p (a b) n -> p a b n", b=2)
                          V("dve", "tensor_copy", [r_ps[PTR]], [r_QTb], out=QT4[0:64, :, 0, t * 128:(t + 1) * 128], in_=ptr4[0:64, 0:4, :])
                          V("dve", "tensor_copy", [r_ps[PTR]], [r_QTb], out=QT4[64:128, :, 1, t * 128:(t + 1) * 128], in_=ptr4[64:128, 0:4, :])

                      for bb_ in range(2):
                          blk = g * 2 + bb_
                          S.dma("sp", pmt[:, bb_, 0, :], c_pm[blk].partition_broadcast(128), writes=[r_pmt], sres=r_pmt)
                          S.dma("sp", pmt[:, bb_, 1, :], c_pa[blk].partition_broadcast(128), writes=[r_pmt], sres=r_pmt)
                          S.dma("sp", pmt[:, bb_, 2, :], c_own[blk].partition_broadcast(128), writes=[r_pmt], sres=r_pmt)

                      yield
                      if "gate" in cfg.skip:
                          V("dve", "memset", [], [r_BTb], ap=BTb[:], constant=0.0)
                      for t in range(4 if "gate" not in cfg.skip else 0):
                          if t == 2:
                              yield
                          bb_ = t // 2
                          pg = psb[PMISC][:, 128:256]
                          for h in range(MH):
                              pr, hh = h // 2, h % 2
                              MM(pg[:, h * 16:(h + 1) * 16], QTb[:, h, t * 128:(t + 1) * 128],
                                 kmT[:, pr, :], True, True, [r_QTb, r_kmT], [r_ps[PMISC]])
                          g0 = gsb[:, 0, :]
                          g1 = gsb[:, 1, :]
                          e1 = gsb[:, 2, :]
                          V("dve", "tensor_tensor", [r_ps[PMISC], r_pmt], [r_gsb], out=g0, in0=pg, in1=pmt[:, bb_, 0, :], op=ALU.mult)
                          V("dve", "tensor_tensor", [r_gsb, r_pmt], [r_gsb], out=g0, in0=g0, in1=pmt[:, bb_, 1, :], op=ALU.add)
                          g03 = g0.rearrange("p (h j) -> p h j", h=MH)
                          g13 = g1.rearrange("p (h j) -> p h j", h=MH)
                          e13 = e1.rearrange("p (h j) -> p h j", h=MH)
                          src3 = g03
                          for k in range(3):
                              V("dve", "tensor_reduce", [r_gsb], [r_gm], out=gm[:, k, :], in_=src3, axis=AX.X, op=ALU.max)
                              if k == 2:
                                  break
                              mb = gm[:, k, :].unsqueeze(2).broadcast_to([128, MH, 16])
                              V("dve", "tensor_tensor", [r_gsb, r_gm], [r_gsb], out=e13, in0=src3, in1=mb, op=ALU.is_ge)
                              V("dve", "scalar_tensor_tensor", [r_gsb], [r_gsb], out=g13, in0=e13, scalar=-BIG, in1=src3,
                                op0=ALU.mult, op1=ALU.add)
                              src3 = g13
                          mb = gm[:, 2, :].unsqueeze(2).broadcast_to([128, MH, 16])
                          V("dve", "tensor_tensor", [r_gsb, r_gm], [r_gsb], out=e13, in0=g03, in1=mb, op=ALU.is_ge)
                          V("dve", "tensor_tensor", [r_gsb, r_pmt], [r_gsb], out=e1, in0=e1, in1=pmt[:, bb_, 0, :], op=ALU.mult)
                          V("dve", "tensor_tensor", [r_gsb, r_pmt], [r_gsb], out=e1, in0=e1, in1=pmt[:, bb_, 2, :], op=ALU.add)
                          V("dve", "tensor_scalar", [r_gsb], [r_btm], out=bias_tm[:], in0=e1, scalar1=-1.0, scalar2=-NEG,
                            op0=ALU.add, op1=ALU.mult)
                          pb = psb[PTR][:].bitcast(BF16)[0:16, :].rearrange("p (h n) -> p h n", h=MH)
                          for h in range(MH):
                              TR(pb[:, h, :], bias_tm[:, h * 16:(h + 1) * 16], identb[:], [r_btm, r_ident], [r_ps[PTR]])
                          V("dve", "tensor_copy", [r_ps[PTR]], [r_BTb], out=BTb[:, :, t * 128:(t + 1) * 128], in_=pb)


                  def attend(s, g, pre):
                      QTb, r_QTb, BTb, r_BTb = QTs[g % 2], r_QTs[g % 2], BTs[g % 2], r_BTs[g % 2]
                      tok0 = s * S_ + g * G
                      gi = tok0 // G
                      ptr4 = psb[PTR][:].bitcast(BF16).rearrange("p (c n) -> p c n", c=8)
                      nch = 4 * (g + 1)
                      if "attn" in cfg.skip:
                          V("dve", "memset", [], [r_ya], ap=ya[:], constant=0.0)
                      items = [(h, c) for h in range(MH if "attn" not in cfg.skip else 0) for c in range(nch)]
                      PIPE = 2
                      pst4 = [3, 4, 7]
                      slots = {}

                      def emit_st(h, c):
                          pr = h // 2
                          gk = c // 4
                          si = pst4[ring["st"] % 3]
                          ring["st"] += 1
                          own_grp = (gk == g)
                          MM(psb[si][:], KT[:, pr, c * 128:(c + 1) * 128], QTb[:, h, :],
                             True, False, [r_KT[gk], r_QTb], [r_ps[si]])
                          MM(psb[si][:], oh[:, c * 128:(c + 1) * 128], BTb[:, h, :], False, not own_grp,
                             [r_oh, r_BTb], [r_ps[si]])
                          if own_grp:
                              MM(psb[si][:], identb[:], cmb[:, c % 4, :], False, True, [r_ident, r_cmb], [r_ps[si]])
                          pi = ring["pt"] % NPT
                          ring["pt"] += 1
                          ACT(PT[pi][:], psb[si][:], AF.Exp, [r_ps[si]], [r_PT[pi]], scale=1.0 / 8.0)
                          slots[(h, c)] = pi

                      def emit_pv(h, c):
                          gk = c // 4
                          a_i = h % 2
                          PACC = PACCS[a_i]
                          acc = psb[PACC][:, 0:260].rearrange("p (q d) -> p q d", q=4)
                          if c == 0:
                              MM(psb[PACC][:, 0:260], zerob[0:1, 0:128], zerob[0:1, 0:260], True, False,
                                 [r_zero], [r_ps[PACC]], skip_group_check=True)
                          pi = slots.pop((h, c))
                          for qs in range(4):
                              MM(acc[:, qs, :], PT[pi][:, qs * 128:(qs + 1) * 128], VA[:, c, h, :], False, c == nch - 1,
                                 [r_PT[pi], r_VA[gk]], [r_ps[PACC]], skip_group_check=True)
                          if c == nch - 1:
                              V("dve", "reciprocal", [r_ps[PACC]], [r_rcp[a_i]], out=rcp[:, a_i, :], in_=acc[:, :, MD])
                              V("dve", "tensor_tensor", [r_ps[PACC], r_rcp[a_i]], [r_ya], out=ya[:, :, h * MD:(h + 1) * MD],
                                in0=acc[:, :, 0:MD], in1=rcp[:, a_i, :].unsqueeze(2).broadcast_to([128, 4, MD]), op=ALU.mult)

                      pull = {0, len(items) // 4, len(items) // 2, (3 * len(items)) // 4}
                      for idx in range(len(items) + PIPE):
                          if pre is not None and idx in pull:
                              next(pre, None)
                          if idx < len(items):
                              emit_st(*items[idx])
                          if idx >= PIPE:
                              emit_pv(*items[idx - PIPE])
                      yi = gi % 2
                      for t in range(4):
                          for kc in range(4):
                              TR(ptr4[:, kc, :], ya[:, t, kc * 128:(kc + 1) * 128], identb[:], [r_ya, r_ident], [r_ps[PTR]])
                          V("dve", "tensor_copy", [r_ps[PTR]], [r_yaT[yi]], out=yaT[yi][:, :, t * 128:(t + 1) * 128], in_=ptr4[:, 0:4, :])
                      S.dma("sp", yas[gi], yaT[yi][:], reads=[r_yaT[yi]], writes=[r_yas[gi]], sres=r_yaT[yi], load=False)
                      if hooks:
                          hooks.pop(0)()
                      if cfg.dbg:
                          for nm, tl, rr in (("ya", ya, r_ya), ("yaT", yaT[yi], r_yaT[yi]), ("QT", QTb, r_QTb)):
                              final_tokens.append(S.dma("pool", dbg_t[nm][gi], tl[:], reads=[rr], writes=[r_dbg], sres=r_dbg))
                          final_tokens.append(S.dma("pool", dbg_t["BT"][gi], BTb[:], reads=[r_BTb], writes=[r_dbg], sres=r_dbg))
                      if pre is not None:
                          for _ in pre:
                              pass

                  for s in range(NSEQ):
                      V("pool", "memset", [], [r_kmT], ap=kmT[:], constant=0.0)
                      for _ in prelude(s, 0):
                          pass
                      for g in range(NG):
                          attend(s, g, prelude(s, g + 1) if g + 1 < NG else None)
            while hooks:
                hooks.pop(0)()
            S.barrier()

            with ExitStack() as st:
              if "B" in cfg.parts:
                  set_ring(st, 6)
                  uu, r_uu = cload(st, "uu_B", [128, 128], F32, c_u)
                  tri, r_tri = cload(st, "tri_B", [128, 128], F32, c_tri)
                  lnm_g, r_lng = cload(st, "lnm_g_B", [128, D], F32, ln_mg[l].partition_broadcast(128))
                  lnm_b, r_lnb = cload(st, "lnm_b_B", [128, D], F32, ln_mb[l].partition_broadcast(128))
                  gnt, r_gnt = cload(st, "gnt_B", [128, 512], F32, gn_g[l].partition_broadcast(128))
                  wupa = sb(st, "wupa_B", [32, 512], BF16)
                  r_wupa = S.res("wupa")
                  V("pool", "memset", [], [r_wupa], ap=wupa[:], constant=0.0)
                  S.dma("pool", wupa[0:16, :], w_up[l], reads=[], writes=[r_wupa], sres=r_wupa)
                  S.dma("pool", wupa[16:17, :], b_gg[l].unsqueeze(0), reads=[], writes=[r_wupa], sres=r_wupa)

                  stf = sb(st, "stf_B", [128, GH, 128], F32)
                  stb = sb(st, "stb_B", [128, GH, 128], BF16)
                  r_stf = [S.res(f"stf{h}") for h in range(GH)]
                  r_stb = [S.res(f"stb{h}") for h in range(GH)]
                  lgT = sb(st, "lgT_B", [32, G], BF16)
                  r_lgT = S.res("lgT")
                  V("pool", "memset", [], [r_lgT], ap=lgT[:], constant=1.0)

                  NXB = 2
                  xf = [sb(st, f"xf_B{i}", [128, D], F32) for i in range(NXB)]
                  r_xf = [S.res(f"xf{i}") for i in range(NXB)]
                  xb = [sb(st, f"xb_B{i}", [128, D], BF16) for i in range(NXB)]
                  r_xb = [S.res(f"xb{i}") for i in range(NXB)]
                  xT = sb(st, "xT_B", [128, 8, G], BF16)
                  r_xT = S.res("xT")
                  yaT = sb(st, "yaTb_B", [128, 4, G], BF16)
                  r_yaT = S.res("yaTb")
                  ybT = sb(st, "ybT_B", [128, 4, G], BF16)
                  r_ybT = S.res("ybT")
                  spt = sb(st, "spt_B", [128, 4, 512], F32)
                  r_spt = [S.res(f"spt{t}") for t in range(4)]
                  eq = sb(st, "eq_B", [128, GH, G], F32)
                  ek = sb(st, "ek_B", [128, GH, G], F32)
                  r_eq = [S.res(f"eq{h}") for h in range(GH)]
                  r_ek = [S.res(f"ek{h}") for h in range(GH)]
                  qdT = sb(st, "qdT_B", [128, GH, G], BF16)
                  kiT = sb(st, "kiT_B", [128, GH, G], BF16)
                  r_qdT = [S.res(f"qdT{h}") for h in range(GH)]
                  r_kiT = [S.res(f"kiT{h}") for h in range(GH)]
                  kiM = sb(st, "kiM_B", [128, 2, GH, 128], BF16)
                  r_kiM = [S.res("kiM0"), S.res("kiM1")]
                  vg = sb(st, "vg_B", [128, 4, 512], BF16)
                  r_vg = [S.res(f"vg{t}") for t in range(4)]
                  rgs = sb(st, "rgs_B", [128, 4, 512], BF16)
                  r_rgs = [S.res(f"rgs{t}") for t in range(4)]
                  attm = sb(st, "attm_B", [128, 2, GH, 128], BF16)
                  r_attm = [S.res("attm0"), S.res("attm1")]
                  stmp = sb(st, "stmp_B", [128, 2, GH, 128], F32)
                  r_stmp = [S.res("stmp0"), S.res("stmp1")]
                  hn = sb(st, "hn_B", [128, 40], F32)
                  r_hn = S.res("hn")
                  ob = sb(st, "ob_B", [128, 512], F32)
                  r_ob = S.res("ob")
                  yb = sb(st, "yb_B", [128, 512], BF16)
                  r_yb = S.res("yb")
                  sg = sb(st, "sg_B", [128, 2, G], F32)
                  r_sg = [S.res("sg0"), S.res("sg1")]
                  mT = sb(st, "mT_B", [128, 8, G], BF16)
                  r_mT = S.res("mT")
                  tin = [sb(st, f"tin_B{i}", [128, D], F32) for i in range(2)]
                  r_tin = [S.res(f"tin{i}") for i in range(2)]
                  x1o = [sb(st, f"x1o_B{i}", [128, D], F32) for i in range(2)]
                  r_x1o = [S.res(f"x1o{i}") for i in range(2)]
                  lst = sb(st, "lst_B", [128, 16], F32)
                  r_lst = S.res("lst")
                  ring = {"big": 0, "x": 0, "ln": 0}

                  pbigB = [0, 1, 2, 4, 5]

                  def nbig():
                      i = pbigB[ring["big"] % len(pbigB)]
                      ring["big"] += 1
                      return i

                  for s in range(NSEQ):
                      V("pool", "memset", [], [r_stf[0]], ap=stf[:], constant=0.0)
                      V("pool", "memset", [], [r_stb[0]], ap=stb[:], constant=0.0)
                      for g in range(NG):
                          tok0 = s * S_ + g * G
                          gi = tok0 // G
                          for t in range(4):
                              i = ring["x"] % NXB
                              ring["x"] += 1
                              r0 = tok0 + t * 128
                              S.dma("pool", xf[i][:], x_src[r0:r0 + 128, :], reads=[r_xsrc[gi]], writes=[r_xf[i]], sres=r_xf[i])
                              ACT(xb[i][:], xf[i][:], AF.Copy, [r_xf[i]], [r_xb[i]])
                              ptr = psb[PTR][:].bitcast(BF16).rearrange("p (c n) -> p c n", c=8)
                              for c in range(8):
                                  TR(ptr[:, c, :], xb[i][:, c * 128:(c + 1) * 128], identb[:], [r_xb[i], r_ident], [r_ps[PTR]])
                              V("dve", "tensor_copy", [r_ps[PTR]], [r_xT], out=xT[:, :, t * 128:(t + 1) * 128], in_=ptr)
                          S.dma("pool", yaT[:], yas[gi], reads=[r_yas[gi]], writes=[r_yaT], sres=r_yaT)
                          ptr4 = psb[PTR][:].bitcast(BF16).rearrange("p (c n) -> p c n", c=8)

                          def proj_tm(key, consume):
                              wv, rw = wload((l, key), "c8")
                              for t in range(4):
                                  pi = nbig()
                                  for c in range(8):
                                      MM(psb[pi][:], xT[:, c, t * 128:(t + 1) * 128], wv[:, c, :], c == 0, c == 7,
                                         [r_xT, rw], [r_ps[pi]])
                                  consume(t, psb[pi], r_ps[pi])

                          wv, rw = wload((l, "lg"), "lg")
                          pi = nbig()
                          for c in range(8):
                              MM(psb[pi][0:16, :], wv[:, c, :], xT[:, c, :], c == 0, c == 7, [r_xT, rw], [r_ps[pi]])
                          V("dve", "tensor_copy", [r_ps[pi]], [r_lgT], out=lgT[0:16, :], in_=psb[pi][0:16, :])
                          for t in range(4):
                              pi = nbig()
                              MM(psb[pi][:], lgT[:, t * 128:(t + 1) * 128], wupa[:], True, True, [r_lgT, r_wupa], [r_ps[pi]])
                              ACT(spt[:, t, :], psb[pi][:], AF.Exp, [r_ps[pi]], [r_spt[t]], scale=-1.0)
                              ACT(spt[:, t, :], spt[:, t, :], AF.Ln, [r_spt[t]], [r_spt[t]], bias=1.0)
                          for h in range(GH):
                              pi = nbig()
                              for t in range(4):
                                  MM(psb[pi][:, t * 128:(t + 1) * 128], spt[:, t, h * 128:(h + 1) * 128], uu[:], True, True,
                                     [r_spt[t], r_uu], [r_ps[pi]])
                              ACT(eq[:, h, :], psb[pi][:], AF.Exp, [r_ps[pi]], [r_eq[h]])
                              ACT(ek[:, h, :], psb[pi][:], AF.Exp, [r_ps[pi]], [r_ek[h]], scale=-1.0)
                          wv, rw = wload((l, "qg"), "c8")
                          for h in range(GH):
                              pi = nbig()
                              for c in range(8):
                                  MM(psb[pi][:], wv[:, c, h * 128:(h + 1) * 128], xT[:, c, :], c == 0, c == 7, [r_xT, rw], [r_ps[pi]])
                              V("dve", "scalar_tensor_tensor", [r_ps[pi], r_eq[h]], [r_qdT[h]], out=qdT[:, h, :], in0=psb[pi][:],
                                scalar=GK ** -0.5, in1=eq[:, h, :], op0=ALU.mult, op1=ALU.mult)
                          wv, rw = wload((l, "kg"), "c8")
                          for h in range(GH):
                              pi = nbig()
                              for c in range(8):
                                  MM(psb[pi][:], wv[:, c, h * 128:(h + 1) * 128], xT[:, c, :], c == 0, c == 7, [r_xT, rw], [r_ps[pi]])
                              V("dve", "tensor_tensor", [r_ps[pi], r_ek[h]], [r_kiT[h]], out=kiT[:, h, :], in0=psb[pi][:],
                                in1=ek[:, h, :], op=ALU.mult)

                          def cons_vg(t, ps, rps):
                              ACT(vg[:, t, :], ps[:], AF.Copy, [rps], [r_vg[t]])
                          proj_tm("vg", cons_vg)

                          def cons_rg(t, ps, rps):
                              ACT(rgs[:, t, :], ps[:], AF.Silu, [rps], [r_rgs[t]])
                          proj_tm("rg", cons_rg)

                          dec4s = {}

                          def gla_stage1(t):
                              j = t % 2
                              cs = slice(t * 128, (t + 1) * 128)
                              for h in range(GH):
                                  TR(ptr4[:, h, :], kiT[:, h, cs], identb[:], [r_kiT[h], r_ident], [r_ps[PTR]])
                              V("dve", "tensor_copy", [r_ps[PTR]], [r_kiM[j]], out=kiM[:, j, :, :], in_=ptr4[:, 0:4, :])
                              pa = nbig()
                              for h in range(GH):
                                  MM(psb[pa][:, h * 128:(h + 1) * 128], kiT[:, h, cs], qdT[:, h, cs], True, True, [r_kiT[h], r_qdT[h]], [r_ps[pa]])
                              V("dve", "tensor_tensor", [r_ps[pa], r_tri], [r_attm[j]], out=attm[:, j, :, :],
                                in0=psb[pa][:].rearrange("p (h n) -> p h n", h=GH), in1=tri[:].unsqueeze(1).broadcast_to([128, GH, 128]), op=ALU.mult)

                          def gla_stage2(t):
                              j = t % 2
                              cs = slice(t * 128, (t + 1) * 128)
                              PO = PMISC if t % 2 == 0 else 3
                              po = psb[PO]
                              for h in range(GH):
                                  MM(po[:, h * 128:(h + 1) * 128], attm[:, j, h, :], vg[:, t, h * 128:(h + 1) * 128], True, False,
                                     [r_attm[j], r_vg[t]], [r_ps[PO]])
                                  MM(po[:, h * 128:(h + 1) * 128], qdT[:, h, cs], stb[:, h, :], False, True,
                                     [r_qdT[h], r_stb[0]], [r_ps[PO]])
                              pd = nbig()
                              for h in range(GH):
                                  MM(psb[pd][:, h * 128:(h + 1) * 128], kiM[:, j, h, :], vg[:, t, h * 128:(h + 1) * 128], True, True,
                                     [r_kiM[j], r_vg[t]], [r_ps[pd]])
                              V("dve", "tensor_tensor", [r_ps[pd], r_stf[0]], [r_stmp[j]], out=stmp[:, j, :, :],
                                in0=psb[pd][:].rearrange("p (h n) -> p h n", h=GH), in1=stf[:], op=ALU.add)
                              dec4 = eq[:, :, t * 128 + 127:t * 128 + 128].broadcast_to([128, GH, 128])
                              V("dve", "tensor_tensor", [r_stmp[j]] + r_eq, [r_stf[0]], out=stf[:], in0=stmp[:, j, :, :], in1=dec4, op=ALU.mult)
                              V("dve", "tensor_tensor", [r_stmp[j]] + r_eq, [r_stb[0]], out=stb[:], in0=stmp[:, j, :, :], in1=dec4, op=ALU.mult)
                              return po, PO

                          gla_stage1(0)
                          for t in range(4):
                              if t + 1 < 4:
                                  gla_stage1(t + 1)
                              po, PO = gla_stage2(t)
                              for h in range(GH):
                                  V("dve", "bn_stats", [r_ps[PO]], [r_hn], out=hn[:, h * 6:(h + 1) * 6], in_=po[:, h * 128:(h + 1) * 128])
                                  V("dve", "bn_aggr", [r_hn], [r_hn], out=hn[:, 24 + 2 * h:26 + 2 * h], in_=hn[:, h * 6:(h + 1) * 6])
                              mv = hn[:, 24:32].rearrange("p (h k) -> p h k", h=GH)
                              V("dve", "tensor_scalar_add", [r_hn], [r_hn], out=hn[:, 32:36], in0=mv[:, :, 1], scalar1=LN_EPS)
                              ACT(hn[:, 32:36], hn[:, 32:36], AF.Sqrt, [r_hn], [r_hn])
                              V("dve", "reciprocal", [r_hn], [r_hn], out=hn[:, 32:36], in_=hn[:, 32:36])
                              for h in range(GH):
                                  V("dve", "tensor_scalar", [r_ps[PO], r_hn], [r_ob], out=ob[:, h * 128:(h + 1) * 128],
                                    in0=po[:, h * 128:(h + 1) * 128], scalar1=hn[:, 24 + 2 * h:25 + 2 * h], scalar2=hn[:, 32 + h:33 + h],
                                    op0=ALU.subtract, op1=ALU.mult)
                              V("pool", "tensor_tensor", [r_ob, r_gnt], [r_ob], out=ob[:], in0=ob[:], in1=gnt[:], op=ALU.mult)
                              V("pool", "tensor_tensor", [r_ob, r_rgs[t]], [r_yb], out=yb[:], in0=ob[:], in1=rgs[:, t, :], op=ALU.mult)
                              for kc in range(4):
                                  TR(ptr4[:, kc, :], yb[:, kc * 128:(kc + 1) * 128], identb[:], [r_yb, r_ident], [r_ps[PTR]])
                              V("dve", "tensor_copy", [r_ps[PTR]], [r_ybT], out=ybT[:, :, t * 128:(t + 1) * 128], in_=ptr4[:, 0:4, :])

                          wa_v, rwa = wload((l, "wa"), "c4")
                          wb_v, rwb = wload((l, "wb"), "c4")
                          for half in range(2):
                              ga_v, rga = wload((l, f"ga{half}"), "c8")
                              gb_v, rgb = wload((l, f"gb{half}"), "c8")
                              for mm_ in range(4):
                                  m = half * 4 + mm_
                                  cs = slice(mm_ * 128, (mm_ + 1) * 128)
                                  ms = slice(m * 128, (m + 1) * 128)
                                  pi = nbig()
                                  for c in range(8):
                                      MM(psb[pi][:], ga_v[:, c, cs], xT[:, c, :], c == 0, c == 7, [r_xT, rga], [r_ps[pi]])
                                  ACT(sg[:, 0, :], psb[pi][:], AF.Sigmoid, [r_ps[pi]], [r_sg[0]])
                                  pi = nbig()
                                  for c in range(4):
                                      MM(psb[pi][:], wa_v[:, c, ms], yaT[:, c, :], c == 0, c == 3, [r_yaT, rwa], [r_ps[pi]])
                                  V("dve", "tensor_tensor", [r_ps[pi], r_sg[0]], [r_sg[0]], out=sg[:, 0, :], in0=psb[pi][:], in1=sg[:, 0, :], op=ALU.mult)
                                  pi = nbig()
                                  for c in range(8):
                                      MM(psb[pi][:], gb_v[:, c, cs], xT[:, c, :], c == 0, c == 7, [r_xT, rgb], [r_ps[pi]])
                                  ACT(sg[:, 1, :], psb[pi][:], AF.Sigmoid, [r_ps[pi]], [r_sg[1]])
                                  pi = nbig()
                                  for c in range(4):
                                      MM(psb[pi][:], wb_v[:, c, ms], ybT[:, c, :], c == 0, c == 3, [r_ybT, rwb], [r_ps[pi]])
                                  V("dve", "tensor_tensor", [r_ps[pi], r_sg[1]], [r_sg[1]], out=sg[:, 1, :], in0=psb[pi][:], in1=sg[:, 1, :], op=ALU.mult)
                                  V("pool", "tensor_tensor", [r_sg[0], r_sg[1]], [r_mT], out=mT[:, m, :], in0=sg[:, 0, :], in1=sg[:, 1, :], op=ALU.add)

                          if cfg.dbg:
                              final_tokens.append(S.dma("pool", dbg_t["ybT"][gi], ybT[:], reads=[r_ybT], writes=[r_dbg], sres=r_dbg))
                              final_tokens.append(S.dma("pool", dbg_t["mT"][gi], mT[:], reads=[r_mT], writes=[r_dbg], sres=r_dbg))
                          wo_v0, rwo0 = wload((l, "wo0"), "c8")
                          wo_v1, rwo1 = wload((l, "wo1"), "c8")
                          for t in range(4):
                              i = ring["ln"] % 2
                              ring["ln"] += 1
                              r0 = tok0 + t * 128
                              S.dma("pool", tin[i][:], x_src[r0:r0 + 128, :], reads=[r_xsrc[gi]], writes=[r_tin[i]], sres=r_tin[i])
                              for half, (wv_, rw_) in enumerate(((wo_v0, rwo0), (wo_v1, rwo1))):
                                  pi = nbig()
                                  for c in range(8):
                                      MM(psb[pi][:], mT[:, c, t * 128:(t + 1) * 128], wv_[:, c, :], c == 0, c == 7, [r_mT, rw_], [r_ps[pi]])
                                  hs = slice(half * 512, (half + 1) * 512)
                                  V("dve", "scalar_tensor_tensor", [r_ps[pi], r_tin[i]], [r_tin[i]], out=tin[i][:, hs], in0=tin[i][:, hs],
                                    scalar=ALPHA, in1=psb[pi][:], op0=ALU.mult, op1=ALU.add)
                              layer_norm_tile((lst, r_lst), tin[i], r_tin[i], lnm_g, lnm_b, [r_lng, r_lnb], x1o[i][:], r_x1o[i], eng="dve")
                              S.dma("pool", x1s[r0:r0 + 128, :], x1o[i][:], reads=[r_x1o[i]], writes=[r_x1s[gi]], sres=r_x1o[i], load=False)
            S.barrier()

        def phase_ffn(l, dst, r_dst, is_final):
            moe = (l % 2 == 1)
            with ExitStack() as st:
                set_ring(st, 6)
                lnf_g = sb(st, "lnf_g", [128, D], F32)
                lnf_b = sb(st, "lnf_b", [128, D], F32)
                r_lnf = S.res("lnf")
                S.dma("pool", lnf_g[:], ln_fg[l].partition_broadcast(128), writes=[r_lnf], sres=r_lnf)
                S.dma("pool", lnf_b[:], ln_fb[l].partition_broadcast(128), writes=[r_lnf], sres=r_lnf)
                xf = [sb(st, f"fxf{i}", [128, D], F32) for i in range(2)]
                r_xf = [S.res(f"fxf{i}") for i in range(2)]
                xb = [sb(st, f"fxb{i}", [128, D], BF16) for i in range(2)]
                r_xb = [S.res(f"fxb{i}") for i in range(2)]
                xT = sb(st, "fxT", [128, 8, G], BF16)
                r_xT = S.res("fxT")
                hT = sb(st, "hT", [128, 28, G], BF16)
                r_hT = [S.res(f"hT{i}") for i in range(7)]
                sgt = [sb(st, f"sgt{i}", [128, G], BF16) for i in range(2)]
                r_sgt = [S.res(f"sgt{i}") for i in range(2)]
                tin = [sb(st, f"ftin{i}", [128, D], F32) for i in range(4)]
                r_tin = [S.res(f"ftin{i}") for i in range(4)]
                yo = [sb(st, f"fyo{i}", [128, D], F32) for i in range(4)]
                r_yo = [S.res(f"fyo{i}") for i in range(4)]
                lst = sb(st, "flst", [128, 16], F32)
                r_lst = S.res("flst")
                if moe:
                    wr = sb(st, "wr", [128, NE, D], F32)
                    r_wr = S.res("wr")
                    for e in range(NE):
                        S.dma("pool", wr[:, e, :], m_rT[e].partition_broadcast(128), writes=[r_wr], sres=r_wr)
                    acc = sb(st, "macc", [128, 4, D], F32)
                    r_acc = [S.res(f"macc{t}") for t in range(4)]
                    lg = sb(st, "mlg", [128, 4, 32], F32)
                    r_lg = [S.res(f"mlg{t}") for t in range(4)]
                    junk = sb(st, "mjunk", [128, D], F32)
                    r_junk = S.res("mjunk")
                ring = {"big": 0, "x": 0, "sg": 0, "ln": 0}
                pbig = [0, 1, 2, 3]
                pdn = [4, 5]
                PTR = 6

                def nbig():
                    i = pbig[ring["big"] % len(pbig)]
                    ring["big"] += 1
                    return i

                for gi in range(T // G):
                    tok0 = gi * G
                    for t in range(4):
                        i = ring["x"] % 2
                        ring["x"] += 1
                        r0 = tok0 + t * 128
                        S.dma("pool", xf[i][:], x1s[r0:r0 + 128, :], reads=[r_x1s[gi]], writes=[r_xf[i]], sres=r_xf[i])
                        ACT(xb[i][:], xf[i][:], AF.Copy, [r_xf[i]], [r_xb[i]])
                        ptr = psb[PTR][:].bitcast(BF16).rearrange("p (c n) -> p c n", c=8)
                        for c in range(8):
                            TR(ptr[:, c, :], xb[i][:, c * 128:(c + 1) * 128], identb[:], [r_xb[i], r_ident], [r_ps[PTR]])
                        V("dve", "tensor_copy", [r_ps[PTR]], [r_xT], out=xT[:, :, t * 128:(t + 1) * 128], in_=ptr)
                        if moe:
                            for e in range(NE):
                                V("dve", "tensor_tensor", [r_xf[i], r_wr], [r_junk], out=junk[:], in0=xf[i][:],
                                  in1=wr[:, e, :], op=ALU.mult)
                                V("dve", "tensor_reduce", [r_junk], [r_lg[t]], out=lg[:, t, e:e + 1], in_=junk[:], axis=AX.X, op=ALU.add)
                            L = lg[:, t, 0:8]
                            m1 = lg[:, t, 8:9]
                            m2 = lg[:, t, 9:10]
                            E1 = lg[:, t, 10:18]
                            L2 = lg[:, t, 18:26]
                            V("dve", "tensor_reduce", [r_lg[t]], [r_lg[t]], out=m1, in_=L, axis=AX.X, op=ALU.max)
                            V("dve", "tensor_scalar", [r_lg[t]], [r_lg[t]], out=E1, in0=L, scalar1=m1, scalar2=None, op0=ALU.is_ge)
                            V("dve", "scalar_tensor_tensor", [r_lg[t]], [r_lg[t]], out=L2, in0=E1, scalar=-BIG, in1=L, op0=ALU.mult, op1=ALU.add)
                            V("dve", "tensor_reduce", [r_lg[t]], [r_lg[t]], out=m2, in_=L2, axis=AX.X, op=ALU.max)
                            V("dve", "tensor_scalar", [r_lg[t]], [r_lg[t]], out=E1, in0=L, scalar1=m2, scalar2=None, op0=ALU.is_ge)
                            V("dve", "tensor_scalar", [r_lg[t]], [r_lg[t]], out=L2, in0=L, scalar1=m1, scalar2=None, op0=ALU.subtract)
                            ACT(L2, L2, AF.Exp, [r_lg[t]], [r_lg[t]])
                            V("dve", "tensor_tensor", [r_lg[t]], [r_lg[t]], out=L2, in0=L2, in1=E1, op=ALU.mult)
                            V("dve", "tensor_reduce", [r_lg[t]], [r_lg[t]], out=lg[:, t, 26:27], in_=L2, axis=AX.X, op=ALU.add)
                            V("dve", "reciprocal", [r_lg[t]], [r_lg[t]], out=lg[:, t, 26:27], in_=lg[:, t, 26:27])
                            V("dve", "tensor_scalar", [r_lg[t]], [r_lg[t]], out=E1, in0=L2, scalar1=lg[:, t, 26:27], scalar2=None, op0=ALU.mult)

                    for e in range(NE if moe else 1):
                        kg = (lambda i: ("m", e, "g", i)) if moe else (lambda i: ("f", "g", i))
                        ku = (lambda i: ("m", e, "u", i)) if moe else (lambda i: ("f", "u", i))
                        kd = (lambda i: ("m", e, "d", i)) if moe else (lambda i: ("f", "d", i))
                        for bi in range(7):
                            wg_v, rwg = wload(kg(bi), "c8")
                            wu_v, rwu = wload(ku(bi), "c8")
                            for j in range(4):
                                fc = bi * 4 + j
                                js = slice(j * 128, (j + 1) * 128)
                                pg = nbig()
                                for c in range(8):
                                    MM(psb[pg][:], wg_v[:, c, js], xT[:, c, :], c == 0, c == 7, [r_xT, rwg], [r_ps[pg]])
                                k = ring["sg"] % 2
                                ring["sg"] += 1
                                ACT(sgt[k][:], psb[pg][:], AF.Silu, [r_ps[pg]], [r_sgt[k]])
                                pu = nbig()
                                for c in range(8):
                                    MM(psb[pu][:], wu_v[:, c, js], xT[:, c, :], c == 0, c == 7, [r_xT, rwu], [r_ps[pu]])
                                V("dve", "tensor_tensor", [r_ps[pu], r_sgt[k]], [r_hT[bi]], out=hT[:, fc, :], in0=psb[pu][:], in1=sgt[k][:], op=ALU.mult)
                        dblocks = []
                        for half in range(2):
                            accp = [nbig() for _ in range(4)]
                            for bi in range(7):
                                wd_v, rwd = wload(kd(bi), "c4")
                                for t in range(4):
                                    for j in range(4):
                                        fc = bi * 4 + j
                                        MM(psb[accp[t]][:], hT[:, fc, t * 128:(t + 1) * 128], wd_v[:, j, half * 512:(half + 1) * 512],
                                           fc == 0, fc == 27, [r_hT[bi], rwd], [r_ps[accp[t]]])
                            hs = slice(half * 512, (half + 1) * 512)
                            for t in range(4):
                                if moe:
                                    cw = lg[:, t, 10 + e:11 + e]
                                    if e == 0:
                                        V("dve", "tensor_scalar", [r_ps[accp[t]], r_lg[t]], [r_acc[t]], out=acc[:, t, hs], in0=psb[accp[t]][:],
                                          scalar1=cw, scalar2=None, op0=ALU.mult)
                                    else:
                                        V("dve", "scalar_tensor_tensor", [r_ps[accp[t]], r_lg[t], r_acc[t]], [r_acc[t]], out=acc[:, t, hs],
                                          in0=psb[accp[t]][:], scalar=cw, in1=acc[:, t, hs], op0=ALU.mult, op1=ALU.add)
                                else:
                                    if half == 0:
                                        i = t
                                        r0 = tok0 + t * 128
                                        S.dma("pool", tin[i][:], x1s[r0:r0 + 128, :], reads=[r_x1s[gi]], writes=[r_tin[i]], sres=r_tin[i])
                                        dblocks.append(i)
                                    i = dblocks[t]
                                    V("dve", "scalar_tensor_tensor", [r_ps[accp[t]], r_tin[i]], [r_tin[i]], out=tin[i][:, hs], in0=tin[i][:, hs],
                                      scalar=ALPHA, in1=psb[accp[t]][:], op0=ALU.mult, op1=ALU.add)
                                    if half == 1:
                                        r0 = tok0 + t * 128
                                        layer_norm_tile((lst, r_lst), tin[i], r_tin[i], lnf_g, lnf_b, [r_lnf], yo[i][:], r_yo[i], eng="pool")
                                        o = S.dma("pool", dst[r0:r0 + 128, :], yo[i][:], reads=[r_yo[i]], writes=[r_dst[gi]], sres=r_yo[i], load=False)
                                        if is_final:
                                            final_tokens.append(o)
                    if moe:
                        for t in range(4):
                            i = t
                            r0 = tok0 + t * 128
                            S.dma("pool", tin[i][:], x1s[r0:r0 + 128, :], reads=[r_x1s[gi]], writes=[r_tin[i]], sres=r_tin[i])
                            V("dve", "scalar_tensor_tensor", [r_acc[t], r_tin[i]], [r_tin[i]], out=tin[i][:], in0=tin[i][:],
                              scalar=ALPHA, in1=acc[:, t, :], op0=ALU.mult, op1=ALU.add)
                            layer_norm_tile((lst, r_lst), tin[i], r_tin[i], lnf_g, lnf_b, [r_lnf], yo[i][:], r_yo[i], eng="pool")
                            o = S.dma("pool", dst[r0:r0 + 128, :], yo[i][:], reads=[r_yo[i]], writes=[r_dst[gi]], sres=r_yo[i], load=False)
                            if is_final:
                                final_tokens.append(o)
            S.barrier()

        def phase_moe(l, dst, r_dst, is_final):
            IOA = bass.IndirectOffsetOnAxis
            NT = T // 128
            wsc_rows = wsm.rearrange("b p n -> (b p) n")
            moe_blk0 = [r_wsc[tab[("m", e, "g", 0)]] for e in range(NE)]
            r_s2t = S.res("s2t")
            r_s2t_parts = []
            r_yg = [S.res(f"yg{j}") for j in range(NGRP)]
            PTR, PMISC = 6, 7
            with ExitStack() as so:
                set_ring(so, 6)
                lg = sb(so, "mlg", [128, NT, 32], F32)
                r_lg = S.res("mlg")
                selm = sb(so, "selm", [128, NT, NE], F32)
                hot1 = sb(so, "hot1", [128, NT, NE], F32)
                hot2 = sb(so, "hot2", [128, NT, NE], F32)
                rankg = sb(so, "rankg", [128, NT, NE], F32)
                r_rt = S.res("route")
                cwk = sb(so, "cwk", [128, NT, 2], F32)
                r_cwk = S.res("cwk")
                run = sb(so, "runc", [128, 48], F32)
                r_run = S.res("runc")
                widx = sb(so, "widx", [128, NGRP, 21], I32)
                r_widx = S.res("widx")
                sl0i = sb(so, "sl0i", [128, NT], I32)
                sl1i = sb(so, "sl1i", [128, NT], I32)
                r_sli = S.res("sli")
                tokc = sb(so, "tokc", [128, NT], I32)
                r_tokc = S.res("tokc")
                S.dma("pool", tokc[:], c_tok, writes=[r_tokc], sres=r_tokc)

                with ExitStack() as st:
                    wr = sb(st, "wr", [128, NE, D], F32)
                    r_wr = S.res("wr")
                    for e in range(NE):
                        S.dma("pool", wr[:, e, :], m_rT[e].partition_broadcast(128), writes=[r_wr], sres=r_wr)
                    xf = [sb(st, f"rxf{i}", [128, D], F32) for i in range(2)]
                    r_xf = [S.res(f"rxf{i}") for i in range(2)]
                    junk = sb(st, "rjunk", [128, D], F32)
                    r_junk = S.res("rjunk")
                    tris, r_tris = sb(st, "tris", [128, 128], F32), S.res("tris")
                    S.dma("pool", tris[:], c_tris, writes=[r_tris], sres=r_tris)
                    onesf, r_onesf = sb(st, "onesf", [128, 128], F32), S.res("onesf")
                    V("pool", "memset", [], [r_onesf], ap=onesf[:], constant=1.0)
                    thr, r_thr = sb(st, "thr", [128, 16], F32), S.res("thr")
                    S.dma("pool", thr[:], c_thr, writes=[r_thr], sres=r_thr)
                    jj, r_jj = sb(st, "jj", [128, NGRP], F32), S.res("jj")
                    S.dma("pool", jj[:], c_jj, writes=[r_jj], sres=r_jj)
                    wcst, r_wcst = sb(st, "wcst", [128, 21], F32), S.res("wcst")
                    S.dma("pool", wcst[:], c_wcst, writes=[r_wcst], sres=r_wcst)
                    cmp3 = sb(st, "cmp3", [128, NE, 16], F32)
                    ej = sb(st, "ej", [128, NGRP], F32)
                    widxf = sb(st, "widxf", [128, NGRP, 21], F32)
                    slotf = sb(st, "slotf", [128, NT, NE], F32)
                    stmp = sb(st, "rstmp", [128, NT, NE], F32)
                    s01f = sb(st, "s01f", [128, 2, NT], F32)
                    zi = sb(st, "zi", [128, NSLOT // 128], I32)
                    r_zi = S.res("zi")
                    V("pool", "memset", [], [r_zi], ap=zi[:], constant=0)
                    S.dma("pool", s2t.rearrange("(p n) o -> p (n o)", p=128), zi[:], reads=[r_zi], writes=[r_s2t], sres=r_s2t)
                    r_misc = S.res("rmisc")
                    V("dve", "memset", [], [r_run], ap=run[:], constant=0.0)

                    for i in range(NT):
                        b = i % 2
                        r0 = i * 128
                        S.dma("pool", xf[b][:], x1s[r0:r0 + 128, :], reads=[r_x1s[r0 // G]], writes=[r_xf[b]], sres=r_xf[b])
                        for e in range(NE):
                            V("dve", "scalar_tensor_tensor", [r_xf[b], r_wr], [r_junk, r_lg], out=junk[:], in0=xf[b][:], scalar=1.0, in1=wr[:, e, :],
                              op0=ALU.mult, op1=ALU.mult, accum_out=lg[:, i, e:e + 1])
                        L = lg[:, i, 0:8]
                        m1 = lg[:, i, 8:9]
                        m2 = lg[:, i, 9:10]
                        L2 = lg[:, i, 18:26]
                        dd = lg[:, i, 26:27]
                        H1 = hot1[:, i, :]
                        H2 = hot2[:, i, :]
                        V("dve", "tensor_reduce", [r_lg], [r_lg], out=m1, in_=L, axis=AX.X, op=ALU.max)
                        V("dve", "tensor_scalar", [r_lg], [r_rt], out=H1, in0=L, scalar1=m1, scalar2=None, op0=ALU.is_ge)
                        V("dve", "scalar_tensor_tensor", [r_lg, r_rt], [r_lg], out=L2, in0=H1, scalar=-BIG, in1=L, op0=ALU.mult, op1=ALU.add)
                        V("dve", "tensor_reduce", [r_lg], [r_lg], out=m2, in_=L2, axis=AX.X, op=ALU.max)
                        V("dve", "tensor_scalar", [r_lg], [r_rt], out=H2, in0=L2, scalar1=m2, scalar2=None, op0=ALU.is_ge)
                        V("dve", "tensor_tensor", [r_rt], [r_rt], out=selm[:, i, :], in0=H1, in1=H2, op=ALU.add)
                        V("dve", "tensor_tensor", [r_lg], [r_lg], out=dd, in0=m2, in1=m1, op=ALU.subtract)
                        ACT(dd, dd, AF.Exp, [r_lg], [r_lg])
                        V("dve", "tensor_scalar_add", [r_lg], [r_cwk], out=cwk[:, i, 0:1], in0=dd, scalar1=1.0)
                        V("dve", "reciprocal", [r_cwk], [r_cwk], out=cwk[:, i, 0:1], in_=cwk[:, i, 0:1])
                        V("dve", "tensor_tensor", [r_cwk, r_lg], [r_cwk], out=cwk[:, i, 1:2], in0=cwk[:, i, 0:1], in1=dd, op=ALU.mult)
                        pr = psb[PMISC][:, 0:16]
                        MM(pr[:, 0:8], tris[:], selm[:, i, :], True, True, [r_tris, r_rt], [r_ps[PMISC]])
                        MM(pr[:, 8:16], onesf[:], selm[:, i, :], True, True, [r_onesf, r_rt], [r_ps[PMISC]])
                        V("dve", "tensor_tensor", [r_ps[PMISC], r_run], [r_rt], out=rankg[:, i, :], in0=pr[:, 0:8], in1=run[:, 0:8], op=ALU.add)
                        V("dve", "tensor_tensor", [r_ps[PMISC], r_run], [r_run], out=run[:, 0:8], in0=pr[:, 8:16], in1=run[:, 0:8], op=ALU.add)

                    cnt, ng, gend, base = run[:, 0:8], run[:, 8:16], run[:, 16:24], run[:, 24:32]
                    V("dve", "tensor_tensor", [r_run, r_thr], [r_misc], out=cmp3[:], in0=cnt.unsqueeze(2).broadcast_to([128, NE, 16]),
                      in1=thr[:].unsqueeze(1).broadcast_to([128, NE, 16]), op=ALU.is_gt)
                    V("dve", "tensor_reduce", [r_misc], [r_run], out=ng, in_=cmp3[:], axis=AX.X, op=ALU.add)
                    V("dve", "tensor_copy", [r_run], [r_run], out=gend[:, 0:1], in_=ng[:, 0:1])
                    for e in range(1, NE):
                        V("dve", "tensor_tensor", [r_run], [r_run], out=gend[:, e:e + 1], in0=gend[:, e - 1:e], in1=ng[:, e:e + 1], op=ALU.add)
                    V("dve", "tensor_tensor", [r_run], [r_run], out=base, in0=gend, in1=ng, op=ALU.subtract)
                    V("dve", "tensor_scalar_mul", [r_run], [r_run], out=base, in0=base, scalar1=512.0)
                    V("dve", "tensor_scalar", [r_jj, r_run], [r_misc], out=ej[:], in0=jj[:], scalar1=gend[:, 0:1], scalar2=None, op0=ALU.is_ge)
                    for e in range(1, NE):
                        V("dve", "scalar_tensor_tensor", [r_jj, r_run, r_misc], [r_misc], out=ej[:], in0=jj[:], scalar=gend[:, e:e + 1], in1=ej[:],
                          op0=ALU.is_ge, op1=ALU.add)
                    V("dve", "tensor_scalar_min", [r_misc], [r_misc], out=ej[:], in0=ej[:], scalar1=float(NE - 1))
                    V("dve", "scalar_tensor_tensor", [r_misc, r_wcst], [r_misc], out=widxf[:], in0=ej[:].unsqueeze(2).broadcast_to([128, NGRP, 21]),
                      scalar=float(21 * 128), in1=wcst[:].unsqueeze(1).broadcast_to([128, NGRP, 21]), op0=ALU.mult, op1=ALU.add)
                    V("dve", "tensor_copy", [r_misc], [r_widx], out=widx[:], in_=widxf[:])
                    V("dve", "tensor_tensor", [r_rt, r_run], [r_misc], out=slotf[:], in0=rankg[:], in1=base.unsqueeze(1).broadcast_to([128, NT, NE]), op=ALU.add)
                    for k, hot in enumerate((hot1, hot2)):
                        V("dve", "tensor_tensor", [r_misc, r_rt], [r_misc], out=stmp[:], in0=slotf[:], in1=hot[:], op=ALU.mult)
                        V("dve", "tensor_reduce", [r_misc], [r_misc], out=s01f[:, k, :], in_=stmp[:], axis=AX.X, op=ALU.add)
                    V("dve", "tensor_copy", [r_misc], [r_sli], out=sl0i[:], in_=s01f[:, 0, :])
                    V("dve", "tensor_copy", [r_misc], [r_sli], out=sl1i[:], in_=s01f[:, 1, :])
                    sc_st = S.new_stream("scat")
                    sc_st.q = "pool"
                    sc_ops = []
                    for i in range(NT):
                        for sli in (sl0i, sl1i):
                            rr = S.res("s2tw")
                            r_s2t_parts.append(rr)
                            sc_ops.append(S.dma("pool", None, None, reads=[r_sli, r_tokc, r_s2t], writes=[rr], sres=rr, stream=sc_st,
                                                fn=(lambda e, sli=sli, i=i: e.indirect_dma_start(out=s2t[:, :], out_offset=IOA(ap=sli[:, i:i + 1], axis=0),
                                                                                                  in_=tokc[:, i:i + 1], in_offset=None))))
                    for o_ in sc_ops:
                        o_.dval = sc_st.count * 16
                with ExitStack() as st:
                    tix = [sb(st, f"tix{i}", [128, 4], I32) for i in range(2)]
                    r_tix = [S.res(f"tix{i}") for i in range(2)]
                    xg = [[sb(st, f"xg{i}_{q}", [128, D], BF16) for q in range(4)] for i in range(2)]
                    r_xg = [[S.res(f"xg{i}_{q}") for q in range(4)] for i in range(2)]
                    xT = sb(st, "exT", [128, 8, G], BF16)
                    r_xT = S.res("exT")
                    hT = sb(st, "ehT", [128, 28, G], BF16)
                    r_hT = [S.res(f"ehT{i}") for i in range(7)]
                    sgt = [sb(st, f"esgt{i}", [128, G], BF16) for i in range(2)]
                    r_sgt = [S.res(f"esgt{i}") for i in range(2)]
                    yo = [sb(st, f"eyo{i}", [128, D], F32) for i in range(4)]
                    r_yo = [S.res(f"eyo{i}") for i in range(4)]
                    ring = {"big": 0, "sg": 0}
                    pbig = [0, 1, 2, 3]

                    def nbig():
                        i = pbig[ring["big"] % len(pbig)]
                        ring["big"] += 1
                        return i

                    def wload_dyn(j, k, view):
                        i = wstate["i"] % len(wring)
                        wstate["i"] += 1
                        S.dma("pool", None, None, reads=[r_widx] + moe_blk0, writes=[r_wring[i]], sres=r_wring[i],
                              fn=(lambda e, i=i, j=j, k=k: e.indirect_dma_start(out=wring[i][:], out_offset=None, in_=wsc_rows,
                                                                               in_offset=IOA(ap=widx[:, j, k:k + 1], axis=0))))
                        t = wring[i]
                        v = t[:].rearrange("p (c n) -> p c n", c=8 if view == "c8" else 4)
                        return v, r_wring[i]

                    def gather_group(j):
                        jb = j % 2
                        for q in range(4):
                            s0 = j * 512 + q * 128
                            S.dma("sp", tix[jb][:, q:q + 1], s2t[s0:s0 + 128, :], reads=[r_s2t] + r_s2t_parts, writes=[r_tix[jb]], sres=r_tix[jb])
                        for q in range(4):
                            S.dma("pool", None, None, reads=[r_tix[jb]] + r_x1s, writes=[r_xg[jb][q]], sres=r_xg[jb][q],
                                  fn=(lambda e, jb=jb, q=q: e.indirect_dma_start(out=xg[jb][q][:], out_offset=None, in_=x1s[:, :],
                                                                                 in_offset=IOA(ap=tix[jb][:, q:q + 1], axis=0))))

                    gather_group(0)
                    for j in range(NGRP):
                        jb = j % 2
                        if j + 1 < NGRP:
                            gather_group(j + 1)
                        for q in range(4):
                            PB = PTR if q % 2 == 0 else PMISC
                            ptr = psb[PB][:].bitcast(BF16).rearrange("p (c n) -> p c n", c=8)
                            for c in range(8):
                                TR(ptr[:, c, :], xg[jb][q][:, c * 128:(c + 1) * 128], identb[:], [r_xg[jb][q], r_ident], [r_ps[PB]])
                            V("dve", "tensor_copy", [r_ps[PB]], [r_xT], out=xT[:, :, q * 128:(q + 1) * 128], in_=ptr)
                        for bi in range(7):
                            wg_v, rwg = wload_dyn(j, bi, "c8")
                            wu_v, rwu = wload_dyn(j, 7 + bi, "c8")
                            for jn in range(4):
                                fc = bi * 4 + jn
                                js = slice(jn * 128, (jn + 1) * 128)
                                pg = nbig()
                                for c in range(8):
                                    MM(psb[pg][:], wg_v[:, c, js], xT[:, c, :], c == 0, c == 7, [r_xT, rwg], [r_ps[pg]])
                                k = ring["sg"] % 2
                                ring["sg"] += 1
                                ACT(sgt[k][:], psb[pg][:], AF.Silu, [r_ps[pg]], [r_sgt[k]])
                                pu = nbig()
                                for c in range(8):
                                    MM(psb[pu][:], wu_v[:, c, js], xT[:, c, :], c == 0, c == 7, [r_xT, rwu], [r_ps[pu]])
                                V("dve", "tensor_tensor", [r_ps[pu], r_sgt[k]], [r_hT[bi]], out=hT[:, fc, :], in0=psb[pu][:], in1=sgt[k][:], op=ALU.mult)
                        for half in range(2):
                            accp = [nbig() for _ in range(4)]
                            for bi in range(7):
                                wd_v, rwd = wload_dyn(j, 14 + bi, "c4")
                                for t in range(4):
                                    for jn in range(4):
                                        fc = bi * 4 + jn
                                        MM(psb[accp[t]][:], hT[:, fc, t * 128:(t + 1) * 128], wd_v[:, jn, half * 512:(half + 1) * 512],
                                           fc == 0, fc == 27, [r_hT[bi], rwd], [r_ps[accp[t]]])
                            hs = slice(half * 512, (half + 1) * 512)
                            for t in range(4):
                                ACT(yo[t][:, hs], psb[accp[t]][:], AF.Copy, [r_ps[accp[t]]], [r_yo[t]])
                                if half == 1:
                                    s0 = j * 512 + t * 128
                                    S.dma("sp", ygs[s0:s0 + 128, :], yo[t][:], reads=[r_yo[t]], writes=[r_yg[j]], sres=r_yo[t], load=False)
                with ExitStack() as st:
                    lnf_g = sb(st, "elnf_g", [128, D], F32)
                    lnf_b = sb(st, "elnf_b", [128, D], F32)
                    r_lnf = S.res("elnf")
                    S.dma("pool", lnf_g[:], ln_fg[l].partition_broadcast(128), writes=[r_lnf], sres=r_lnf)
                    S.dma("pool", lnf_b[:], ln_fb[l].partition_broadcast(128), writes=[r_lnf], sres=r_lnf)
                    y0 = [sb(st, f"oy0_{i}", [128, D], F32) for i in range(2)]
                    y1 = [sb(st, f"oy1_{i}", [128, D], F32) for i in range(2)]
                    r_y0 = [S.res(f"oy0_{i}") for i in range(2)]
                    r_y1 = [S.res(f"oy1_{i}") for i in range(2)]
                    tin = [sb(st, f"otin{i}", [128, D], F32) for i in range(2)]
                    r_tin = [S.res(f"otin{i}") for i in range(2)]
                    yo = [sb(st, f"oyo{i}", [128, D], F32) for i in range(2)]
                    r_yo = [S.res(f"oyo{i}") for i in range(2)]
                    lst = sb(st, "olst", [128, 16], F32)
                    r_lst = S.res("olst")
                    for i in range(NT):
                        b = i % 2
                        r0 = i * 128
                        gi = r0 // G
                        S.dma("sp", tin[b][:], x1s[r0:r0 + 128, :], reads=[r_x1s[gi]], writes=[r_tin[b]], sres=r_tin[b])
                        for yt, ryt, sli in ((y0, r_y0, sl0i), (y1, r_y1, sl1i)):
                            S.dma("pool", None, None, reads=[r_sli] + r_yg, writes=[ryt[b]], sres=ryt[b],
                                  fn=(lambda e, yt=yt, sli=sli, b=b, i=i: e.indirect_dma_start(out=yt[b][:], out_offset=None, in_=ygs[:, :],
                                                                                                in_offset=IOA(ap=sli[:, i:i + 1], axis=0))))
                        ACT(tin[b][:], tin[b][:], AF.Copy, [r_tin[b]], [r_tin[b]], scale=ALPHA)
                        V("dve", "scalar_tensor_tensor", [r_y0[b], r_cwk, r_tin[b]], [r_tin[b]], out=tin[b][:], in0=y0[b][:], scalar=cwk[:, i, 0:1],
                          in1=tin[b][:], op0=ALU.mult, op1=ALU.add)
                        V("dve", "scalar_tensor_tensor", [r_y1[b], r_cwk, r_tin[b]], [r_tin[b]], out=tin[b][:], in0=y1[b][:], scalar=cwk[:, i, 1:2],
                          in1=tin[b][:], op0=ALU.mult, op1=ALU.add)
                        layer_norm_tile((lst, r_lst), tin[b], r_tin[b], lnf_g, lnf_b, [r_lnf], yo[b][:], r_yo[b], eng="dve")
                        o = S.dma("sp", dst[r0:r0 + 128, :], yo[b][:], reads=[r_yo[b]], writes=[r_dst[gi]], sres=r_yo[b], load=False)
                        if is_final:
                            final_tokens.append(o)
            S.barrier()

        r_xin = [S.res(f"xin{i}") for i in range(T // G)]
        r_yout = [S.res(f"yout{i}") for i in range(T // G)]
        pend = []
        for n_, l in enumerate(cfg.layers):
            if n_ == 0 and cfg.do_mix:
                prepass_layer(l)
            else:
                pend.append(lambda l=l: prepass_layer(l))
            if l % 2 == 0:
                pend.append(prepass_ffn)
            else:
                for e in range(NE):
                    pend.append(lambda e=e: prepass_moe([e]))
        if not cfg.do_mix:
            while pend:
                pend.pop(0)()
        last = cfg.layers[-1]
        for l in cfg.layers:
            src, rsrc = (x_in, r_xin) if l == cfg.layers[0] else (xs1, r_xs1)
            if cfg.do_mix:
                hk, pend = pend, []
                phase_mixer(l, src, rsrc, hooks=hk)
            if (not cfg.do_mix) or ("B" not in cfg.parts):
                for gi in range(T // G):
                    S.dma("pool", x1s[gi * G:(gi + 1) * G, :], src[gi * G:(gi + 1) * G, :], reads=[rsrc[gi]], writes=[r_x1s[gi]],
                          sres=r_x1s[gi])
            if not cfg.do_ffn:
                for gi in range(T // G):
                    o = S.dma("pool", y_out[gi * G:(gi + 1) * G, :], x1s[gi * G:(gi + 1) * G, :], reads=[r_x1s[gi]], writes=[r_yout[gi]],
                              sres=r_yout[gi])
                    final_tokens.append(o)
            if cfg.do_ffn:
                ph = phase_moe if (l % 2 == 1 and cfg.sparse) else phase_ffn
                if l == last:
                    ph(l, y_out, r_yout, True)
                else:
                    ph(l, xs1, r_xs1, False)
        stats = S.emit(final_tokens)
        print("instr/wait per engine:", stats, "sems:", S.nsem, flush=True)
    return nc


def host_consts(S_):
    c = {}
    c["c_ident"] = np.eye(128, dtype=np.float32)
    inv = np.power(np.float32(500000.0), -np.arange(0, 16, 2, dtype=np.float32) / np.float32(16)).astype(np.float32)
    ang = (np.arange(S_, dtype=np.float32)[:, None] * inv[None, :]).astype(np.float32)
    co, si = np.cos(ang).astype(np.float32), np.sin(ang).astype(np.float32)
    c["c_cc"] = np.concatenate([co, co], 1).astype(np.float32)
    c["c_ss"] = np.concatenate([-si, si], 1).astype(np.float32)
    p = np.arange(128)[:, None]
    f = np.arange(512)[None, :]
    c["c_cmb"] = np.stack([np.where(cl * 128 + p <= f, 0.0, NEG) for cl in range(4)]).astype(np.float32)
    tok = np.arange(S_)[None, :]
    c["c_oh"] = (tok // MBLK == np.arange(16)[:, None]).astype(np.float32)
    j = np.tile(np.arange(16), MH)[None, :]
    blk = np.arange(16)[:, None]
    pm = (j < blk).astype(np.float32)
    c["c_pm"] = pm
    c["c_pa"] = ((pm - 1.0) * BIG).astype(np.float32)
    c["c_own"] = (j == blk).astype(np.float32)
    s = np.arange(128)[:, None]
    t = np.arange(128)[None, :]
    c["c_u"] = np.where(s <= t, -1.0 / 16.0, 0.0).astype(np.float32)
    c["c_tri"] = (s <= t).astype(np.float32)
    c["c_tris"] = (s < t).astype(np.float32)
    return c


def host_consts_moe(T):
    c = {}
    ntt = T // 128
    ngrp = (2 * T) // 512 + NE
    p = np.arange(128)[:, None]
    c["c_tok"] = (np.arange(ntt)[None, :] * 128 + p).astype(np.int32)
    tab, _ = block_table()
    base_m = tab[("m", 0, "g", 0)]
    c["c_wcst"] = (np.arange(21)[None, :] * 128 + p).astype(np.float32)
    c["c_jj"] = np.broadcast_to(np.arange(ngrp, dtype=np.float32)[None, :], (128, ngrp)).copy()
    c["c_thr"] = np.broadcast_to((np.arange(16, dtype=np.float32) * 512.0)[None, :], (128, 16)).copy()
    return c


_CACHE = {}


def kernel(**inputs):
    ncores = 8
    S_ = 4096
    nseq = 2
    if "prog" not in _CACHE:
        _CACHE["prog"] = build_program(Cfg(S=S_, NSEQ=nseq))
    nc = _CACHE["prog"]
    x = np.ascontiguousarray(np.asarray(inputs["x"], dtype=np.float32))
    consts = host_consts(S_)
    consts.update(host_consts_moe(S_ * nseq))
    shared = {k: np.ascontiguousarray(np.asarray(v, dtype=np.float32)) for k, v in inputs.items() if k != "x"}
    shared["moe_w_router_T"] = np.ascontiguousarray(shared["moe_w_router"][0].T)
    in_maps = []
    for c in range(ncores):
        m = dict(shared)
        m.update(consts)
        m["x"] = x[c * nseq:(c + 1) * nseq].reshape(nseq * S_, D)
        in_maps.append(m)
    res = run_bass_kernel_spmd(nc, in_maps, core_ids=list(range(ncores)))
    out = np.concatenate([np.asarray(r["y"]).reshape(nseq, S_, D) for r in res.results], axis=0)
    return out.astype(np.float32)
```

```python
import math
from contextlib import ExitStack

import numpy as np
import concourse.bass as bass
import concourse.mybir as mybir
from concourse.bass_utils import run_bass_kernel_spmd

F32 = mybir.dt.float32
BF16 = mybir.dt.bfloat16
I32 = mybir.dt.int32
AF = mybir.ActivationFunctionType
ALU = mybir.AluOpType
AX = mybir.AxisListType

D = 1024
DEPTH = 2
MH, MD = 8, 64
MBLK = 256
GH, GK = 4, 128
FF = 3584
NE = 8
IN_COLS = 5648
ALPHA = (2 * DEPTH) ** 0.25
LN_EPS = 1e-5
G = 512
NEG = -240000.0
BIG = 1.0e30

ENGS = ("pe", "act", "dve", "pool", "sp")
EPOCH = 12000
DMA_EPOCH = 1500


class Res:
    __slots__ = ("name", "writer", "readers", "stream_w", "stream_r")

    def __init__(self, name):
        self.name = name
        self.writer = None
        self.readers = []
        self.stream_w = None
        self.stream_r = None


class Stream:
    __slots__ = ("sem", "count", "name", "q")

    def __init__(self, name):
        self.name = name
        self.sem = None
        self.count = 0
        self.q = None


class Op:
    __slots__ = ("eng", "fn", "deps", "needs_inc", "epoch", "val", "is_dma", "stream", "dval")

    def __init__(self, eng, fn):
        self.eng = eng
        self.fn = fn
        self.deps = []
        self.needs_inc = False
        self.epoch = 0
        self.val = 0
        self.is_dma = False
        self.stream = None
        self.dval = 0


class Sched:
    def __init__(self, nc, stack):
        self.nc = nc
        self.stack = stack
        self.ops = {e: [] for e in ENGS}
        self.nsem = 0
        self.all_res = []
        self.live_streams = []
        self.free_streams = {"pool": [], "sp": []}

    def res(self, name):
        r = Res(name)
        self.all_res.append(r)
        return r

    def _newsem(self, name):
        self.nsem += 1
        return self.stack.enter_context(self.nc.semaphore(f"{name[:40]}_{self.nsem}"))

    def _collect(self, op, reads, writes):
        deps = []
        for r in reads:
            if r.writer is not None:
                deps.append((r.writer, "raw"))
        for w in writes:
            if w.writer is not None:
                deps.append((w.writer, "waw"))
            for t in w.readers:
                deps.append((t, "war"))
        seen = set()
        for t, kind in deps:
            if t is op or id(t) in seen:
                continue
            if (not t.is_dma) and (not op.is_dma) and t.eng == op.eng:
                if op.eng == "pe":
                    continue
            seen.add(id(t))
            op.deps.append(t)
            if not t.is_dma:
                t.needs_inc = True
        for r in reads:
            if not op.is_dma:
                r.readers = [t for t in r.readers if t.is_dma or t.eng != op.eng]
            r.readers.append(op)
        for w in writes:
            w.writer = op
            w.readers = []

    def op(self, eng, fn, reads=(), writes=()):
        o = Op(eng, fn)
        self._collect(o, reads, writes)
        self.ops[eng].append(o)
        return o

    def new_stream(self, name):
        st = Stream(name)
        st.sem = self._newsem(name[:48])
        return st

    def dma(self, q, out, in_, reads=(), writes=(), sres=None, load=True, stream=None, fn=None):
        o = Op(q, fn if fn is not None else (lambda e: e.dma_start(out=out, in_=in_)))
        o.is_dma = True
        st = stream if stream is not None else (sres.stream_w if load else sres.stream_r)
        if stream is None and (st is None or st.count >= DMA_EPOCH or st.q != q):
            fl = self.free_streams[q]
            while fl and fl[-1].count >= DMA_EPOCH - 64:
                fl.pop()
            if fl:
                st = fl.pop()
            else:
                st = Stream(sres.name + ("_w" if load else "_r"))
                st.sem = self._newsem(st.name[:48])
                st.q = q
            self.live_streams.append(st)
            if load:
                sres.stream_w = st
            else:
                sres.stream_r = st
        st.count += 1
        o.stream = st
        o.dval = st.count * 16
        self._collect(o, reads, writes)
        self.ops[q].append(o)
        return o

    def barrier(self):
        allr = list(self.all_res)
        for e in ENGS:
            if e == "sp":
                continue
            self.op(e, lambda e_: e_.drain(), reads=(), writes=allr)
        for r in allr:
            r.stream_w = None
            r.stream_r = None
        for st in self.live_streams:
            self.free_streams[st.q].append(st)
        self.live_streams = []

    def emit(self, final_tokens=()):
        nc = self.nc
        esems = {}
        for e in ENGS:
            cnt = 0
            ep = 0
            for o in self.ops[e]:
                if o.is_dma or not o.needs_inc:
                    continue
                if cnt >= EPOCH:
                    ep += 1
                    cnt = 0
                cnt += 1
                o.epoch = ep
                o.val = cnt
                if (e, ep) not in esems:
                    esems[(e, ep)] = self._newsem(f"s_{e}_{ep}")
        stats = {e: [0, 0] for e in ENGS}

        def run(e):
            def body(eng):
                known = {}
                for o in self.ops[e]:
                    need = {}
                    for t in o.deps:
                        if t.is_dma:
                            sem, val = t.stream.sem, t.dval
                        else:
                            sem, val = esems[(t.eng, t.epoch)], t.val
                        k = id(sem)
                        if k not in need or need[k][1] < val:
                            need[k] = (sem, val)
                    for k, (sem, val) in need.items():
                        if known.get(k, 0) >= val:
                            continue
                        known[k] = val
                        eng.wait_ge(sem, val)
                        stats[e][1] += 1
                    ins = o.fn(eng)
                    stats[e][0] += 1
                    if o.is_dma:
                        ins.then_inc(o.stream.sem, 16)
                    elif o.needs_inc:
                        ins.then_inc(esems[(e, o.epoch)], 1)
                if e == "pool":
                    for t in final_tokens:
                        eng.wait_ge(t.stream.sem, t.dval)
            return body

        with nc.Block() as block:
            block.tensor(run("pe"))
            block.scalar(run("act"))
            block.vector(run("dve"))
            block.gpsimd(run("pool"))
            block.sync(run("sp"))
        return stats


INP = [("qm", 0), ("km", 512), ("vm", 1024), ("qg", 1536), ("kg", 2048), ("vg", 2560), ("rg", 3072),
       ("ga0", 3600), ("ga1", 4112), ("gb0", 4624), ("gb1", 5136)]
LG_OFF = 3584


def block_table():
    tab = {}
    n = 0

    def add(key):
        nonlocal n
        tab[key] = n
        n += 1

    for l in range(DEPTH):
        for name, _ in INP:
            add((l, name))
        add((l, "lg"))
        add((l, "wa"))
        add((l, "wb"))
        add((l, "wo0"))
        add((l, "wo1"))
    for i in range(7):
        add(("f", "g", i))
    for i in range(7):
        add(("f", "u", i))
    for i in range(7):
        add(("f", "d", i))
    for e in range(NE):
        for i in range(7):
            add(("m", e, "g", i))
        for i in range(7):
            add(("m", e, "u", i))
        for i in range(7):
            add(("m", e, "d", i))
    return tab, n


class Cfg:
    def __init__(self, S=4096, NSEQ=2, layers=(0, 1), do_mix=True, do_ffn=True, dbg=False, parts="AB", skip=()):
        self.S = S
        self.NSEQ = NSEQ
        self.T = S * NSEQ
        self.layers = layers
        self.do_mix = do_mix
        self.do_ffn = do_ffn
        self.dbg = dbg
        self.parts = parts
        self.sparse = True
        self.skip = set(skip)


def build_program(cfg):
    S_, NSEQ, T = cfg.S, cfg.NSEQ, cfg.T
    NG = S_ // G
    NTS = S_ // 128
    NBK = S_ // MBLK
    nc = bass.Bass("TRN2", target_bir_lowering=False)

    def din(name, shape, dt=F32):
        return nc.dram_tensor(name, list(shape), dt, kind="ExternalInput").ap()

    x_in = din("x", [T, D])
    w_in = din("w_in", [DEPTH, D, IN_COLS])
    w_up = din("w_gla_gate_up", [DEPTH, 16, 512])
    b_gg = din("b_gla_gate", [DEPTH, 512])
    gn_g = din("gla_norm_g", [DEPTH, 512])
    w_ba = din("w_branch_a", [DEPTH, 512, D])
    w_bb = din("w_branch_b", [DEPTH, 512, D])
    w_o = din("w_out", [DEPTH, D, D])
    ln_mg = din("ln_mix_g", [DEPTH, D])
    ln_mb = din("ln_mix_b", [DEPTH, D])
    f_g = din("ffn_w_gate", [1, D, FF])
    f_u = din("ffn_w_up", [1, D, FF])
    f_d = din("ffn_w_down", [1, FF, D])
    m_r = din("moe_w_router", [1, D, NE])
    m_g = din("moe_w_gate", [1, NE, D, FF])
    m_u = din("moe_w_up", [1, NE, D, FF])
    m_d = din("moe_w_down", [1, NE, FF, D])
    ln_fg = din("ln_ffn_g", [DEPTH, D])
    ln_fb = din("ln_ffn_b", [DEPTH, D])
    c_ident = din("c_ident", [128, 128])
    c_cc = din("c_cc", [S_, 16])
    c_ss = din("c_ss", [S_, 16])
    c_cmb = din("c_cmb", [4, 128, 512])
    c_oh = din("c_oh", [16, S_])
    c_pm = din("c_pm", [16, 128])
    c_pa = din("c_pa", [16, 128])
    c_own = din("c_own", [16, 128])
    c_u = din("c_u", [128, 128])
    c_tri = din("c_tri", [128, 128])
    m_rT = din("moe_w_router_T", [NE, D])
    NTT = T // 128
    NGRP = (2 * T + NE * 511) // 512
    NSLOT = NGRP * 512
    c_tok = din("c_tok", [128, NTT], I32)
    c_wcst = din("c_wcst", [128, 21])
    c_jj = din("c_jj", [128, NGRP])
    c_thr = din("c_thr", [128, 16])
    c_tris = din("c_tris", [128, 128])

    tab, NBLKS = block_table()
    BASE_M = tab[("m", 0, "g", 0)]
    wsc_d = nc.dram_tensor("wsc", [BASE_M, 128, 4096], BF16, kind="Internal").ap()
    wsm = nc.dram_tensor("wsm", [NE * 21, 128, 4096], BF16, kind="Internal").ap()

    class _W:
        def __getitem__(self, b):
            return wsc_d[b] if b < BASE_M else wsm[b - BASE_M]
    wsc = _W()
    x1s = nc.dram_tensor("x1s", [T, D], F32, kind="Internal").ap()
    xs1 = nc.dram_tensor("xs1", [T, D], F32, kind="Internal").ap()
    yas = nc.dram_tensor("yas", [T // G, 128, 4, G], BF16, kind="Internal").ap()
    s2t = nc.dram_tensor("s2t", [NSLOT, 1], I32, kind="Internal").ap()
    ygs = nc.dram_tensor("ygs", [NSLOT, D], F32, kind="Internal").ap()
    y_out = nc.dram_tensor("y", [T, D], F32, kind="ExternalOutput").ap()
    dbg_t = {}
    if cfg.dbg:
        NGT = T // G
        for nm, shp in (("ya", [NGT, 128, 4, 512]), ("ybT", [NGT, 128, 4, G]), ("yaT", [NGT, 128, 4, G]), ("QT", [NGT, 128, MH, G]),
                        ("mT", [NGT, 128, 8, G]), ("BT", [NGT, 16, MH, G])):
            dbg_t[nm] = nc.dram_tensor("dbg_" + nm, shp, BF16, kind="ExternalOutput").ap()

    with ExitStack() as top:
        S = Sched(nc, top)
        r_wsc = [S.res(f"wsc{i}") for i in range(NBLKS)]
        r_x1s = [S.res(f"x1s{i}") for i in range(T // G)]
        r_xs1 = [S.res(f"xs1{i}") for i in range(T // G)]
        r_yas = [S.res(f"yas{i}") for i in range(T // G)]
        final_tokens = []
        r_dbg = S.res("dbg")

        pre = {"st": None, "ops": []}

        def pre_begin(name):
            pre["st"] = S.new_stream(name)
            pre["ops"] = []

        def pre_end():
            for o in pre["ops"]:
                o.dval = pre["st"].count * 16

        def conv(key, src, view):
            b = tab[key]
            if view == "c8":
                dst = wsc[b].rearrange("p (c n) -> p c n", c=8)
                s = src.rearrange("(c p) n -> p c n", p=128)
            elif view == "c4":
                dst = wsc[b].rearrange("p (c n) -> p c n", c=4)
                s = src.rearrange("(c p) n -> p c n", p=128)
            elif view == "lg":
                dst = wsc[b][:, 0:128].rearrange("p (c n) -> p c n", c=8)
                s = src.rearrange("(c p) n -> p c n", p=128)
            pre["ops"].append(S.dma("pool", dst, s, writes=[r_wsc[b]], sres=r_wsc[b], load=True, stream=pre["st"]))

        def prepass_layer(l):
            pre_begin(f"pre_l{l}a")
            for name, off in INP[:3]:
                conv((l, name), w_in[l][:, off:off + 512], "c8")
            pre_end()
            pre_begin(f"pre_l{l}")
            for name, off in INP[3:]:
                conv((l, name), w_in[l][:, off:off + 512], "c8")
            conv((l, "lg"), w_in[l][:, LG_OFF:LG_OFF + 16], "lg")
            conv((l, "wa"), w_ba[l], "c4")
            conv((l, "wb"), w_bb[l], "c4")
            conv((l, "wo0"), w_o[l][:, 0:512], "c8")
            conv((l, "wo1"), w_o[l][:, 512:1024], "c8")
            pre_end()

        def prepass_ffn():
            pre_begin("pre_ffn")
            for i in range(7):
                conv(("f", "g", i), f_g[0][:, i * 512:(i + 1) * 512], "c8")
                conv(("f", "u", i), f_u[0][:, i * 512:(i + 1) * 512], "c8")
                conv(("f", "d", i), f_d[0][i * 512:(i + 1) * 512, :], "c4")
            pre_end()

        def prepass_moe(experts=range(NE)):
            for e in experts:
                pre_begin(f"pre_moe{e}")
                for i in range(7):
                    conv(("m", e, "g", i), m_g[0][e][:, i * 512:(i + 1) * 512], "c8")
                    conv(("m", e, "u", i), m_u[0][e][:, i * 512:(i + 1) * 512], "c8")
                    conv(("m", e, "d", i), m_d[0][e][i * 512:(i + 1) * 512, :], "c4")
                pre_end()

        sbn = {"n": 0}

        def sb(st, name, shape, dt):
            sbn["n"] += 1
            return st.enter_context(nc.sbuf_tensor(f"{name}_{sbn['n']}", list(shape), dt))

        psb = [top.enter_context(nc.psum_tensor(f"ps{i}", [128, 512], F32)) for i in range(8)]
        r_ps = [S.res(f"ps{i}") for i in range(8)]

        identb = sb(top, "identb", [128, 128], BF16)
        r_ident = S.res("ident")
        S.dma("pool", identb[:], c_ident, writes=[r_ident], sres=r_ident)
        lgR = sb(top, "lgR", [128, T // 128, NE], F32)
        r_lgR = S.res("lgR")
        identf = sb(top, "identf", [128, 128], F32)
        r_identf = S.res("identf")
        S.dma("pool", identf[:], c_ident, writes=[r_identf], sres=r_identf)

        wring = []
        r_wring = []
        wstate = {"i": 0}

        def set_ring(st, n):
            wring[:] = [sb(st, f"wring{i}", [128, 4096], BF16) for i in range(n)]
            r_wring[:] = [S.res(f"wring{i}") for i in range(n)]
            wstate["i"] = 0

        def wload(key, view):
            i = wstate["i"] % len(wring)
            wstate["i"] += 1
            b = tab[key]
            if view == "lg":
                S.dma("sp", wring[i][:, 0:128], wsc[b][:, 0:128], reads=[r_wsc[b]], writes=[r_wring[i]], sres=r_wring[i])
            else:
                S.dma("sp", wring[i][:], wsc[b], reads=[r_wsc[b]], writes=[r_wring[i]], sres=r_wring[i])
            t = wring[i]
            if view == "c8":
                v = t[:].rearrange("p (c n) -> p c n", c=8)
            elif view == "c4":
                v = t[:].rearrange("p (c n) -> p c n", c=4)
            else:
                v = t[:, 0:128].rearrange("p (c n) -> p c n", c=8)
            return v, r_wring[i]

        def MM(out, lhsT, rhs, start, stop, R, W, **kw):
            S.op("pe", lambda e: e.matmul(out, lhsT=lhsT, rhs=rhs, start=start, stop=stop, **kw), reads=R, writes=W)

        def TR(out, in_, ident, R, W):
            S.op("pe", lambda e: e.transpose(out=out, in_=in_, identity=ident), reads=R, writes=W)

        def ACT(out, in_, func, R, W, **kw):
            S.op("act", lambda e: e.activation(out=out, in_=in_, func=func, **kw), reads=R, writes=W)

        def V(eng, meth, R, W, **kw):
            S.op(eng, lambda e: getattr(e, meth)(**kw), reads=R, writes=W)

        def layer_norm_tile(st_tiles, tin, r_tin, gb, bb, r_gbl, out, r_out, eng="dve"):
            stt, r_st = st_tiles
            V("dve", "bn_stats", [r_tin], [r_st], out=stt[:, 0:6], in_=tin[:, 0:512])
            V("dve", "bn_stats", [r_tin], [r_st], out=stt[:, 6:12], in_=tin[:, 512:1024])
            V("dve", "bn_aggr", [r_st], [r_st], out=stt[:, 12:14], in_=stt[:, 0:12])
            V("dve", "tensor_scalar_add", [r_st], [r_st], out=stt[:, 13:14], in0=stt[:, 13:14], scalar1=LN_EPS)
            ACT(stt[:, 13:14], stt[:, 13:14], AF.Sqrt, [r_st], [r_st])
            V("dve", "reciprocal", [r_st], [r_st], out=stt[:, 13:14], in_=stt[:, 13:14])
            V("dve", "tensor_scalar", [r_tin, r_st], [r_tin], out=tin[:], in0=tin[:], scalar1=stt[:, 12:13],
              scalar2=stt[:, 13:14], op0=ALU.subtract, op1=ALU.mult)
            V(eng, "tensor_tensor", [r_tin] + r_gbl, [r_tin], out=tin[:], in0=tin[:], in1=gb[:], op=ALU.mult)
            V(eng, "tensor_tensor", [r_tin] + r_gbl, [r_out], out=out, in0=tin[:], in1=bb[:], op=ALU.add)

        def phase_mixer(l, x_src, r_xsrc, hooks=(), router=False):
            hooks = list(hooks)
            pbig = [0, 1]
            pst = [3, 4]
            PACCS, PTR, PMISC = (2, 5), 6, 7

            def cload(st, name, shape, dt, src, q="pool"):
                t = sb(st, name, shape, dt)
                r = S.res(name)
                S.dma(q, t[:], src, writes=[r], sres=r)
                return t, r

            with ExitStack() as st:
              if "A" in cfg.parts:
                  set_ring(st, 4)
                  cc, r_cc = cload(st, "cc", [128, NTS, 16], F32, c_cc.rearrange("(t p) k -> p t k", p=128))
                  ss, r_ss = cload(st, "ss", [128, NTS, 16], F32, c_ss.rearrange("(t p) k -> p t k", p=128))
                  cmb, r_cmb = cload(st, "cmb", [128, 4, 512], BF16, c_cmb.rearrange("c p n -> p c n"))
                  oh, r_oh = cload(st, "oh", [16, S_], BF16, c_oh)
                  onesb = sb(st, "onesb", [128, 1], BF16)
                  r_ones = S.res("onesb")
                  V("pool", "memset", [], [r_ones], ap=onesb[:], constant=1.0)
                  zerob = sb(st, "zerob", [128, 512], BF16)
                  r_zero = S.res("zerob")
                  V("pool", "memset", [], [r_zero], ap=zerob[:], constant=0.0)

                  KT = sb(st, "KT", [128, 4, S_], BF16)
                  r_KT = [S.res(f"KT{g}") for g in range(NG)]
                  VA = sb(st, "VA", [128, NTS, MH, MD + 1], BF16)
                  r_VA = [S.res(f"VA{g}") for g in range(NG)]
                  V("pool", "memset", [], r_VA, ap=VA[:, :, :, MD:MD + 1], constant=1.0)
                  kmT = sb(st, "kmT", [128, 4, 16], BF16)
                  r_kmT = S.res("kmT")

                  NXB = 2
                  xf = [sb(st, f"xf{i}", [128, D], F32) for i in range(NXB)]
                  r_xf = [S.res(f"xf{i}") for i in range(NXB)]
                  xb = [sb(st, f"xb{i}", [128, D], BF16) for i in range(NXB)]
                  r_xb = [S.res(f"xb{i}") for i in range(NXB)]
                  xT = sb(st, "xT", [128, 8, G], BF16)
                  r_xT = S.res("xT")
                  QA = sb(st, "QA", [128, 4, 512], BF16)
                  r_QA = [S.res(f"QA{t}") for t in range(4)]
                  KA = sb(st, "KA", [128, 4, 512], BF16)
                  r_KA = [S.res(f"KA{t}") for t in range(4)]
                  QTs = [sb(st, f"QT{i}", [128, MH, G], BF16) for i in range(2)]
                  r_QTs = [S.res(f"QT{i}") for i in range(2)]
                  for i in range(2):
                      V("pool", "memset", [], [r_QTs[i]], ap=QTs[i][:], constant=0.0)
                  BTs = [sb(st, f"BT{i}", [16, MH, G], BF16) for i in range(2)]
                  r_BTs = [S.res(f"BT{i}") for i in range(2)]
                  gsb = sb(st, "gsb", [128, 6, 128], F32)
                  r_gsb = S.res("gsb")
                  gm = sb(st, "gm", [128, 3, 8], F32)
                  ksum = sb(st, "ksum", [128, 16], F32)
                  r_gm = S.res("gm")
                  bias_tm = sb(st, "bias_tm", [128, 128], BF16)
                  r_btm = S.res("bias_tm")
                  pmt = sb(st, "pmt", [128, 2, 3, 128], F32)
                  r_pmt = S.res("pmt")
                  rp = sb(st, "rp", [128, 2, 8, 16], F32)
                  r_rp = S.res("rp")
                  NPT = 4
                  PT = [sb(st, f"PT{i}", [128, G], BF16) for i in range(NPT)]
                  r_PT = [S.res(f"PT{i}") for i in range(NPT)]
                  rcp = sb(st, "rcp", [128, 2, 4], F32)
                  r_rcp = [S.res("rcp0"), S.res("rcp1")]
                  ya = sb(st, "ya", [128, 4, 512], BF16)
                  r_ya = S.res("ya")
                  yaT = [sb(st, f"yaT{i}", [128, 4, G], BF16) for i in range(2)]
                  r_yaT = [S.res(f"yaT{i}") for i in range(2)]
                  ring = {"big": 0, "st": 0, "pt": 0, "x": 0}

                  def nbig():
                      i = pbig[ring["big"] % len(pbig)]
                      ring["big"] += 1
                      return i

                  def prelude(s, g):
                      QTb, r_QTb, BTb, r_BTb = QTs[g % 2], r_QTs[g % 2], BTs[g % 2], r_BTs[g % 2]
                      tok0 = s * S_ + g * G
                      gi = tok0 // G
                      for t in range(4):
                          i = ring["x"] % NXB
                          ring["x"] += 1
                          r0 = tok0 + t * 128
                          S.dma("sp", xf[i][:], x_src[r0:r0 + 128, :], reads=[r_xsrc[gi]], writes=[r_xf[i]], sres=r_xf[i])
                          ACT(xb[i][:], xf[i][:], AF.Copy, [r_xf[i]], [r_xb[i]])
                          ptr = psb[PTR][:].bitcast(BF16).rearrange("p (c n) -> p c n", c=8)
                          for c in range(8):
                              TR(ptr[:, c, :], xb[i][:, c * 128:(c + 1) * 128], identb[:], [r_xb[i], r_ident], [r_ps[PTR]])
                          V("dve", "tensor_copy", [r_ps[PTR]], [r_xT], out=xT[:, :, t * 128:(t + 1) * 128], in_=ptr)

                      def proj_tm(key, consume):
                          wv, rw = wload((l, key), "c8")
                          for t in range(4):
                              pi = nbig()
                              for c in range(8):
                                  MM(psb[pi][:], xT[:, c, t * 128:(t + 1) * 128], wv[:, c, :], c == 0, c == 7,
                                     [r_xT, rw], [r_ps[pi]])
                              consume(t, psb[pi], r_ps[pi])

                      def rope_to(dst, r_dst):
                          def consume(t, ps, rps):
                              tt = g * 4 + t
                              p3 = ps[:].rearrange("p (h d) -> p h d", h=MH)
                              d3 = dst[:, t, :].rearrange("p (h d) -> p h d", h=MH)
                              ccb = cc[:, tt, :].unsqueeze(1).broadcast_to([128, MH, 16])
                              ssb = ss[:, tt, :].unsqueeze(1).broadcast_to([128, MH, 16])
                              V("dve", "tensor_tensor", [rps, r_cc], [r_rp], out=rp[:, 0, :, :], in0=p3[:, :, 0:16], in1=ccb, op=ALU.mult)
                              V("dve", "tensor_tensor", [rps, r_ss], [r_rp], out=rp[:, 1, :, 0:8], in0=p3[:, :, 8:16], in1=ssb[:, :, 0:8], op=ALU.mult)
                              V("dve", "tensor_tensor", [rps, r_ss], [r_rp], out=rp[:, 1, :, 8:16], in0=p3[:, :, 0:8], in1=ssb[:, :, 8:16], op=ALU.mult)
                              V("dve", "tensor_tensor", [r_rp], [r_dst[t]], out=d3[:, :, 0:16], in0=rp[:, 0, :, :], in1=rp[:, 1, :, :], op=ALU.add)
                              ACT(d3[:, :, 16:64], p3[:, :, 16:64], AF.Copy, [rps], [r_dst[t]])
                          return consume

                      proj_tm("qm", rope_to(QA, r_QA))
                      proj_tm("km", rope_to(KA, r_KA))

                      def cons_v(t, ps, rps):
                          tt = g * 4 + t
                          ACT(VA[:, tt, :, 0:MD], ps[:].rearrange("p (h d) -> p h d", h=MH), AF.Copy, [rps], [r_VA[g]])
                      proj_tm("vm", cons_v)

                      yield
                      ptr4 = psb[PTR][:].bitcast(BF16).rearrange("p (c n) -> p c n", c=8)
                      for t in range(4):
                          tt = g * 4 + t
                          for pr in range(4):
                              TR(ptr4[:, pr, :], KA[:, t, pr * 128:(pr + 1) * 128], identb[:], [r_KA[t], r_ident], [r_ps[PTR]])
                          V("dve", "tensor_copy", [r_ps[PTR]], [r_KT[g]], out=KT[:, :, tt * 128:(tt + 1) * 128], in_=ptr4[:, 0:4, :])
                      pm3 = psb[PMISC][:, 0:16].rearrange("p (a b) -> p a b", a=4)
                      for t in range(4):
                          for pr in range(4):
                              if "ksum" in cfg.skip:
                                  continue
                              MM(pm3[:, pr, t:t + 1], KA[:, t, pr * 128:(pr + 1) * 128], onesb[:, 0:1], True, True,
                                 [r_KA[t], r_ones], [r_ps[PMISC]])
                      ks3 = ksum[:].rearrange("p (a b) -> p a b", a=4)
                      if "ksum" in cfg.skip:
                          V("dve", "memset", [], [r_gm], ap=ksum[:], constant=0.0)
                      else:
                          V("dve", "tensor_copy", [r_ps[PMISC]], [r_gm], out=ks3, in_=pm3)
                      for bb_ in range(2):
                          blk = g * 2 + bb_
                          V("dve", "tensor_tensor", [r_gm], [r_gm], out=gm[:, 0, 0:4], in0=ks3[:, :, 2 * bb_],
                            in1=ks3[:, :, 2 * bb_ + 1], op=ALU.add)
                          V("dve", "tensor_scalar_mul", [r_gm], [r_kmT], out=kmT[:, :, blk], in0=gm[:, 0, 0:4], scalar1=1.0 / MBLK)

                      for t in range(4):
                          for pr in range(4):
                              TR(ptr4[:, pr, :], QA[:, t, pr * 128:(pr + 1) * 128], identb[:], [r_QA[t], r_ident], [r_ps[PTR]])
                          QT4 = QTb[:].rearrange("p (a b) n -> p a b n", b=2)
                          V("dve", "tensor_copy", [r_ps[PTR]], [r_QTb], out=QT4[0:64, :, 0, t * 128:(t + 1) * 128], in_=ptr4[0:64, 0:4, :])
                          V("dve", "tensor_copy", [r_ps[PTR]], [r_QTb], out=QT4[64:128, :, 1, t * 128:(t + 1) * 128], in_=ptr4[64:128, 0:4, :])

                      for bb_ in range(2):
                          blk = g * 2 + bb_
                          S.dma("sp", pmt[:, bb_, 0, :], c_pm[blk].partition_broadcast(128), writes=[r_pmt], sres=r_pmt)
                          S.dma("sp", pmt[:, bb_, 1, :], c_pa[blk].partition_broadcast(128), writes=[r_pmt], sres=r_pmt)
                          S.dma("sp", pmt[:, bb_, 2, :], c_own[blk].partition_broadcast(128), writes=[r_pmt], sres=r_pmt)

                      yield
                      if "gate" in cfg.skip:
                          V("dve", "memset", [], [r_BTb], ap=BTb[:], constant=0.0)
                      for t in range(4 if "gate" not in cfg.skip else 0):
                          if t == 2:
                              yield
                          bb_ = t // 2
                          pg = psb[PMISC][:, 128:256]
                          for h in range(MH):
                              pr, hh = h // 2, h % 2
                              MM(pg[:, h * 16:(h + 1) * 16], QTb[:, h, t * 128:(t + 1) * 128],
                                 kmT[:, pr, :], True, True, [r_QTb, r_kmT], [r_ps[PMISC]])
                          g0 = gsb[:, 0, :]
                          g1 = gsb[:, 1, :]
                          e1 = gsb[:, 2, :]
                          V("dve", "tensor_tensor", [r_ps[PMISC], r_pmt], [r_gsb], out=g0, in0=pg, in1=pmt[:, bb_, 0, :], op=ALU.mult)
                          V("dve", "tensor_tensor", [r_gsb, r_pmt], [r_gsb], out=g0, in0=g0, in1=pmt[:, bb_, 1, :], op=ALU.add)
                          g03 = g0.rearrange("p (h j) -> p h j", h=MH)
                          g13 = g1.rearrange("p (h j) -> p h j", h=MH)
                          e13 = e1.rearrange("p (h j) -> p h j", h=MH)
                          src3 = g03
                          for k in range(3):
                              V("dve", "tensor_reduce", [r_gsb], [r_gm], out=gm[:, k, :], in_=src3, axis=AX.X, op=ALU.max)
                              if k == 2:
                                  break
                              mb = gm[:, k, :].unsqueeze(2).broadcast_to([128, MH, 16])
                              V("dve", "tensor_tensor", [r_gsb, r_gm], [r_gsb], out=e13, in0=src3, in1=mb, op=ALU.is_ge)
                              V("dve", "scalar_tensor_tensor", [r_gsb], [r_gsb], out=g13, in0=e13, scalar=-BIG, in1=src3,
                                op0=ALU.mult, op1=ALU.add)
                              src3 = g13
                          mb = gm[:, 2, :].unsqueeze(2).broadcast_to([128, MH, 16])
                          V("dve", "tensor_tensor", [r_gsb, r_gm], [r_gsb], out=e13, in0=g03, in1=mb, op=ALU.is_ge)
                          V("dve", "tensor_tensor", [r_gsb, r_pmt], [r_gsb], out=e1, in0=e1, in1=pmt[:, bb_, 0, :], op=ALU.mult)
                          V("dve", "tensor_tensor", [r_gsb, r_pmt], [r_gsb], out=e1, in0=e1, in1=pmt[:, bb_, 2, :], op=ALU.add)
                          V("dve", "tensor_scalar", [r_gsb], [r_btm], out=bias_tm[:], in0=e1, scalar1=-1.0, scalar2=-NEG,
                            op0=ALU.add, op1=ALU.mult)
                          pb = psb[PTR][:].bitcast(BF16)[0:16, :].rearrange("p (h n) -> p h n", h=MH)
                          for h in range(MH):
                              TR(pb[:, h, :], bias_tm[:, h * 16:(h + 1) * 16], identb[:], [r_btm, r_ident], [r_ps[PTR]])
                          V("dve", "tensor_copy", [r_ps[PTR]], [r_BTb], out=BTb[:, :, t * 128:(t + 1) * 128], in_=pb)


                  def attend(s, g, pre):
                      QTb, r_QTb, BTb, r_BTb = QTs[g % 2], r_QTs[g % 2], BTs[g % 2], r_BTs[g % 2]
                      tok0 = s * S_ + g * G
                      gi = tok0 // G
                      ptr4 = psb[PTR][:].bitcast(BF16).rearrange("p (c n) -> p c n", c=8)
                      nch = 4 * (g + 1)
                      if "attn" in cfg.skip:
                          V("dve", "memset", [], [r_ya], ap=ya[:], constant=0.0)
                      items = [(h, c) for h in range(MH if "attn" not in cfg.skip else 0) for c in range(nch)]
                      PIPE = 2
                      pst4 = [3, 4, 7]
                      slots = {}

                      def emit_st(h, c):
                          pr = h // 2
                          gk = c // 4
                          si = pst4[ring["st"] % 3]
                          ring["st"] += 1
                          own_grp = (gk == g)
                          MM(psb[si][:], KT[:, pr, c * 128:(c + 1) * 128], QTb[:, h, :],
                             True, False, [r_KT[gk], r_QTb], [r_ps[si]])
                          MM(psb[si][:], oh[:, c * 128:(c + 1) * 128], BTb[:, h, :], False, not own_grp,
                             [r_oh, r_BTb], [r_ps[si]])
                          if own_grp:
                              MM(psb[si][:], identb[:], cmb[:, c % 4, :], False, True, [r_ident, r_cmb], [r_ps[si]])
                          pi = ring["pt"] % NPT
                          ring["pt"] += 1
                          ACT(PT[pi][:], psb[si][:], AF.Exp, [r_ps[si]], [r_PT[pi]], scale=1.0 / 8.0)
                          slots[(h, c)] = pi

                      def emit_pv(h, c):
                          gk = c // 4
                          a_i = h % 2
                          PACC = PACCS[a_i]
                          acc = psb[PACC][:, 0:260].rearrange("p (q d) -> p q d", q=4)
                          if c == 0:
                              MM(psb[PACC][:, 0:260], zerob[0:1, 0:128], zerob[0:1, 0:260], True, False,
                                 [r_zero], [r_ps[PACC]], skip_group_check=True)
                          pi = slots.pop((h, c))
                          for qs in range(4):
                              MM(acc[:, qs, :], PT[pi][:, qs * 128:(qs + 1) * 128], VA[:, c, h, :], False, c == nch - 1,
                                 [r_PT[pi], r_VA[gk]], [r_ps[PACC]], skip_group_check=True)
                          if c == nch - 1:
                              V("dve", "reciprocal", [r_ps[PACC]], [r_rcp[a_i]], out=rcp[:, a_i, :], in_=acc[:, :, MD])
                              V("dve", "tensor_tensor", [r_ps[PACC], r_rcp[a_i]], [r_ya], out=ya[:, :, h * MD:(h + 1) * MD],
                                in0=acc[:, :, 0:MD], in1=rcp[:, a_i, :].unsqueeze(2).broadcast_to([128, 4, MD]), op=ALU.mult)

                      pull = {0, len(items) // 4, len(items) // 2, (3 * len(items)) // 4}
                      for idx in range(len(items) + PIPE):
                          if pre is not None and idx in pull:
                              next(pre, None)
                          if idx < len(items):
                              emit_st(*items[idx])
                          if idx >= PIPE:
                              emit_pv(*items[idx - PIPE])
                      yi = gi % 2
                      for t in range(4):
                          for kc in range(4):
                              TR(ptr4[:, kc, :], ya[:, t, kc * 128:(kc + 1) * 128], identb[:], [r_ya, r_ident], [r_ps[PTR]])
                          V("dve", "tensor_copy", [r_ps[PTR]], [r_yaT[yi]], out=yaT[yi][:, :, t * 128:(t + 1) * 128], in_=ptr4[:, 0:4, :])
                      S.dma("sp", yas[gi], yaT[yi][:], reads=[r_yaT[yi]], writes=[r_yas[gi]], sres=r_yaT[yi], load=False)
                      if hooks:
                          hooks.pop(0)()
                      if cfg.dbg:
                          for nm, tl, rr in (("ya", ya, r_ya), ("yaT", yaT[yi], r_yaT[yi]), ("QT", QTb, r_QTb)):
                              final_tokens.append(S.dma("pool", dbg_t[nm][gi], tl[:], reads=[rr], writes=[r_dbg], sres=r_dbg))
                          final_tokens.append(S.dma("pool", dbg_t["BT"][gi], BTb[:], reads=[r_BTb], writes=[r_dbg], sres=r_dbg))
                      if pre is not None:
                          for _ in pre:
                              pass

                  for s in range(NSEQ):
                      V("pool", "memset", [], [r_kmT], ap=kmT[:], constant=0.0)
                      for _ in prelude(s, 0):
                          pass
                      for g in range(NG):
                          attend(s, g, prelude(s, g + 1) if g + 1 < NG else None)
            while hooks:
                hooks.pop(0)()
            S.barrier()

            with ExitStack() as st:
              if "B" in cfg.parts:
                  set_ring(st, 6)
                  uu, r_uu = cload(st, "uu_B", [128, 128], F32, c_u)
                  tri, r_tri = cload(st, "tri_B", [128, 128], F32, c_tri)
                  lnm_g, r_lng = cload(st, "lnm_g_B", [128, D], F32, ln_mg[l].partition_broadcast(128))
                  lnm_b, r_lnb = cload(st, "lnm_b_B", [128, D], F32, ln_mb[l].partition_broadcast(128))
                  gnt, r_gnt = cload(st, "gnt_B", [128, 512], F32, gn_g[l].partition_broadcast(128))
                  wupa = sb(st, "wupa_B", [32, 512], BF16)
                  r_wupa = S.res("wupa")
                  V("pool", "memset", [], [r_wupa], ap=wupa[:], constant=0.0)
                  S.dma("pool", wupa[0:16, :], w_up[l], reads=[], writes=[r_wupa], sres=r_wupa)
                  S.dma("pool", wupa[16:17, :], b_gg[l].unsqueeze(0), reads=[], writes=[r_wupa], sres=r_wupa)

                  stf = sb(st, "stf_B", [128, GH, 128], F32)
                  stb = sb(st, "stb_B", [128, GH, 128], BF16)
                  r_stf = [S.res(f"stf{h}") for h in range(GH)]
                  r_stb = [S.res(f"stb{h}") for h in range(GH)]
                  lgT = sb(st, "lgT_B", [32, G], BF16)
                  r_lgT = S.res("lgT")
                  V("pool", "memset", [], [r_lgT], ap=lgT[:], constant=1.0)

                  NXB = 2
                  xf = [sb(st, f"xf_B{i}", [128, D], F32) for i in range(NXB)]
                  r_xf = [S.res(f"xf{i}") for i in range(NXB)]
                  xb = [sb(st, f"xb_B{i}", [128, D], BF16) for i in range(NXB)]
                  r_xb = [S.res(f"xb{i}") for i in range(NXB)]
                  xT = sb(st, "xT_B", [128, 8, G], BF16)
                  r_xT = S.res("xT")
                  yaT = sb(st, "yaTb_B", [128, 4, G], BF16)
                  r_yaT = S.res("yaTb")
                  ybT = sb(st, "ybT_B", [128, 4, G], BF16)
                  r_ybT = S.res("ybT")
                  spt = sb(st, "spt_B", [128, 4, 512], F32)
                  r_spt = [S.res(f"spt{t}") for t in range(4)]
                  eq = sb(st, "eq_B", [128, GH, G], F32)
                  ek = sb(st, "ek_B", [128, GH, G], F32)
                  r_eq = [S.res(f"eq{h}") for h in range(GH)]
                  r_ek = [S.res(f"ek{h}") for h in range(GH)]
                  qdT = sb(st, "qdT_B", [128, GH, G], BF16)
                  kiT = sb(st, "kiT_B", [128, GH, G], BF16)
                  r_qdT = [S.res(f"qdT{h}") for h in range(GH)]
                  r_kiT = [S.res(f"kiT{h}") for h in range(GH)]
                  kiM = sb(st, "kiM_B", [128, 2, GH, 128], BF16)
                  r_kiM = [S.res("kiM0"), S.res("kiM1")]
                  vg = sb(st, "vg_B", [128, 4, 512], BF16)
                  r_vg = [S.res(f"vg{t}") for t in range(4)]
                  rgs = sb(st, "rgs_B", [128, 4, 512], BF16)
                  r_rgs = [S.res(f"rgs{t}") for t in range(4)]
                  attm = sb(st, "attm_B", [128, 2, GH, 128], BF16)
                  r_attm = [S.res("attm0"), S.res("attm1")]
                  stmp = sb(st, "stmp_B", [128, 2, GH, 128], F32)
                  r_stmp = [S.res("stmp0"), S.res("stmp1")]
                  hn = sb(st, "hn_B", [128, 40], F32)
                  r_hn = S.res("hn")
                  ob = sb(st, "ob_B", [128, 512], F32)
                  r_ob = S.res("ob")
                  yb = sb(st, "yb_B", [128, 512], BF16)
                  r_yb = S.res("yb")
                  sg = sb(st, "sg_B", [128, 2, G], F32)
                  r_sg = [S.res("sg0"), S.res("sg1")]
                  mT = sb(st, "mT_B", [128, 8, G], BF16)
                  r_mT = S.res("mT")
                  tin = [sb(st, f"tin_B{i}", [128, D], F32) for i in range(2)]
                  r_tin = [S.res(f"tin{i}") for i in range(2)]
                  x1o = [sb(st, f"x1o_B{i}", [128, D], F32) for i in range(2)]
                  r_x1o = [S.res(f"x1o{i}") for i in range(2)]
                  lst = sb(st, "lst_B", [128, 16], F32)
                  r_lst = S.res("lst")
                  if router:
                      wrB = sb(st, "wrB", [128, NE, D], F32)
                      r_wrB = S.res("wrB")
                      for e in range(NE):
                          S.dma("pool", wrB[:, e, :], m_rT[e].partition_broadcast(128), writes=[r_wrB], sres=r_wrB)
                  ring = {"big": 0, "x": 0, "ln": 0}

                  pbigB = [0, 1, 2, 4, 5]

                  def nbig():
                      i = pbigB[ring["big"] % len(pbigB)]
                      ring["big"] += 1
                      return i

                  for s in range(NSEQ):
                      V("pool", "memset", [], [r_stf[0]], ap=stf[:], constant=0.0)
                      V("pool", "memset", [], [r_stb[0]], ap=stb[:], constant=0.0)
                      for g in range(NG):
                          tok0 = s * S_ + g * G
                          gi = tok0 // G
                          for t in range(4):
                              i = ring["x"] % NXB
                              ring["x"] += 1
                              r0 = tok0 + t * 128
                              S.dma("pool", xf[i][:], x_src[r0:r0 + 128, :], reads=[r_xsrc[gi]], writes=[r_xf[i]], sres=r_xf[i])
                              ACT(xb[i][:], xf[i][:], AF.Copy, [r_xf[i]], [r_xb[i]])
                              ptr = psb[PTR][:].bitcast(BF16).rearrange("p (c n) -> p c n", c=8)
                              for c in range(8):
                                  TR(ptr[:, c, :], xb[i][:, c * 128:(c + 1) * 128], identb[:], [r_xb[i], r_ident], [r_ps[PTR]])
                              V("dve", "tensor_copy", [r_ps[PTR]], [r_xT], out=xT[:, :, t * 128:(t + 1) * 128], in_=ptr)
                          S.dma("pool", yaT[:], yas[gi], reads=[r_yas[gi]], writes=[r_yaT], sres=r_yaT)
                          ptr4 = psb[PTR][:].bitcast(BF16).rearrange("p (c n) -> p c n", c=8)

                          def proj_tm(key, consume):
                              wv, rw = wload((l, key), "c8")
                              for t in range(4):
                                  pi = nbig()
                                  for c in range(8):
                                      MM(psb[pi][:], xT[:, c, t * 128:(t + 1) * 128], wv[:, c, :], c == 0, c == 7,
                                         [r_xT, rw], [r_ps[pi]])
                                  consume(t, psb[pi], r_ps[pi])

                          wv, rw = wload((l, "lg"), "lg")
                          pi = nbig()
                          for c in range(8):
                              MM(psb[pi][0:16, :], wv[:, c, :], xT[:, c, :], c == 0, c == 7, [r_xT, rw], [r_ps[pi]])
                          V("dve", "tensor_copy", [r_ps[pi]], [r_lgT], out=lgT[0:16, :], in_=psb[pi][0:16, :])
                          for t in range(4):
                              pi = nbig()
                              MM(psb[pi][:], lgT[:, t * 128:(t + 1) * 128], wupa[:], True, True, [r_lgT, r_wupa], [r_ps[pi]])
                              ACT(spt[:, t, :], psb[pi][:], AF.Exp, [r_ps[pi]], [r_spt[t]], scale=-1.0)
                              ACT(spt[:, t, :], spt[:, t, :], AF.Ln, [r_spt[t]], [r_spt[t]], bias=1.0)
                          for h in range(GH):
                              pi = nbig()
                              for t in range(4):
                                  MM(psb[pi][:, t * 128:(t + 1) * 128], spt[:, t, h * 128:(h + 1) * 128], uu[:], True, True,
                                     [r_spt[t], r_uu], [r_ps[pi]])
                              ACT(eq[:, h, :], psb[pi][:], AF.Exp, [r_ps[pi]], [r_eq[h]])
                              ACT(ek[:, h, :], psb[pi][:], AF.Exp, [r_ps[pi]], [r_ek[h]], scale=-1.0)
                          wv, rw = wload((l, "qg"), "c8")
                          for h in range(GH):
                              pi = nbig()
                              for c in range(8):
                                  MM(psb[pi][:], wv[:, c, h * 128:(h + 1) * 128], xT[:, c, :], c == 0, c == 7, [r_xT, rw], [r_ps[pi]])
                              V("dve", "scalar_tensor_tensor", [r_ps[pi], r_eq[h]], [r_qdT[h]], out=qdT[:, h, :], in0=psb[pi][:],
                                scalar=GK ** -0.5, in1=eq[:, h, :], op0=ALU.mult, op1=ALU.mult)
                          wv, rw = wload((l, "kg"), "c8")
                          for h in range(GH):
                              pi = nbig()
                              for c in range(8):
                                  MM(psb[pi][:], wv[:, c, h * 128:(h + 1) * 128], xT[:, c, :], c == 0, c == 7, [r_xT, rw], [r_ps[pi]])
                              V("dve", "tensor_tensor", [r_ps[pi], r_ek[h]], [r_kiT[h]], out=kiT[:, h, :], in0=psb[pi][:],
                                in1=ek[:, h, :], op=ALU.mult)

                          def cons_vg(t, ps, rps):
                              ACT(vg[:, t, :], ps[:], AF.Copy, [rps], [r_vg[t]])
                          proj_tm("vg", cons_vg)

                          def cons_rg(t, ps, rps):
                              ACT(rgs[:, t, :], ps[:], AF.Silu, [rps], [r_rgs[t]])
                          proj_tm("rg", cons_rg)

                          dec4s = {}

                          def gla_stage1(t):
                              j = t % 2
                              cs = slice(t * 128, (t + 1) * 128)
                              for h in range(GH):
                                  TR(ptr4[:, h, :], kiT[:, h, cs], identb[:], [r_kiT[h], r_ident], [r_ps[PTR]])
                              V("dve", "tensor_copy", [r_ps[PTR]], [r_kiM[j]], out=kiM[:, j, :, :], in_=ptr4[:, 0:4, :])
                              pa = nbig()
                              for h in range(GH):
                                  MM(psb[pa][:, h * 128:(h + 1) * 128], kiT[:, h, cs], qdT[:, h, cs], True, True, [r_kiT[h], r_qdT[h]], [r_ps[pa]])
                              V("dve", "tensor_tensor", [r_ps[pa], r_tri], [r_attm[j]], out=attm[:, j, :, :],
                                in0=psb[pa][:].rearrange("p (h n) -> p h n", h=GH), in1=tri[:].unsqueeze(1).broadcast_to([128, GH, 128]), op=ALU.mult)

                          def gla_stage2(t):
                              j = t % 2
                              cs = slice(t * 128, (t + 1) * 128)
                              PO = PMISC if t % 2 == 0 else 3
                              po = psb[PO]
                              for h in range(GH):
                                  MM(po[:, h * 128:(h + 1) * 128], attm[:, j, h, :], vg[:, t, h * 128:(h + 1) * 128], True, False,
                                     [r_attm[j], r_vg[t]], [r_ps[PO]])
                                  MM(po[:, h * 128:(h + 1) * 128], qdT[:, h, cs], stb[:, h, :], False, True,
                                     [r_qdT[h], r_stb[0]], [r_ps[PO]])
                              pd = nbig()
                              for h in range(GH):
                                  MM(psb[pd][:, h * 128:(h + 1) * 128], kiM[:, j, h, :], vg[:, t, h * 128:(h + 1) * 128], True, True,
                                     [r_kiM[j], r_vg[t]], [r_ps[pd]])
                              V("dve", "tensor_tensor", [r_ps[pd], r_stf[0]], [r_stmp[j]], out=stmp[:, j, :, :],
                                in0=psb[pd][:].rearrange("p (h n) -> p h n", h=GH), in1=stf[:], op=ALU.add)
                              dec4 = eq[:, :, t * 128 + 127:t * 128 + 128].broadcast_to([128, GH, 128])
                              V("dve", "tensor_tensor", [r_stmp[j]] + r_eq, [r_stf[0]], out=stf[:], in0=stmp[:, j, :, :], in1=dec4, op=ALU.mult)
                              V("dve", "tensor_tensor", [r_stmp[j]] + r_eq, [r_stb[0]], out=stb[:], in0=stmp[:, j, :, :], in1=dec4, op=ALU.mult)
                              return po, PO

                          gla_stage1(0)
                          for t in range(4):
                              if t + 1 < 4:
                                  gla_stage1(t + 1)
                              po, PO = gla_stage2(t)
                              for h in range(GH):
                                  V("dve", "bn_stats", [r_ps[PO]], [r_hn], out=hn[:, h * 6:(h + 1) * 6], in_=po[:, h * 128:(h + 1) * 128])
                                  V("dve", "bn_aggr", [r_hn], [r_hn], out=hn[:, 24 + 2 * h:26 + 2 * h], in_=hn[:, h * 6:(h + 1) * 6])
                              mv = hn[:, 24:32].rearrange("p (h k) -> p h k", h=GH)
                              V("dve", "tensor_scalar_add", [r_hn], [r_hn], out=hn[:, 32:36], in0=mv[:, :, 1], scalar1=LN_EPS)
                              ACT(hn[:, 32:36], hn[:, 32:36], AF.Sqrt, [r_hn], [r_hn])
                              V("dve", "reciprocal", [r_hn], [r_hn], out=hn[:, 32:36], in_=hn[:, 32:36])
                              for h in range(GH):
                                  V("dve", "tensor_scalar", [r_ps[PO], r_hn], [r_ob], out=ob[:, h * 128:(h + 1) * 128],
                                    in0=po[:, h * 128:(h + 1) * 128], scalar1=hn[:, 24 + 2 * h:25 + 2 * h], scalar2=hn[:, 32 + h:33 + h],
                                    op0=ALU.subtract, op1=ALU.mult)
                              V("pool", "tensor_tensor", [r_ob, r_gnt], [r_ob], out=ob[:], in0=ob[:], in1=gnt[:], op=ALU.mult)
                              V("pool", "tensor_tensor", [r_ob, r_rgs[t]], [r_yb], out=yb[:], in0=ob[:], in1=rgs[:, t, :], op=ALU.mult)
                              for kc in range(4):
                                  TR(ptr4[:, kc, :], yb[:, kc * 128:(kc + 1) * 128], identb[:], [r_yb, r_ident], [r_ps[PTR]])
                              V("dve", "tensor_copy", [r_ps[PTR]], [r_ybT], out=ybT[:, :, t * 128:(t + 1) * 128], in_=ptr4[:, 0:4, :])

                          wa_v, rwa = wload((l, "wa"), "c4")
                          wb_v, rwb = wload((l, "wb"), "c4")
                          for half in range(2):
                              ga_v, rga = wload((l, f"ga{half}"), "c8")
                              gb_v, rgb = wload((l, f"gb{half}"), "c8")
                              for mm_ in range(4):
                                  m = half * 4 + mm_
                                  cs = slice(mm_ * 128, (mm_ + 1) * 128)
                                  ms = slice(m * 128, (m + 1) * 128)
                                  pi = nbig()
                                  for c in range(8):
                                      MM(psb[pi][:], ga_v[:, c, cs], xT[:, c, :], c == 0, c == 7, [r_xT, rga], [r_ps[pi]])
                                  ACT(sg[:, 0, :], psb[pi][:], AF.Sigmoid, [r_ps[pi]], [r_sg[0]])
                                  pi = nbig()
                                  for c in range(4):
                                      MM(psb[pi][:], wa_v[:, c, ms], yaT[:, c, :], c == 0, c == 3, [r_yaT, rwa], [r_ps[pi]])
                                  V("dve", "tensor_tensor", [r_ps[pi], r_sg[0]], [r_sg[0]], out=sg[:, 0, :], in0=psb[pi][:], in1=sg[:, 0, :], op=ALU.mult)
                                  pi = nbig()
                                  for c in range(8):
                                      MM(psb[pi][:], gb_v[:, c, cs], xT[:, c, :], c == 0, c == 7, [r_xT, rgb], [r_ps[pi]])
                                  ACT(sg[:, 1, :], psb[pi][:], AF.Sigmoid, [r_ps[pi]], [r_sg[1]])
                                  pi = nbig()
                                  for c in range(4):
                                      MM(psb[pi][:], wb_v[:, c, ms], ybT[:, c, :], c == 0, c == 3, [r_ybT, rwb], [r_ps[pi]])
                                  V("dve", "tensor_tensor", [r_ps[pi], r_sg[1]], [r_sg[1]], out=sg[:, 1, :], in0=psb[pi][:], in1=sg[:, 1, :], op=ALU.mult)
                                  V("pool", "tensor_tensor", [r_sg[0], r_sg[1]], [r_mT], out=mT[:, m, :], in0=sg[:, 0, :], in1=sg[:, 1, :], op=ALU.add)

                          if cfg.dbg:
                              final_tokens.append(S.dma("pool", dbg_t["ybT"][gi], ybT[:], reads=[r_ybT], writes=[r_dbg], sres=r_dbg))
                              final_tokens.append(S.dma("pool", dbg_t["mT"][gi], mT[:], reads=[r_mT], writes=[r_dbg], sres=r_dbg))
                          wo_v0, rwo0 = wload((l, "wo0"), "c8")
                          wo_v1, rwo1 = wload((l, "wo1"), "c8")
                          for t in range(4):
                              i = ring["ln"] % 2
                              ring["ln"] += 1
                              r0 = tok0 + t * 128
                              S.dma("pool", tin[i][:], x_src[r0:r0 + 128, :], reads=[r_xsrc[gi]], writes=[r_tin[i]], sres=r_tin[i])
                              for half, (wv_, rw_) in enumerate(((wo_v0, rwo0), (wo_v1, rwo1))):
                                  pi = nbig()
                                  for c in range(8):
                                      MM(psb[pi][:], mT[:, c, t * 128:(t + 1) * 128], wv_[:, c, :], c == 0, c == 7, [r_mT, rw_], [r_ps[pi]])
                                  hs = slice(half * 512, (half + 1) * 512)
                                  V("dve", "scalar_tensor_tensor", [r_ps[pi], r_tin[i]], [r_tin[i]], out=tin[i][:, hs], in0=tin[i][:, hs],
                                    scalar=ALPHA, in1=psb[pi][:], op0=ALU.mult, op1=ALU.add)
                              layer_norm_tile((lst, r_lst), tin[i], r_tin[i], lnm_g, lnm_b, [r_lng, r_lnb], x1o[i][:], r_x1o[i], eng="dve")
                              if router:
                                  for e in range(NE):
                                      V("dve", "scalar_tensor_tensor", [r_x1o[i], r_wrB], [r_tin[i], r_lgR], out=tin[i][:], in0=x1o[i][:], scalar=1.0,
                                        in1=wrB[:, e, :], op0=ALU.mult, op1=ALU.mult, accum_out=lgR[:, r0 // 128, e:e + 1])
                              S.dma("pool", x1s[r0:r0 + 128, :], x1o[i][:], reads=[r_x1o[i]], writes=[r_x1s[gi]], sres=r_x1o[i], load=False)
            S.barrier()

        def phase_ffn(l, dst, r_dst, is_final):
            moe = (l % 2 == 1)
            with ExitStack() as st:
                set_ring(st, 6)
                lnf_g = sb(st, "lnf_g", [128, D], F32)
                lnf_b = sb(st, "lnf_b", [128, D], F32)
                r_lnf = S.res("lnf")
                S.dma("pool", lnf_g[:], ln_fg[l].partition_broadcast(128), writes=[r_lnf], sres=r_lnf)
                S.dma("pool", lnf_b[:], ln_fb[l].partition_broadcast(128), writes=[r_lnf], sres=r_lnf)
                xf = [sb(st, f"fxf{i}", [128, D], F32) for i in range(2)]
                r_xf = [S.res(f"fxf{i}") for i in range(2)]
                xb = [sb(st, f"fxb{i}", [128, D], BF16) for i in range(2)]
                r_xb = [S.res(f"fxb{i}") for i in range(2)]
                xT = sb(st, "fxT", [128, 8, G], BF16)
                r_xT = S.res("fxT")
                hT = sb(st, "hT", [128, 28, G], BF16)
                r_hT = [S.res(f"hT{i}") for i in range(7)]
                sgt = [sb(st, f"sgt{i}", [128, G], BF16) for i in range(2)]
                r_sgt = [S.res(f"sgt{i}") for i in range(2)]
                tin = [sb(st, f"ftin{i}", [128, D], F32) for i in range(4)]
                r_tin = [S.res(f"ftin{i}") for i in range(4)]
                yo = [sb(st, f"fyo{i}", [128, D], F32) for i in range(4)]
                r_yo = [S.res(f"fyo{i}") for i in range(4)]
                lst = sb(st, "flst", [128, 16], F32)
                r_lst = S.res("flst")
                if moe:
                    wr = sb(st, "wr", [128, NE, D], F32)
                    r_wr = S.res("wr")
                    for e in range(NE):
                        S.dma("pool", wr[:, e, :], m_rT[e].partition_broadcast(128), writes=[r_wr], sres=r_wr)
                    acc = sb(st, "macc", [128, 4, D], F32)
                    r_acc = [S.res(f"macc{t}") for t in range(4)]
                    lg = sb(st, "mlg", [128, 4, 32], F32)
                    r_lg = [S.res(f"mlg{t}") for t in range(4)]
                    junk = sb(st, "mjunk", [128, D], F32)
                    r_junk = S.res("mjunk")
                ring = {"big": 0, "x": 0, "sg": 0, "ln": 0}
                pbig = [0, 1, 2, 3]
                pdn = [4, 5]
                PTR = 6

                def nbig():
                    i = pbig[ring["big"] % len(pbig)]
                    ring["big"] += 1
                    return i

                for gi in range(T // G):
                    tok0 = gi * G
                    for t in range(4):
                        i = ring["x"] % 2
                        ring["x"] += 1
                        r0 = tok0 + t * 128
                        S.dma("pool", xf[i][:], x1s[r0:r0 + 128, :], reads=[r_x1s[gi]], writes=[r_xf[i]], sres=r_xf[i])
                        ACT(xb[i][:], xf[i][:], AF.Copy, [r_xf[i]], [r_xb[i]])
                        ptr = psb[PTR][:].bitcast(BF16).rearrange("p (c n) -> p c n", c=8)
                        for c in range(8):
                            TR(ptr[:, c, :], xb[i][:, c * 128:(c + 1) * 128], identb[:], [r_xb[i], r_ident], [r_ps[PTR]])
                        V("dve", "tensor_copy", [r_ps[PTR]], [r_xT], out=xT[:, :, t * 128:(t + 1) * 128], in_=ptr)
                        if moe:
                            for e in range(NE):
                                V("dve", "tensor_tensor", [r_xf[i], r_wr], [r_junk], out=junk[:], in0=xf[i][:],
                                  in1=wr[:, e, :], op=ALU.mult)
                                V("dve", "tensor_reduce", [r_junk], [r_lg[t]], out=lg[:, t, e:e + 1], in_=junk[:], axis=AX.X, op=ALU.add)
                            L = lg[:, t, 0:8]
                            m1 = lg[:, t, 8:9]
                            m2 = lg[:, t, 9:10]
                            E1 = lg[:, t, 10:18]
                            L2 = lg[:, t, 18:26]
                            V("dve", "tensor_reduce", [r_lg[t]], [r_lg[t]], out=m1, in_=L, axis=AX.X, op=ALU.max)
                            V("dve", "tensor_scalar", [r_lg[t]], [r_lg[t]], out=E1, in0=L, scalar1=m1, scalar2=None, op0=ALU.is_ge)
                            V("dve", "scalar_tensor_tensor", [r_lg[t]], [r_lg[t]], out=L2, in0=E1, scalar=-BIG, in1=L, op0=ALU.mult, op1=ALU.add)
                            V("dve", "tensor_reduce", [r_lg[t]], [r_lg[t]], out=m2, in_=L2, axis=AX.X, op=ALU.max)
                            V("dve", "tensor_scalar", [r_lg[t]], [r_lg[t]], out=E1, in0=L, scalar1=m2, scalar2=None, op0=ALU.is_ge)
                            V("dve", "tensor_scalar", [r_lg[t]], [r_lg[t]], out=L2, in0=L, scalar1=m1, scalar2=None, op0=ALU.subtract)
                            ACT(L2, L2, AF.Exp, [r_lg[t]], [r_lg[t]])
                            V("dve", "tensor_tensor", [r_lg[t]], [r_lg[t]], out=L2, in0=L2, in1=E1, op=ALU.mult)
                            V("dve", "tensor_reduce", [r_lg[t]], [r_lg[t]], out=lg[:, t, 26:27], in_=L2, axis=AX.X, op=ALU.add)
                            V("dve", "reciprocal", [r_lg[t]], [r_lg[t]], out=lg[:, t, 26:27], in_=lg[:, t, 26:27])
                            V("dve", "tensor_scalar", [r_lg[t]], [r_lg[t]], out=E1, in0=L2, scalar1=lg[:, t, 26:27], scalar2=None, op0=ALU.mult)

                    for e in range(NE if moe else 1):
                        kg = (lambda i: ("m", e, "g", i)) if moe else (lambda i: ("f", "g", i))
                        ku = (lambda i: ("m", e, "u", i)) if moe else (lambda i: ("f", "u", i))
                        kd = (lambda i: ("m", e, "d", i)) if moe else (lambda i: ("f", "d", i))
                        for bi in range(7):
                            wg_v, rwg = wload(kg(bi), "c8")
                            wu_v, rwu = wload(ku(bi), "c8")
                            for j in range(4):
                                fc = bi * 4 + j
                                js = slice(j * 128, (j + 1) * 128)
                                pg = nbig()
                                for c in range(8):
                                    MM(psb[pg][:], wg_v[:, c, js], xT[:, c, :], c == 0, c == 7, [r_xT, rwg], [r_ps[pg]])
                                k = ring["sg"] % 2
                                ring["sg"] += 1
                                ACT(sgt[k][:], psb[pg][:], AF.Silu, [r_ps[pg]], [r_sgt[k]])
                                pu = nbig()
                                for c in range(8):
                                    MM(psb[pu][:], wu_v[:, c, js], xT[:, c, :], c == 0, c == 7, [r_xT, rwu], [r_ps[pu]])
                                V("dve", "tensor_tensor", [r_ps[pu], r_sgt[k]], [r_hT[bi]], out=hT[:, fc, :], in0=psb[pu][:], in1=sgt[k][:], op=ALU.mult)
                        dblocks = []
                        for half in range(2):
                            accp = [nbig() for _ in range(4)]
                            for bi in range(7):
                                wd_v, rwd = wload(kd(bi), "c4")
                                for t in range(4):
                                    for j in range(4):
                                        fc = bi * 4 + j
                                        MM(psb[accp[t]][:], hT[:, fc, t * 128:(t + 1) * 128], wd_v[:, j, half * 512:(half + 1) * 512],
                                           fc == 0, fc == 27, [r_hT[bi], rwd], [r_ps[accp[t]]])
                            hs = slice(half * 512, (half + 1) * 512)
                            for t in range(4):
                                if moe:
                                    cw = lg[:, t, 10 + e:11 + e]
                                    if e == 0:
                                        V("dve", "tensor_scalar", [r_ps[accp[t]], r_lg[t]], [r_acc[t]], out=acc[:, t, hs], in0=psb[accp[t]][:],
                                          scalar1=cw, scalar2=None, op0=ALU.mult)
                                    else:
                                        V("dve", "scalar_tensor_tensor", [r_ps[accp[t]], r_lg[t], r_acc[t]], [r_acc[t]], out=acc[:, t, hs],
                                          in0=psb[accp[t]][:], scalar=cw, in1=acc[:, t, hs], op0=ALU.mult, op1=ALU.add)
                                else:
                                    if half == 0:
                                        i = t
                                        r0 = tok0 + t * 128
                                        S.dma("pool", tin[i][:], x1s[r0:r0 + 128, :], reads=[r_x1s[gi]], writes=[r_tin[i]], sres=r_tin[i])
                                        dblocks.append(i)
                                    i = dblocks[t]
                                    V("dve", "scalar_tensor_tensor", [r_ps[accp[t]], r_tin[i]], [r_tin[i]], out=tin[i][:, hs], in0=tin[i][:, hs],
                                      scalar=ALPHA, in1=psb[accp[t]][:], op0=ALU.mult, op1=ALU.add)
                                    if half == 1:
                                        r0 = tok0 + t * 128
                                        layer_norm_tile((lst, r_lst), tin[i], r_tin[i], lnf_g, lnf_b, [r_lnf], yo[i][:], r_yo[i], eng="pool")
                                        o = S.dma("pool", dst[r0:r0 + 128, :], yo[i][:], reads=[r_yo[i]], writes=[r_dst[gi]], sres=r_yo[i], load=False)
                                        if is_final:
                                            final_tokens.append(o)
                    if moe:
                        for t in range(4):
                            i = t
                            r0 = tok0 + t * 128
                            S.dma("pool", tin[i][:], x1s[r0:r0 + 128, :], reads=[r_x1s[gi]], writes=[r_tin[i]], sres=r_tin[i])
                            V("dve", "scalar_tensor_tensor", [r_acc[t], r_tin[i]], [r_tin[i]], out=tin[i][:], in0=tin[i][:],
                              scalar=ALPHA, in1=acc[:, t, :], op0=ALU.mult, op1=ALU.add)
                            layer_norm_tile((lst, r_lst), tin[i], r_tin[i], lnf_g, lnf_b, [r_lnf], yo[i][:], r_yo[i], eng="pool")
                            o = S.dma("pool", dst[r0:r0 + 128, :], yo[i][:], reads=[r_yo[i]], writes=[r_dst[gi]], sres=r_yo[i], load=False)
                            if is_final:
                                final_tokens.append(o)
            S.barrier()

        def phase_moe(l, dst, r_dst, is_final):
            IOA = bass.IndirectOffsetOnAxis
            NT = T // 128
            wsc_rows = wsm.rearrange("b p n -> (b p) n")
            moe_blk0 = [r_wsc[tab[("m", e, "g", 0)]] for e in range(NE)]
            r_s2t = S.res("s2t")
            r_s2t_parts = []
            r_yg = [S.res(f"yg{j}") for j in range(NGRP)]
            PTR, PMISC = 6, 7
            with ExitStack() as so:
                set_ring(so, 6)
                lg = sb(so, "mlg", [128, NT, 32], F32)
                r_lg = S.res("mlg")
                selm = sb(so, "selm", [128, NT, NE], F32)
                hot1 = sb(so, "hot1", [128, NT, NE], F32)
                hot2 = sb(so, "hot2", [128, NT, NE], F32)
                rankg = sb(so, "rankg", [128, NT, NE], F32)
                r_rt = S.res("route")
                cwk = sb(so, "cwk", [128, NT, 2], F32)
                r_cwk = S.res("cwk")
                run = sb(so, "runc", [128, 48], F32)
                r_run = S.res("runc")
                widx = sb(so, "widx", [128, NGRP, 21], I32)
                r_widx = S.res("widx")
                sl0i = sb(so, "sl0i", [128, NT], I32)
                sl1i = sb(so, "sl1i", [128, NT], I32)
                r_sli = S.res("sli")
                tokc = sb(so, "tokc", [128, NT], I32)
                r_tokc = S.res("tokc")
                S.dma("pool", tokc[:], c_tok, writes=[r_tokc], sres=r_tokc)

                with ExitStack() as st:
                    if not cfg.do_mix:
                        wr = sb(st, "wr", [128, NE, D], F32)
                        r_wr = S.res("wr")
                        for e in range(NE):
                            S.dma("pool", wr[:, e, :], m_rT[e].partition_broadcast(128), writes=[r_wr], sres=r_wr)
                        xf = [sb(st, f"rxf{i}", [128, D], F32) for i in range(2)]
                        r_xf = [S.res(f"rxf{i}") for i in range(2)]
                        junk = sb(st, "rjunk", [128, D], F32)
                        r_junk = S.res("rjunk")
                    tris, r_tris = sb(st, "tris", [128, 128], F32), S.res("tris")
                    S.dma("pool", tris[:], c_tris, writes=[r_tris], sres=r_tris)
                    onesf, r_onesf = sb(st, "onesf", [128, 128], F32), S.res("onesf")
                    V("pool", "memset", [], [r_onesf], ap=onesf[:], constant=1.0)
                    thr, r_thr = sb(st, "thr", [128, 16], F32), S.res("thr")
                    S.dma("pool", thr[:], c_thr, writes=[r_thr], sres=r_thr)
                    jj, r_jj = sb(st, "jj", [128, NGRP], F32), S.res("jj")
                    S.dma("pool", jj[:], c_jj, writes=[r_jj], sres=r_jj)
                    wcst, r_wcst = sb(st, "wcst", [128, 21], F32), S.res("wcst")
                    S.dma("pool", wcst[:], c_wcst, writes=[r_wcst], sres=r_wcst)
                    cmp3 = sb(st, "cmp3", [128, NE, 16], F32)
                    ej = sb(st, "ej", [128, NGRP], F32)
                    widxf = sb(st, "widxf", [128, NGRP, 21], F32)
                    slotf = sb(st, "slotf", [128, NT, NE], F32)
                    stmp = sb(st, "rstmp", [128, NT, NE], F32)
                    s01f = sb(st, "s01f", [128, 2, NT], F32)
                    zi = sb(st, "zi", [128, NSLOT // 128], I32)
                    r_zi = S.res("zi")
                    V("pool", "memset", [], [r_zi], ap=zi[:], constant=0)
                    S.dma("pool", s2t.rearrange("(p n) o -> p (n o)", p=128), zi[:], reads=[r_zi], writes=[r_s2t], sres=r_s2t)
                    r_misc = S.res("rmisc")
                    V("dve", "memset", [], [r_run], ap=run[:], constant=0.0)

                    for i in range(NT):
                        b = i % 2
                        r0 = i * 128
                        if cfg.do_mix:
                            V("dve", "tensor_copy", [r_lgR], [r_lg], out=lg[:, i, 0:8], in_=lgR[:, i, :])
                        else:
                            S.dma("pool", xf[b][:], x1s[r0:r0 + 128, :], reads=[r_x1s[r0 // G]], writes=[r_xf[b]], sres=r_xf[b])
                            for e in range(NE):
                                V("dve", "scalar_tensor_tensor", [r_xf[b], r_wr], [r_junk, r_lg], out=junk[:], in0=xf[b][:], scalar=1.0, in1=wr[:, e, :],
                                  op0=ALU.mult, op1=ALU.mult, accum_out=lg[:, i, e:e + 1])
                        L = lg[:, i, 0:8]
                        m1 = lg[:, i, 8:9]
                        m2 = lg[:, i, 9:10]
                        L2 = lg[:, i, 18:26]
                        dd = lg[:, i, 26:27]
                        H1 = hot1[:, i, :]
                        H2 = hot2[:, i, :]
                        V("dve", "tensor_reduce", [r_lg], [r_lg], out=m1, in_=L, axis=AX.X, op=ALU.max)
                        V("dve", "tensor_scalar", [r_lg], [r_rt], out=H1, in0=L, scalar1=m1, scalar2=None, op0=ALU.is_ge)
                        V("dve", "scalar_tensor_tensor", [r_lg, r_rt], [r_lg], out=L2, in0=H1, scalar=-BIG, in1=L, op0=ALU.mult, op1=ALU.add)
                        V("dve", "tensor_reduce", [r_lg], [r_lg], out=m2, in_=L2, axis=AX.X, op=ALU.max)
                        V("dve", "tensor_scalar", [r_lg], [r_rt], out=H2, in0=L2, scalar1=m2, scalar2=None, op0=ALU.is_ge)
                        V("dve", "tensor_tensor", [r_rt], [r_rt], out=selm[:, i, :], in0=H1, in1=H2, op=ALU.add)
                        V("dve", "tensor_tensor", [r_lg], [r_lg], out=dd, in0=m2, in1=m1, op=ALU.subtract)
                        ACT(dd, dd, AF.Exp, [r_lg], [r_lg])
                        V("dve", "tensor_scalar_add", [r_lg], [r_cwk], out=cwk[:, i, 0:1], in0=dd, scalar1=1.0)
                        V("dve", "reciprocal", [r_cwk], [r_cwk], out=cwk[:, i, 0:1], in_=cwk[:, i, 0:1])
                        V("dve", "tensor_tensor", [r_cwk, r_lg], [r_cwk], out=cwk[:, i, 1:2], in0=cwk[:, i, 0:1], in1=dd, op=ALU.mult)
                        pr = psb[PMISC][:, 0:16]
                        MM(pr[:, 0:8], tris[:], selm[:, i, :], True, True, [r_tris, r_rt], [r_ps[PMISC]])
                        MM(pr[:, 8:16], onesf[:], selm[:, i, :], True, True, [r_onesf, r_rt], [r_ps[PMISC]])
                        V("dve", "tensor_tensor", [r_ps[PMISC], r_run], [r_rt], out=rankg[:, i, :], in0=pr[:, 0:8], in1=run[:, 0:8], op=ALU.add)
                        V("dve", "tensor_tensor", [r_ps[PMISC], r_run], [r_run], out=run[:, 0:8], in0=pr[:, 8:16], in1=run[:, 0:8], op=ALU.add)

                    cnt, ng, gend, base = run[:, 0:8], run[:, 8:16], run[:, 16:24], run[:, 24:32]
                    V("dve", "tensor_tensor", [r_run, r_thr], [r_misc], out=cmp3[:], in0=cnt.unsqueeze(2).broadcast_to([128, NE, 16]),
                      in1=thr[:].unsqueeze(1).broadcast_to([128, NE, 16]), op=ALU.is_gt)
                    V("dve", "tensor_reduce", [r_misc], [r_run], out=ng, in_=cmp3[:], axis=AX.X, op=ALU.add)
                    V("dve", "tensor_copy", [r_run], [r_run], out=gend[:, 0:1], in_=ng[:, 0:1])
                    for e in range(1, NE):
                        V("dve", "tensor_tensor", [r_run], [r_run], out=gend[:, e:e + 1], in0=gend[:, e - 1:e], in1=ng[:, e:e + 1], op=ALU.add)
                    V("dve", "tensor_tensor", [r_run], [r_run], out=base, in0=gend, in1=ng, op=ALU.subtract)
                    V("dve", "tensor_scalar_mul", [r_run], [r_run], out=base, in0=base, scalar1=512.0)
                    V("dve", "tensor_scalar", [r_jj, r_run], [r_misc], out=ej[:], in0=jj[:], scalar1=gend[:, 0:1], scalar2=None, op0=ALU.is_ge)
                    for e in range(1, NE):
                        V("dve", "scalar_tensor_tensor", [r_jj, r_run, r_misc], [r_misc], out=ej[:], in0=jj[:], scalar=gend[:, e:e + 1], in1=ej[:],
                          op0=ALU.is_ge, op1=ALU.add)
                    V("dve", "tensor_scalar_min", [r_misc], [r_misc], out=ej[:], in0=ej[:], scalar1=float(NE - 1))
                    V("dve", "scalar_tensor_tensor", [r_misc, r_wcst], [r_misc], out=widxf[:], in0=ej[:].unsqueeze(2).broadcast_to([128, NGRP, 21]),
                      scalar=float(21 * 128), in1=wcst[:].unsqueeze(1).broadcast_to([128, NGRP, 21]), op0=ALU.mult, op1=ALU.add)
                    V("dve", "tensor_copy", [r_misc], [r_widx], out=widx[:], in_=widxf[:])
                    V("dve", "tensor_tensor", [r_rt, r_run], [r_misc], out=slotf[:], in0=rankg[:], in1=base.unsqueeze(1).broadcast_to([128, NT, NE]), op=ALU.add)
                    for k, hot in enumerate((hot1, hot2)):
                        V("dve", "tensor_tensor", [r_misc, r_rt], [r_misc], out=stmp[:], in0=slotf[:], in1=hot[:], op=ALU.mult)
                        V("dve", "tensor_reduce", [r_misc], [r_misc], out=s01f[:, k, :], in_=stmp[:], axis=AX.X, op=ALU.add)
                    V("dve", "tensor_copy", [r_misc], [r_sli], out=sl0i[:], in_=s01f[:, 0, :])
                    V("dve", "tensor_copy", [r_misc], [r_sli], out=sl1i[:], in_=s01f[:, 1, :])
                    sc_st = S.new_stream("scat")
                    sc_st.q = "pool"
                    sc_ops = []
                    for i in range(NT):
                        for sli in (sl0i, sl1i):
                            rr = S.res("s2tw")
                            r_s2t_parts.append(rr)
                            sc_ops.append(S.dma("pool", None, None, reads=[r_sli, r_tokc, r_s2t], writes=[rr], sres=rr, stream=sc_st,
                                                fn=(lambda e, sli=sli, i=i: e.indirect_dma_start(out=s2t[:, :], out_offset=IOA(ap=sli[:, i:i + 1], axis=0),
                                                                                                  in_=tokc[:, i:i + 1], in_offset=None))))
                    for o_ in sc_ops:
                        o_.dval = sc_st.count * 16
                S.barrier()
                with ExitStack() as st:
                    tix = [sb(st, f"tix{i}", [128, 4], I32) for i in range(2)]
                    r_tix = [S.res(f"tix{i}") for i in range(2)]
                    xg = [[sb(st, f"xg{i}_{q}", [128, D], BF16) for q in range(4)] for i in range(2)]
                    r_xg = [[S.res(f"xg{i}_{q}") for q in range(4)] for i in range(2)]
                    xT = sb(st, "exT", [128, 8, G], BF16)
                    r_xT = S.res("exT")
                    hT = sb(st, "ehT", [128, 28, G], BF16)
                    r_hT = [S.res(f"ehT{i}") for i in range(7)]
                    sgt = [sb(st, f"esgt{i}", [128, G], BF16) for i in range(2)]
                    r_sgt = [S.res(f"esgt{i}") for i in range(2)]
                    yo = [sb(st, f"eyo{i}", [128, D], F32) for i in range(4)]
                    r_yo = [S.res(f"eyo{i}") for i in range(4)]
                    ring = {"big": 0, "sg": 0}
                    pbig = [0, 1, 2, 3]

                    def nbig():
                        i = pbig[ring["big"] % len(pbig)]
                        ring["big"] += 1
                        return i

                    def wload_dyn(j, k, view):
                        i = wstate["i"] % len(wring)
                        wstate["i"] += 1
                        S.dma("pool", None, None, reads=[r_widx] + moe_blk0, writes=[r_wring[i]], sres=r_wring[i],
                              fn=(lambda e, i=i, j=j, k=k: e.indirect_dma_start(out=wring[i][:], out_offset=None, in_=wsc_rows,
                                                                               in_offset=IOA(ap=widx[:, j, k:k + 1], axis=0))))
                        t = wring[i]
                        v = t[:].rearrange("p (c n) -> p c n", c=8 if view == "c8" else 4)
                        return v, r_wring[i]

                    def gather_group(j):
                        jb = j % 2
                        for q in range(4):
                            s0 = j * 512 + q * 128
                            S.dma("sp", tix[jb][:, q:q + 1], s2t[s0:s0 + 128, :], reads=[r_s2t] + r_s2t_parts, writes=[r_tix[jb]], sres=r_tix[jb])
                        for q in range(4):
                            S.dma("pool", None, None, reads=[r_tix[jb]] + r_x1s, writes=[r_xg[jb][q]], sres=r_xg[jb][q],
                                  fn=(lambda e, jb=jb, q=q: e.indirect_dma_start(out=xg[jb][q][:], out_offset=None, in_=x1s[:, :],
                                                                                 in_offset=IOA(ap=tix[jb][:, q:q + 1], axis=0))))

                    gather_group(0)
                    for j in range(NGRP):
                        jb = j % 2
                        if j + 1 < NGRP:
                            gather_group(j + 1)
                        for q in range(4):
                            PB = PTR if q % 2 == 0 else PMISC
                            ptr = psb[PB][:].bitcast(BF16).rearrange("p (c n) -> p c n", c=8)
                            for c in range(8):
                                TR(ptr[:, c, :], xg[jb][q][:, c * 128:(c + 1) * 128], identb[:], [r_xg[jb][q], r_ident], [r_ps[PB]])
                            V("dve", "tensor_copy", [r_ps[PB]], [r_xT], out=xT[:, :, q * 128:(q + 1) * 128], in_=ptr)
                        for bi in range(7):
                            wg_v, rwg = wload_dyn(j, bi, "c8")
                            wu_v, rwu = wload_dyn(j, 7 + bi, "c8")
                            for jn in range(4):
                                fc = bi * 4 + jn
                                js = slice(jn * 128, (jn + 1) * 128)
                                pg = nbig()
                                for c in range(8):
                                    MM(psb[pg][:], wg_v[:, c, js], xT[:, c, :], c == 0, c == 7, [r_xT, rwg], [r_ps[pg]])
                                k = ring["sg"] % 2
                                ring["sg"] += 1
                                ACT(sgt[k][:], psb[pg][:], AF.Silu, [r_ps[pg]], [r_sgt[k]])
                                pu = nbig()
                                for c in range(8):
                                    MM(psb[pu][:], wu_v[:, c, js], xT[:, c, :], c == 0, c == 7, [r_xT, rwu], [r_ps[pu]])
                                V("dve", "tensor_tensor", [r_ps[pu], r_sgt[k]], [r_hT[bi]], out=hT[:, fc, :], in0=psb[pu][:], in1=sgt[k][:], op=ALU.mult)
                        for half in range(2):
                            accp = [nbig() for _ in range(4)]
                            for bi in range(7):
                                wd_v, rwd = wload_dyn(j, 14 + bi, "c4")
                                for t in range(4):
                                    for jn in range(4):
                                        fc = bi * 4 + jn
                                        MM(psb[accp[t]][:], hT[:, fc, t * 128:(t + 1) * 128], wd_v[:, jn, half * 512:(half + 1) * 512],
                                           fc == 0, fc == 27, [r_hT[bi], rwd], [r_ps[accp[t]]])
                            hs = slice(half * 512, (half + 1) * 512)
                            for t in range(4):
                                ACT(yo[t][:, hs], psb[accp[t]][:], AF.Copy, [r_ps[accp[t]]], [r_yo[t]])
                                if half == 1:
                                    s0 = j * 512 + t * 128
                                    S.dma("sp", ygs[s0:s0 + 128, :], yo[t][:], reads=[r_yo[t]], writes=[r_yg[j]], sres=r_yo[t], load=False)
                S.barrier()
                with ExitStack() as st:
                    lnf_g = sb(st, "elnf_g", [128, D], F32)
                    lnf_b = sb(st, "elnf_b", [128, D], F32)
                    r_lnf = S.res("elnf")
                    S.dma("pool", lnf_g[:], ln_fg[l].partition_broadcast(128), writes=[r_lnf], sres=r_lnf)
                    S.dma("pool", lnf_b[:], ln_fb[l].partition_broadcast(128), writes=[r_lnf], sres=r_lnf)
                    y0 = [sb(st, f"oy0_{i}", [128, D], F32) for i in range(2)]
                    y1 = [sb(st, f"oy1_{i}", [128, D], F32) for i in range(2)]
                    r_y0 = [S.res(f"oy0_{i}") for i in range(2)]
                    r_y1 = [S.res(f"oy1_{i}") for i in range(2)]
                    tin = [sb(st, f"otin{i}", [128, D], F32) for i in range(2)]
                    r_tin = [S.res(f"otin{i}") for i in range(2)]
                    yo = [sb(st, f"oyo{i}", [128, D], F32) for i in range(2)]
                    r_yo = [S.res(f"oyo{i}") for i in range(2)]
                    lst = sb(st, "olst", [128, 16], F32)
                    r_lst = S.res("olst")
                    for i in range(NT):
                        b = i % 2
                        r0 = i * 128
                        gi = r0 // G
                        S.dma("sp", tin[b][:], x1s[r0:r0 + 128, :], reads=[r_x1s[gi]], writes=[r_tin[b]], sres=r_tin[b])
                        for yt, ryt, sli in ((y0, r_y0, sl0i), (y1, r_y1, sl1i)):
                            S.dma("pool", None, None, reads=[r_sli] + r_yg, writes=[ryt[b]], sres=ryt[b],
                                  fn=(lambda e, yt=yt, sli=sli, b=b, i=i: e.indirect_dma_start(out=yt[b][:], out_offset=None, in_=ygs[:, :],
                                                                                                in_offset=IOA(ap=sli[:, i:i + 1], axis=0))))
                        ACT(tin[b][:], tin[b][:], AF.Copy, [r_tin[b]], [r_tin[b]], scale=ALPHA)
                        V("dve", "scalar_tensor_tensor", [r_y0[b], r_cwk, r_tin[b]], [r_tin[b]], out=tin[b][:], in0=y0[b][:], scalar=cwk[:, i, 0:1],
                          in1=tin[b][:], op0=ALU.mult, op1=ALU.add)
                        V("dve", "scalar_tensor_tensor", [r_y1[b], r_cwk, r_tin[b]], [r_tin[b]], out=tin[b][:], in0=y1[b][:], scalar=cwk[:, i, 1:2],
                          in1=tin[b][:], op0=ALU.mult, op1=ALU.add)
                        layer_norm_tile((lst, r_lst), tin[b], r_tin[b], lnf_g, lnf_b, [r_lnf], yo[b][:], r_yo[b], eng="dve")
                        o = S.dma("sp", dst[r0:r0 + 128, :], yo[b][:], reads=[r_yo[b]], writes=[r_dst[gi]], sres=r_yo[b], load=False)
                        if is_final:
                            final_tokens.append(o)
            S.barrier()

        r_xin = [S.res(f"xin{i}") for i in range(T // G)]
        r_yout = [S.res(f"yout{i}") for i in range(T // G)]
        pend = []
        for n_, l in enumerate(cfg.layers):
            if n_ == 0 and cfg.do_mix:
                prepass_layer(l)
            else:
                pend.append(lambda l=l: prepass_layer(l))
            if l % 2 == 0:
                pend.append(prepass_ffn)
            else:
                for e in range(NE):
                    pend.append(lambda e=e: prepass_moe([e]))
        if not cfg.do_mix:
            while pend:
                pend.pop(0)()
        last = cfg.layers[-1]
        for l in cfg.layers:
            src, rsrc = (x_in, r_xin) if l == cfg.layers[0] else (xs1, r_xs1)
            if cfg.do_mix:
                hk, pend = pend, []
                phase_mixer(l, src, rsrc, hooks=hk, router=(l % 2 == 1 and cfg.do_ffn and cfg.sparse))
            if (not cfg.do_mix) or ("B" not in cfg.parts):
                for gi in range(T // G):
                    S.dma("pool", x1s[gi * G:(gi + 1) * G, :], src[gi * G:(gi + 1) * G, :], reads=[rsrc[gi]], writes=[r_x1s[gi]],
                          sres=r_x1s[gi])
            if not cfg.do_ffn:
                for gi in range(T // G):
                    o = S.dma("pool", y_out[gi * G:(gi + 1) * G, :], x1s[gi * G:(gi + 1) * G, :], reads=[r_x1s[gi]], writes=[r_yout[gi]],
                              sres=r_yout[gi])
                    final_tokens.append(o)
            if cfg.do_ffn:
                ph = phase_moe if (l % 2 == 1 and cfg.sparse) else phase_ffn
                if l == last:
                    ph(l, y_out, r_yout, True)
                else:
                    ph(l, xs1, r_xs1, False)
        stats = S.emit(final_tokens)
        print("instr/wait per engine:", stats, "sems:", S.nsem, flush=True)
    return nc


def host_consts(S_):
    c = {}
    c["c_ident"] = np.eye(128, dtype=np.float32)
    inv = np.power(np.float32(500000.0), -np.arange(0, 16, 2, dtype=np.float32) / np.float32(16)).astype(np.float32)
    ang = (np.arange(S_, dtype=np.float32)[:, None] * inv[None, :]).astype(np.float32)
    co, si = np.cos(ang).astype(np.float32), np.sin(ang).astype(np.float32)
    c["c_cc"] = np.concatenate([co, co], 1).astype(np.float32)
    c["c_ss"] = np.concatenate([-si, si], 1).astype(np.float32)
    p = np.arange(128)[:, None]
    f = np.arange(512)[None, :]
    c["c_cmb"] = np.stack([np.where(cl * 128 + p <= f, 0.0, NEG) for cl in range(4)]).astype(np.float32)
    tok = np.arange(S_)[None, :]
    c["c_oh"] = (tok // MBLK == np.arange(16)[:, None]).astype(np.float32)
    j = np.tile(np.arange(16), MH)[None, :]
    blk = np.arange(16)[:, None]
    pm = (j < blk).astype(np.float32)
    c["c_pm"] = pm
    c["c_pa"] = ((pm - 1.0) * BIG).astype(np.float32)
    c["c_own"] = (j == blk).astype(np.float32)
    s = np.arange(128)[:, None]
    t = np.arange(128)[None, :]
    c["c_u"] = np.where(s <= t, -1.0 / 16.0, 0.0).astype(np.float32)
    c["c_tri"] = (s <= t).astype(np.float32)
    c["c_tris"] = (s < t).astype(np.float32)
    return c


def host_consts_moe(T):
    c = {}
    ntt = T // 128
    ngrp = (2 * T + NE * 511) // 512
    p = np.arange(128)[:, None]
    c["c_tok"] = (np.arange(ntt)[None, :] * 128 + p).astype(np.int32)
    tab, _ = block_table()
    base_m = tab[("m", 0, "g", 0)]
    c["c_wcst"] = (np.arange(21)[None, :] * 128 + p).astype(np.float32)
    c["c_jj"] = np.broadcast_to(np.arange(ngrp, dtype=np.float32)[None, :], (128, ngrp)).copy()
    c["c_thr"] = np.broadcast_to((np.arange(16, dtype=np.float32) * 512.0)[None, :], (128, 16)).copy()
    return c


_CACHE = {}


def kernel(**inputs):
    ncores = 8
    S_ = 4096
    nseq = 2
    if "prog" not in _CACHE:
        _CACHE["prog"] = build_program(Cfg(S=S_, NSEQ=nseq))
    nc = _CACHE["prog"]
    x = np.ascontiguousarray(np.asarray(inputs["x"], dtype=np.float32))
    consts = host_consts(S_)
    consts.update(host_consts_moe(S_ * nseq))
    shared = {k: np.ascontiguousarray(np.asarray(v, dtype=np.float32)) for k, v in inputs.items() if k != "x"}
    shared["moe_w_router_T"] = np.ascontiguousarray(shared["moe_w_router"][0].T)
    in_maps = []
    for c in range(ncores):
        m = dict(shared)
        m.update(consts)
        m["x"] = x[c * nseq:(c + 1) * nseq].reshape(nseq * S_, D)
        in_maps.append(m)
    res = run_bass_kernel_spmd(nc, in_maps, core_ids=list(range(ncores)))
    out = np.concatenate([np.asarray(r["y"]).reshape(nseq, S_, D) for r in res.results], axis=0)
    return out.astype(np.float32)
```

```python
import math
from contextlib import ExitStack

import numpy as np
import concourse.bass as bass
import concourse.mybir as mybir
from concourse.bass_utils import run_bass_kernel_spmd

F32 = mybir.dt.float32
BF16 = mybir.dt.bfloat16
I32 = mybir.dt.int32
AF = mybir.ActivationFunctionType
ALU = mybir.AluOpType
AX = mybir.AxisListType

D = 1024
DEPTH = 2
MH, MD = 8, 64
MBLK = 256
GH, GK = 4, 128
FF = 3584
NE = 8
IN_COLS = 5648
ALPHA = (2 * DEPTH) ** 0.25
LN_EPS = 1e-5
G = 512
NEG = -240000.0
BIG = 1.0e30

ENGS = ("pe", "act", "dve", "pool", "sp")
EPOCH = 12000
DMA_EPOCH = 1500


class Res:
    __slots__ = ("name", "writer", "readers", "stream_w", "stream_r")

    def __init__(self, name):
        self.name = name
        self.writer = None
        self.readers = []
        self.stream_w = None
        self.stream_r = None


class Stream:
    __slots__ = ("sem", "count", "name", "q")

    def __init__(self, name):
        self.name = name
        self.sem = None
        self.count = 0
        self.q = None


class Op:
    __slots__ = ("eng", "fn", "deps", "needs_inc", "epoch", "val", "is_dma", "stream", "dval")

    def __init__(self, eng, fn):
        self.eng = eng
        self.fn = fn
        self.deps = []
        self.needs_inc = False
        self.epoch = 0
        self.val = 0
        self.is_dma = False
        self.stream = None
        self.dval = 0


class Sched:
    def __init__(self, nc, stack):
        self.nc = nc
        self.stack = stack
        self.ops = {e: [] for e in ENGS}
        self.nsem = 0
        self.all_res = []
        self.live_streams = []
        self.free_streams = {"pool": [], "sp": []}

    def res(self, name):
        r = Res(name)
        self.all_res.append(r)
        return r

    def _newsem(self, name):
        self.nsem += 1
        return self.stack.enter_context(self.nc.semaphore(f"{name[:40]}_{self.nsem}"))

    def _collect(self, op, reads, writes):
        deps = []
        for r in reads:
            if r.writer is not None:
                deps.append((r.writer, "raw"))
        for w in writes:
            if w.writer is not None:
                deps.append((w.writer, "waw"))
            for t in w.readers:
                deps.append((t, "war"))
        seen = set()
        for t, kind in deps:
            if t is op or id(t) in seen:
                continue
            if (not t.is_dma) and (not op.is_dma) and t.eng == op.eng:
                if op.eng == "pe":
                    continue
            seen.add(id(t))
            op.deps.append(t)
            if not t.is_dma:
                t.needs_inc = True
        for r in reads:
            if not op.is_dma:
                r.readers = [t for t in r.readers if t.is_dma or t.eng != op.eng]
            r.readers.append(op)
        for w in writes:
            w.writer = op
            w.readers = []

    def op(self, eng, fn, reads=(), writes=()):
        o = Op(eng, fn)
        self._collect(o, reads, writes)
        self.ops[eng].append(o)
        return o

    def new_stream(self, name):
        st = Stream(name)
        st.sem = self._newsem(name[:48])
        return st

    def dma(self, q, out, in_, reads=(), writes=(), sres=None, load=True, stream=None, fn=None):
        o = Op(q, fn if fn is not None else (lambda e: e.dma_start(out=out, in_=in_)))
        o.is_dma = True
        st = stream if stream is not None else (sres.stream_w if load else sres.stream_r)
        if stream is None and (st is None or st.count >= DMA_EPOCH or st.q != q):
            fl = self.free_streams[q]
            while fl and fl[-1].count >= DMA_EPOCH - 64:
                fl.pop()
            if fl:
                st = fl.pop()
            else:
                st = Stream(sres.name + ("_w" if load else "_r"))
                st.sem = self._newsem(st.name[:48])
                st.q = q
            self.live_streams.append(st)
            if load:
                sres.stream_w = st
            else:
                sres.stream_r = st
        st.count += 1
        o.stream = st
        o.dval = st.count * 16
        self._collect(o, reads, writes)
        self.ops[q].append(o)
        return o

    def barrier(self):
        allr = list(self.all_res)
        for e in ENGS:
            if e == "sp":
                continue
            self.op(e, lambda e_: e_.drain(), reads=(), writes=allr)
        for r in allr:
            r.stream_w = None
            r.stream_r = None
        for st in self.live_streams:
            self.free_streams[st.q].append(st)
        self.live_streams = []

    def emit(self, final_tokens=()):
        nc = self.nc
        esems = {}
        for e in ENGS:
            cnt = 0
            ep = 0
            for o in self.ops[e]:
                if o.is_dma or not o.needs_inc:
                    continue
                if cnt >= EPOCH:
                    ep += 1
                    cnt = 0
                cnt += 1
                o.epoch = ep
                o.val = cnt
                if (e, ep) not in esems:
                    esems[(e, ep)] = self._newsem(f"s_{e}_{ep}")
        stats = {e: [0, 0] for e in ENGS}

        def run(e):
            def body(eng):
                known = {}
                for o in self.ops[e]:
                    need = {}
                    for t in o.deps:
                        if t.is_dma:
                            sem, val = t.stream.sem, t.dval
                        else:
                            sem, val = esems[(t.eng, t.epoch)], t.val
                        k = id(sem)
                        if k not in need or need[k][1] < val:
                            need[k] = (sem, val)
                    for k, (sem, val) in need.items():
                        if known.get(k, 0) >= val:
                            continue
                        known[k] = val
                        eng.wait_ge(sem, val)
                        stats[e][1] += 1
                    ins = o.fn(eng)
                    stats[e][0] += 1
                    if o.is_dma:
                        ins.then_inc(o.stream.sem, 16)
                    elif o.needs_inc:
                        ins.then_inc(esems[(e, o.epoch)], 1)
                if e == "pool":
                    for t in final_tokens:
                        eng.wait_ge(t.stream.sem, t.dval)
            return body

        with nc.Block() as block:
            block.tensor(run("pe"))
            block.scalar(run("act"))
            block.vector(run("dve"))
            block.gpsimd(run("pool"))
            block.sync(run("sp"))
        return stats


INP = [("qm", 0), ("km", 512), ("vm", 1024), ("qg", 1536), ("kg", 2048), ("vg", 2560), ("rg", 3072),
       ("ga0", 3600), ("ga1", 4112), ("gb0", 4624), ("gb1", 5136)]
LG_OFF = 3584


def block_table():
    tab = {}
    n = 0

    def add(key):
        nonlocal n
        tab[key] = n
        n += 1

    for l in range(DEPTH):
        for name, _ in INP:
            add((l, name))
        add((l, "lg"))
        add((l, "wa"))
        add((l, "wb"))
        add((l, "wo0"))
        add((l, "wo1"))
    for i in range(7):
        add(("f", "g", i))
    for i in range(7):
        add(("f", "u", i))
    for i in range(7):
        add(("f", "d", i))
    for e in range(NE):
        for i in range(7):
            add(("m", e, "g", i))
        for i in range(7):
            add(("m", e, "u", i))
        for i in range(7):
            add(("m", e, "d", i))
    return tab, n


class Cfg:
    def __init__(self, S=4096, NSEQ=2, layers=(0, 1), do_mix=True, do_ffn=True, dbg=False, parts="AB", skip=()):
        self.S = S
        self.NSEQ = NSEQ
        self.T = S * NSEQ
        self.layers = layers
        self.do_mix = do_mix
        self.do_ffn = do_ffn
        self.dbg = dbg
        self.parts = parts
        self.sparse = True
        self.skip = set(skip)


def build_program(cfg):
    S_, NSEQ, T = cfg.S, cfg.NSEQ, cfg.T
    NG = S_ // G
    NTS = S_ // 128
    NBK = S_ // MBLK
    nc = bass.Bass("TRN2", target_bir_lowering=False)

    def din(name, shape, dt=F32):
        return nc.dram_tensor(name, list(shape), dt, kind="ExternalInput").ap()

    x_in = din("x", [T, D])
    w_in = din("w_in", [DEPTH, D, IN_COLS])
    w_up = din("w_gla_gate_up", [DEPTH, 16, 512])
    b_gg = din("b_gla_gate", [DEPTH, 512])
    gn_g = din("gla_norm_g", [DEPTH, 512])
    w_ba = din("w_branch_a", [DEPTH, 512, D])
    w_bb = din("w_branch_b", [DEPTH, 512, D])
    w_o = din("w_out", [DEPTH, D, D])
    ln_mg = din("ln_mix_g", [DEPTH, D])
    ln_mb = din("ln_mix_b", [DEPTH, D])
    f_g = din("ffn_w_gate", [1, D, FF])
    f_u = din("ffn_w_up", [1, D, FF])
    f_d = din("ffn_w_down", [1, FF, D])
    m_r = din("moe_w_router", [1, D, NE])
    m_g = din("moe_w_gate", [1, NE, D, FF])
    m_u = din("moe_w_up", [1, NE, D, FF])
    m_d = din("moe_w_down", [1, NE, FF, D])
    ln_fg = din("ln_ffn_g", [DEPTH, D])
    ln_fb = din("ln_ffn_b", [DEPTH, D])
    c_ident = din("c_ident", [128, 128])
    c_cc = din("c_cc", [S_, 16])
    c_ss = din("c_ss", [S_, 16])
    c_cmb = din("c_cmb", [4, 128, 512])
    c_oh = din("c_oh", [16, S_])
    c_pm = din("c_pm", [16, 128])
    c_pa = din("c_pa", [16, 128])
    c_own = din("c_own", [16, 128])
    c_u = din("c_u", [128, 128])
    c_tri = din("c_tri", [128, 128])
    m_rT = din("moe_w_router_T", [NE, D])
    NTT = T // 128
    NGRP = (2 * T + NE * 511) // 512
    NSLOT = NGRP * 512
    c_tok = din("c_tok", [128, NTT], I32)
    c_wcst = din("c_wcst", [128, 21])
    c_jj = din("c_jj", [128, NGRP])
    c_thr = din("c_thr", [128, 16])
    c_tris = din("c_tris", [128, 128])

    tab, NBLKS = block_table()
    BASE_M = tab[("m", 0, "g", 0)]
    wsc_d = nc.dram_tensor("wsc", [BASE_M, 128, 4096], BF16, kind="Internal").ap()
    wsm = nc.dram_tensor("wsm", [NE * 21, 128, 4096], BF16, kind="Internal").ap()

    class _W:
        def __getitem__(self, b):
            return wsc_d[b] if b < BASE_M else wsm[b - BASE_M]
    wsc = _W()
    x1s = nc.dram_tensor("x1s", [T, D], F32, kind="Internal").ap()
    xs1 = nc.dram_tensor("xs1", [T, D], F32, kind="Internal").ap()
    yas = nc.dram_tensor("yas", [T // G, 128, 4, G], BF16, kind="Internal").ap()
    s2t = nc.dram_tensor("s2t", [NSLOT, 1], I32, kind="Internal").ap()
    ygs = nc.dram_tensor("ygs", [NSLOT, D], F32, kind="Internal").ap()
    y_out = nc.dram_tensor("y", [T, D], F32, kind="ExternalOutput").ap()
    dbg_t = {}
    if cfg.dbg:
        NGT = T // G
        for nm, shp in (("ya", [NGT, 128, 4, 512]), ("ybT", [NGT, 128, 4, G]), ("yaT", [NGT, 128, 4, G]), ("QT", [NGT, 128, MH, G]),
                        ("mT", [NGT, 128, 8, G]), ("BT", [NGT, 16, MH, G])):
            dbg_t[nm] = nc.dram_tensor("dbg_" + nm, shp, BF16, kind="ExternalOutput").ap()

    with ExitStack() as top:
        S = Sched(nc, top)
        r_wsc = [S.res(f"wsc{i}") for i in range(NBLKS)]
        r_x1s = [S.res(f"x1s{i}") for i in range(T // G)]
        r_xs1 = [S.res(f"xs1{i}") for i in range(T // G)]
        r_yas = [S.res(f"yas{i}") for i in range(T // G)]
        final_tokens = []
        r_dbg = S.res("dbg")

        pre = {"st": None, "ops": []}

        def pre_begin(name):
            pre["st"] = S.new_stream(name)
            pre["ops"] = []

        def pre_end():
            for o in pre["ops"]:
                o.dval = pre["st"].count * 16

        def conv(key, src, view):
            b = tab[key]
            if view == "c8":
                dst = wsc[b].rearrange("p (c n) -> p c n", c=8)
                s = src.rearrange("(c p) n -> p c n", p=128)
            elif view == "c4":
                dst = wsc[b].rearrange("p (c n) -> p c n", c=4)
                s = src.rearrange("(c p) n -> p c n", p=128)
            elif view == "lg":
                dst = wsc[b][:, 0:128].rearrange("p (c n) -> p c n", c=8)
                s = src.rearrange("(c p) n -> p c n", p=128)
            pre["ops"].append(S.dma("pool", dst, s, writes=[r_wsc[b]], sres=r_wsc[b], load=True, stream=pre["st"]))

        def prepass_layer(l):
            pre_begin(f"pre_l{l}a")
            for name, off in INP[:3]:
                conv((l, name), w_in[l][:, off:off + 512], "c8")
            pre_end()
            pre_begin(f"pre_l{l}")
            for name, off in INP[3:]:
                conv((l, name), w_in[l][:, off:off + 512], "c8")
            conv((l, "lg"), w_in[l][:, LG_OFF:LG_OFF + 16], "lg")
            conv((l, "wa"), w_ba[l], "c4")
            conv((l, "wb"), w_bb[l], "c4")
            conv((l, "wo0"), w_o[l][:, 0:512], "c8")
            conv((l, "wo1"), w_o[l][:, 512:1024], "c8")
            pre_end()

        def prepass_ffn():
            pre_begin("pre_ffn")
            for i in range(7):
                conv(("f", "g", i), f_g[0][:, i * 512:(i + 1) * 512], "c8")
                conv(("f", "u", i), f_u[0][:, i * 512:(i + 1) * 512], "c8")
                conv(("f", "d", i), f_d[0][i * 512:(i + 1) * 512, :], "c4")
            pre_end()

        def prepass_moe(experts=range(NE)):
            for e in experts:
                pre_begin(f"pre_moe{e}")
                for i in range(7):
                    conv(("m", e, "g", i), m_g[0][e][:, i * 512:(i + 1) * 512], "c8")
                    conv(("m", e, "u", i), m_u[0][e][:, i * 512:(i + 1) * 512], "c8")
                    conv(("m", e, "d", i), m_d[0][e][i * 512:(i + 1) * 512, :], "c4")
                pre_end()

        sbn = {"n": 0}

        def sb(st, name, shape, dt):
            sbn["n"] += 1
            return st.enter_context(nc.sbuf_tensor(f"{name}_{sbn['n']}", list(shape), dt))

        psb = [top.enter_context(nc.psum_tensor(f"ps{i}", [128, 512], F32)) for i in range(8)]
        r_ps = [S.res(f"ps{i}") for i in range(8)]

        identb = sb(top, "identb", [128, 128], BF16)
        r_ident = S.res("ident")
        S.dma("pool", identb[:], c_ident, writes=[r_ident], sres=r_ident)
        lgR = sb(top, "lgR", [128, T // 128, NE], F32)
        r_lgR = S.res("lgR")
        identf = sb(top, "identf", [128, 128], F32)
        r_identf = S.res("identf")
        S.dma("pool", identf[:], c_ident, writes=[r_identf], sres=r_identf)

        wring = []
        r_wring = []
        wstate = {"i": 0}

        def set_ring(st, n):
            wring[:] = [sb(st, f"wring{i}", [128, 4096], BF16) for i in range(n)]
            r_wring[:] = [S.res(f"wring{i}") for i in range(n)]
            wstate["i"] = 0

        def wload(key, view, alt=False):
            i = wstate["i"] % len(wring)
            wstate["i"] += 1
            b = tab[key]
            q = "pool" if (alt and i % 2 == 1) else "sp"
            if view == "lg":
                S.dma(q, wring[i][:, 0:128], wsc[b][:, 0:128], reads=[r_wsc[b]], writes=[r_wring[i]], sres=r_wring[i])
            else:
                S.dma(q, wring[i][:], wsc[b], reads=[r_wsc[b]], writes=[r_wring[i]], sres=r_wring[i])
            t = wring[i]
            if view == "c8":
                v = t[:].rearrange("p (c n) -> p c n", c=8)
            elif view == "c4":
                v = t[:].rearrange("p (c n) -> p c n", c=4)
            else:
                v = t[:, 0:128].rearrange("p (c n) -> p c n", c=8)
            return v, r_wring[i]

        def MM(out, lhsT, rhs, start, stop, R, W, **kw):
            S.op("pe", lambda e: e.matmul(out, lhsT=lhsT, rhs=rhs, start=start, stop=stop, **kw), reads=R, writes=W)

        def TR(out, in_, ident, R, W):
            S.op("pe", lambda e: e.transpose(out=out, in_=in_, identity=ident), reads=R, writes=W)

        def ACT(out, in_, func, R, W, **kw):
            S.op("act", lambda e: e.activation(out=out, in_=in_, func=func, **kw), reads=R, writes=W)

        def V(eng, meth, R, W, **kw):
            S.op(eng, lambda e: getattr(e, meth)(**kw), reads=R, writes=W)

        def layer_norm_tile(st_tiles, tin, r_tin, gb, bb, r_gbl, out, r_out, eng="dve"):
            stt, r_st = st_tiles
            V("dve", "bn_stats", [r_tin], [r_st], out=stt[:, 0:6], in_=tin[:, 0:512])
            V("dve", "bn_stats", [r_tin], [r_st], out=stt[:, 6:12], in_=tin[:, 512:1024])
            V("dve", "bn_aggr", [r_st], [r_st], out=stt[:, 12:14], in_=stt[:, 0:12])
            V("dve", "tensor_scalar_add", [r_st], [r_st], out=stt[:, 13:14], in0=stt[:, 13:14], scalar1=LN_EPS)
            ACT(stt[:, 13:14], stt[:, 13:14], AF.Sqrt, [r_st], [r_st])
            V("dve", "reciprocal", [r_st], [r_st], out=stt[:, 13:14], in_=stt[:, 13:14])
            V("dve", "tensor_scalar", [r_tin, r_st], [r_tin], out=tin[:], in0=tin[:], scalar1=stt[:, 12:13],
              scalar2=stt[:, 13:14], op0=ALU.subtract, op1=ALU.mult)
            V(eng, "tensor_tensor", [r_tin] + r_gbl, [r_tin], out=tin[:], in0=tin[:], in1=gb[:], op=ALU.mult)
            V(eng, "tensor_tensor", [r_tin] + r_gbl, [r_out], out=out, in0=tin[:], in1=bb[:], op=ALU.add)

        def phase_mixer(l, x_src, r_xsrc, hooks=(), router=False):
            hooks = list(hooks)
            pbig = [0, 1]
            pst = [3, 4]
            PACCS, PTR, PMISC = (2, 5), 6, 7

            def cload(st, name, shape, dt, src, q="pool"):
                t = sb(st, name, shape, dt)
                r = S.res(name)
                S.dma(q, t[:], src, writes=[r], sres=r)
                return t, r

            with ExitStack() as st:
              if "A" in cfg.parts:
                  set_ring(st, 4)
                  cc, r_cc = cload(st, "cc", [128, NTS, 16], F32, c_cc.rearrange("(t p) k -> p t k", p=128))
                  ss, r_ss = cload(st, "ss", [128, NTS, 16], F32, c_ss.rearrange("(t p) k -> p t k", p=128))
                  cmb, r_cmb = cload(st, "cmb", [128, 4, 512], BF16, c_cmb.rearrange("c p n -> p c n"))
                  oh, r_oh = cload(st, "oh", [16, S_], BF16, c_oh)
                  onesb = sb(st, "onesb", [128, 1], BF16)
                  r_ones = S.res("onesb")
                  V("pool", "memset", [], [r_ones], ap=onesb[:], constant=1.0)
                  zerob = sb(st, "zerob", [128, 512], BF16)
                  r_zero = S.res("zerob")
                  V("pool", "memset", [], [r_zero], ap=zerob[:], constant=0.0)

                  KT = sb(st, "KT", [128, 4, S_], BF16)
                  r_KT = [S.res(f"KT{g}") for g in range(NG)]
                  VA = sb(st, "VA", [128, NTS, MH, MD + 1], BF16)
                  r_VA = [S.res(f"VA{g}") for g in range(NG)]
                  V("pool", "memset", [], r_VA, ap=VA[:, :, :, MD:MD + 1], constant=1.0)
                  kmT = sb(st, "kmT", [128, 4, 16], BF16)
                  r_kmT = S.res("kmT")

                  NXB = 2
                  xf = [sb(st, f"xf{i}", [128, D], F32) for i in range(NXB)]
                  r_xf = [S.res(f"xf{i}") for i in range(NXB)]
                  xb = [sb(st, f"xb{i}", [128, D], BF16) for i in range(NXB)]
                  r_xb = [S.res(f"xb{i}") for i in range(NXB)]
                  xT = sb(st, "xT", [128, 8, G], BF16)
                  r_xT = S.res("xT")
                  QA = sb(st, "QA", [128, 4, 512], BF16)
                  r_QA = [S.res(f"QA{t}") for t in range(4)]
                  KA = sb(st, "KA", [128, 4, 512], BF16)
                  r_KA = [S.res(f"KA{t}") for t in range(4)]
                  QTs = [sb(st, f"QT{i}", [128, MH, G], BF16) for i in range(2)]
                  r_QTs = [S.res(f"QT{i}") for i in range(2)]
                  for i in range(2):
                      V("pool", "memset", [], [r_QTs[i]], ap=QTs[i][:], constant=0.0)
                  BTs = [sb(st, f"BT{i}", [16, MH, G], BF16) for i in range(2)]
                  r_BTs = [S.res(f"BT{i}") for i in range(2)]
                  gsb = sb(st, "gsb", [128, 6, 128], F32)
                  r_gsb = S.res("gsb")
                  gm = sb(st, "gm", [128, 3, 8], F32)
                  ksum = sb(st, "ksum", [128, 16], F32)
                  r_gm = S.res("gm")
                  bias_tm = sb(st, "bias_tm", [128, 128], BF16)
                  r_btm = S.res("bias_tm")
                  pmt = sb(st, "pmt", [128, 2, 3, 128], F32)
                  r_pmt = S.res("pmt")
                  rp = sb(st, "rp", [128, 2, 8, 16], F32)
                  r_rp = S.res("rp")
                  NPT = 4
                  PT = [sb(st, f"PT{i}", [128, G], BF16) for i in range(NPT)]
                  r_PT = [S.res(f"PT{i}") for i in range(NPT)]
                  rcp = sb(st, "rcp", [128, 2, 4], F32)
                  r_rcp = [S.res("rcp0"), S.res("rcp1")]
                  ya = sb(st, "ya", [128, 4, 512], BF16)
                  r_ya = S.res("ya")
                  yaT = [sb(st, f"yaT{i}", [128, 4, G], BF16) for i in range(2)]
                  r_yaT = [S.res(f"yaT{i}") for i in range(2)]
                  ring = {"big": 0, "st": 0, "pt": 0, "x": 0}

                  def nbig():
                      i = pbig[ring["big"] % len(pbig)]
                      ring["big"] += 1
                      return i

                  def prelude(s, g):
                      QTb, r_QTb, BTb, r_BTb = QTs[g % 2], r_QTs[g % 2], BTs[g % 2], r_BTs[g % 2]
                      tok0 = s * S_ + g * G
                      gi = tok0 // G
                      for t in range(4):
                          i = ring["x"] % NXB
                          ring["x"] += 1
                          r0 = tok0 + t * 128
                          S.dma("sp", xf[i][:], x_src[r0:r0 + 128, :], reads=[r_xsrc[gi]], writes=[r_xf[i]], sres=r_xf[i])
                          ACT(xb[i][:], xf[i][:], AF.Copy, [r_xf[i]], [r_xb[i]])
                          ptr = psb[PTR][:].bitcast(BF16).rearrange("p (c n) -> p c n", c=8)
                          for c in range(8):
                              TR(ptr[:, c, :], xb[i][:, c * 128:(c + 1) * 128], identb[:], [r_xb[i], r_ident], [r_ps[PTR]])
                          V("dve", "tensor_copy", [r_ps[PTR]], [r_xT], out=xT[:, :, t * 128:(t + 1) * 128], in_=ptr)

                      def proj_tm(key, consume):
                          wv, rw = wload((l, key), "c8")
                          for t in range(4):
                              pi = nbig()
                              for c in range(8):
                                  MM(psb[pi][:], xT[:, c, t * 128:(t + 1) * 128], wv[:, c, :], c == 0, c == 7,
                                     [r_xT, rw], [r_ps[pi]])
                              consume(t, psb[pi], r_ps[pi])

                      def rope_to(dst, r_dst):
                          def consume(t, ps, rps):
                              tt = g * 4 + t
                              p3 = ps[:].rearrange("p (h d) -> p h d", h=MH)
                              d3 = dst[:, t, :].rearrange("p (h d) -> p h d", h=MH)
                              ccb = cc[:, tt, :].unsqueeze(1).broadcast_to([128, MH, 16])
                              ssb = ss[:, tt, :].unsqueeze(1).broadcast_to([128, MH, 16])
                              V("dve", "tensor_tensor", [rps, r_cc], [r_rp], out=rp[:, 0, :, :], in0=p3[:, :, 0:16], in1=ccb, op=ALU.mult)
                              V("dve", "tensor_tensor", [rps, r_ss], [r_rp], out=rp[:, 1, :, 0:8], in0=p3[:, :, 8:16], in1=ssb[:, :, 0:8], op=ALU.mult)
                              V("dve", "tensor_tensor", [rps, r_ss], [r_rp], out=rp[:, 1, :, 8:16], in0=p3[:, :, 0:8], in1=ssb[:, :, 8:16], op=ALU.mult)
                              V("dve", "tensor_tensor", [r_rp], [r_dst[t]], out=d3[:, :, 0:16], in0=rp[:, 0, :, :], in1=rp[:, 1, :, :], op=ALU.add)
                              ACT(d3[:, :, 16:64], p3[:, :, 16:64], AF.Copy, [rps], [r_dst[t]])
                          return consume

                      proj_tm("qm", rope_to(QA, r_QA))
                      proj_tm("km", rope_to(KA, r_KA))

                      def cons_v(t, ps, rps):
                          tt = g * 4 + t
                          ACT(VA[:, tt, :, 0:MD], ps[:].rearrange("p (h d) -> p h d", h=MH), AF.Copy, [rps], [r_VA[g]])
                      proj_tm("vm", cons_v)

                      yield
                      ptr4 = psb[PTR][:].bitcast(BF16).rearrange("p (c n) -> p c n", c=8)
                      for t in range(4):
                          tt = g * 4 + t
                          for pr in range(4):
                              TR(ptr4[:, pr, :], KA[:, t, pr * 128:(pr + 1) * 128], identb[:], [r_KA[t], r_ident], [r_ps[PTR]])
                          V("dve", "tensor_copy", [r_ps[PTR]], [r_KT[g]], out=KT[:, :, tt * 128:(tt + 1) * 128], in_=ptr4[:, 0:4, :])
                      pm3 = psb[PMISC][:, 0:16].rearrange("p (a b) -> p a b", a=4)
                      for t in range(4):
                          for pr in range(4):
                              if "ksum" in cfg.skip:
                                  continue
                              MM(pm3[:, pr, t:t + 1], KA[:, t, pr * 128:(pr + 1) * 128], onesb[:, 0:1], True, True,
                                 [r_KA[t], r_ones], [r_ps[PMISC]])
                      ks3 = ksum[:].rearrange("p (a b) -> p a b", a=4)
                      if "ksum" in cfg.skip:
                          V("dve", "memset", [], [r_gm], ap=ksum[:], constant=0.0)
                      else:
                          V("dve", "tensor_copy", [r_ps[PMISC]], [r_gm], out=ks3, in_=pm3)
                      for bb_ in range(2):
                          blk = g * 2 + bb_
                          V("dve", "tensor_tensor", [r_gm], [r_gm], out=gm[:, 0, 0:4], in0=ks3[:, :, 2 * bb_],
                            in1=ks3[:, :, 2 * bb_ + 1], op=ALU.add)
                          V("dve", "tensor_scalar_mul", [r_gm], [r_kmT], out=kmT[:, :, blk], in0=gm[:, 0, 0:4], scalar1=1.0 / MBLK)

                      for t in range(4):
                          for pr in range(4):
                              TR(ptr4[:, pr, :], QA[:, t, pr * 128:(pr + 1) * 128], identb[:], [r_QA[t], r_ident], [r_ps[PTR]])
                          QT4 = QTb[:].rearrange("p (a b) n -> p a b n", b=2)
                          V("dve", "tensor_copy", [r_ps[PTR]], [r_QTb], out=QT4[0:64, :, 0, t * 128:(t + 1) * 128], in_=ptr4[0:64, 0:4, :])
                          V("dve", "tensor_copy", [r_ps[PTR]], [r_QTb], out=QT4[64:128, :, 1, t * 128:(t + 1) * 128], in_=ptr4[64:128, 0:4, :])

                      for bb_ in range(2):
                          blk = g * 2 + bb_
                          S.dma("sp", pmt[:, bb_, 0, :], c_pm[blk].partition_broadcast(128), writes=[r_pmt], sres=r_pmt)
                          S.dma("sp", pmt[:, bb_, 1, :], c_pa[blk].partition_broadcast(128), writes=[r_pmt], sres=r_pmt)
                          S.dma("sp", pmt[:, bb_, 2, :], c_own[blk].partition_broadcast(128), writes=[r_pmt], sres=r_pmt)

                      yield
                      if "gate" in cfg.skip:
                          V("dve", "memset", [], [r_BTb], ap=BTb[:], constant=0.0)
                      for t in range(4 if "gate" not in cfg.skip else 0):
                          if t == 2:
                              yield
                          bb_ = t // 2
                          pg = psb[PMISC][:, 128:256]
                          for h in range(MH):
                              pr, hh = h // 2, h % 2
                              MM(pg[:, h * 16:(h + 1) * 16], QTb[:, h, t * 128:(t + 1) * 128],
                                 kmT[:, pr, :], True, True, [r_QTb, r_kmT], [r_ps[PMISC]])
                          g0 = gsb[:, 0, :]
                          g1 = gsb[:, 1, :]
                          e1 = gsb[:, 2, :]
                          V("dve", "tensor_tensor", [r_ps[PMISC], r_pmt], [r_gsb], out=g0, in0=pg, in1=pmt[:, bb_, 0, :], op=ALU.mult)
                          V("dve", "tensor_tensor", [r_gsb, r_pmt], [r_gsb], out=g0, in0=g0, in1=pmt[:, bb_, 1, :], op=ALU.add)
                          g03 = g0.rearrange("p (h j) -> p h j", h=MH)
                          g13 = g1.rearrange("p (h j) -> p h j", h=MH)
                          e13 = e1.rearrange("p (h j) -> p h j", h=MH)
                          src3 = g03
                          for k in range(3):
                              V("dve", "tensor_reduce", [r_gsb], [r_gm], out=gm[:, k, :], in_=src3, axis=AX.X, op=ALU.max)
                              if k == 2:
                                  break
                              mb = gm[:, k, :].unsqueeze(2).broadcast_to([128, MH, 16])
                              V("dve", "tensor_tensor", [r_gsb, r_gm], [r_gsb], out=e13, in0=src3, in1=mb, op=ALU.is_ge)
                              V("dve", "scalar_tensor_tensor", [r_gsb], [r_gsb], out=g13, in0=e13, scalar=-BIG, in1=src3,
                                op0=ALU.mult, op1=ALU.add)
                              src3 = g13
                          mb = gm[:, 2, :].unsqueeze(2).broadcast_to([128, MH, 16])
                          V("dve", "tensor_tensor", [r_gsb, r_gm], [r_gsb], out=e13, in0=g03, in1=mb, op=ALU.is_ge)
                          V("dve", "tensor_tensor", [r_gsb, r_pmt], [r_gsb], out=e1, in0=e1, in1=pmt[:, bb_, 0, :], op=ALU.mult)
                          V("dve", "tensor_tensor", [r_gsb, r_pmt], [r_gsb], out=e1, in0=e1, in1=pmt[:, bb_, 2, :], op=ALU.add)
                          V("dve", "tensor_scalar", [r_gsb], [r_btm], out=bias_tm[:], in0=e1, scalar1=-1.0, scalar2=-NEG,
                            op0=ALU.add, op1=ALU.mult)
                          pb = psb[PTR][:].bitcast(BF16)[0:16, :].rearrange("p (h n) -> p h n", h=MH)
                          for h in range(MH):
                              TR(pb[:, h, :], bias_tm[:, h * 16:(h + 1) * 16], identb[:], [r_btm, r_ident], [r_ps[PTR]])
                          V("dve", "tensor_copy", [r_ps[PTR]], [r_BTb], out=BTb[:, :, t * 128:(t + 1) * 128], in_=pb)


                  def attend(s, g, pre):
                      QTb, r_QTb, BTb, r_BTb = QTs[g % 2], r_QTs[g % 2], BTs[g % 2], r_BTs[g % 2]
                      tok0 = s * S_ + g * G
                      gi = tok0 // G
                      ptr4 = psb[PTR][:].bitcast(BF16).rearrange("p (c n) -> p c n", c=8)
                      nch = 4 * (g + 1)
                      if "attn" in cfg.skip:
                          V("dve", "memset", [], [r_ya], ap=ya[:], constant=0.0)
                      items = [(h, c) for h in range(MH if "attn" not in cfg.skip else 0) for c in range(nch)]
                      PIPE = 2
                      pst4 = [3, 4, 7]
                      slots = {}

                      def emit_st(h, c):
                          pr = h // 2
                          gk = c // 4
                          si = pst4[ring["st"] % 3]
                          ring["st"] += 1
                          own_grp = (gk == g)
                          MM(psb[si][:], KT[:, pr, c * 128:(c + 1) * 128], QTb[:, h, :],
                             True, False, [r_KT[gk], r_QTb], [r_ps[si]])
                          MM(psb[si][:], oh[:, c * 128:(c + 1) * 128], BTb[:, h, :], False, not own_grp,
                             [r_oh, r_BTb], [r_ps[si]])
                          if own_grp:
                              MM(psb[si][:], identb[:], cmb[:, c % 4, :], False, True, [r_ident, r_cmb], [r_ps[si]])
                          pi = ring["pt"] % NPT
                          ring["pt"] += 1
                          ACT(PT[pi][:], psb[si][:], AF.Exp, [r_ps[si]], [r_PT[pi]], scale=1.0 / 8.0)
                          slots[(h, c)] = pi

                      def emit_pv(h, c):
                          gk = c // 4
                          a_i = h % 2
                          PACC = PACCS[a_i]
                          acc = psb[PACC][:, 0:260].rearrange("p (q d) -> p q d", q=4)
                          if c == 0:
                              MM(psb[PACC][:, 0:260], zerob[0:1, 0:128], zerob[0:1, 0:260], True, False,
                                 [r_zero], [r_ps[PACC]], skip_group_check=True)
                          pi = slots.pop((h, c))
                          for qs in range(4):
                              MM(acc[:, qs, :], PT[pi][:, qs * 128:(qs + 1) * 128], VA[:, c, h, :], False, c == nch - 1,
                                 [r_PT[pi], r_VA[gk]], [r_ps[PACC]], skip_group_check=True)
                          if c == nch - 1:
                              V("dve", "reciprocal", [r_ps[PACC]], [r_rcp[a_i]], out=rcp[:, a_i, :], in_=acc[:, :, MD])
                              V("dve", "tensor_tensor", [r_ps[PACC], r_rcp[a_i]], [r_ya], out=ya[:, :, h * MD:(h + 1) * MD],
                                in0=acc[:, :, 0:MD], in1=rcp[:, a_i, :].unsqueeze(2).broadcast_to([128, 4, MD]), op=ALU.mult)

                      pull = {0, len(items) // 4, len(items) // 2, (3 * len(items)) // 4}
                      for idx in range(len(items) + PIPE):
                          if pre is not None and idx in pull:
                              next(pre, None)
                          if idx < len(items):
                              emit_st(*items[idx])
                          if idx >= PIPE:
                              emit_pv(*items[idx - PIPE])
                      yi = gi % 2
                      for t in range(4):
                          for kc in range(4):
                              TR(ptr4[:, kc, :], ya[:, t, kc * 128:(kc + 1) * 128], identb[:], [r_ya, r_ident], [r_ps[PTR]])
                          V("dve", "tensor_copy", [r_ps[PTR]], [r_yaT[yi]], out=yaT[yi][:, :, t * 128:(t + 1) * 128], in_=ptr4[:, 0:4, :])
                      S.dma("sp", yas[gi], yaT[yi][:], reads=[r_yaT[yi]], writes=[r_yas[gi]], sres=r_yaT[yi], load=False)
                      if hooks:
                          hooks.pop(0)()
                      if cfg.dbg:
                          for nm, tl, rr in (("ya", ya, r_ya), ("yaT", yaT[yi], r_yaT[yi]), ("QT", QTb, r_QTb)):
                              final_tokens.append(S.dma("pool", dbg_t[nm][gi], tl[:], reads=[rr], writes=[r_dbg], sres=r_dbg))
                          final_tokens.append(S.dma("pool", dbg_t["BT"][gi], BTb[:], reads=[r_BTb], writes=[r_dbg], sres=r_dbg))
                      if pre is not None:
                          for _ in pre:
                              pass

                  for s in range(NSEQ):
                      V("pool", "memset", [], [r_kmT], ap=kmT[:], constant=0.0)
                      for _ in prelude(s, 0):
                          pass
                      for g in range(NG):
                          attend(s, g, prelude(s, g + 1) if g + 1 < NG else None)
            while hooks:
                hooks.pop(0)()
            S.barrier()

            with ExitStack() as st:
              if "B" in cfg.parts:
                  set_ring(st, 6)
                  uu, r_uu = cload(st, "uu_B", [128, 128], F32, c_u)
                  tri, r_tri = cload(st, "tri_B", [128, 128], F32, c_tri)
                  lnm_g, r_lng = cload(st, "lnm_g_B", [128, D], F32, ln_mg[l].partition_broadcast(128))
                  lnm_b, r_lnb = cload(st, "lnm_b_B", [128, D], F32, ln_mb[l].partition_broadcast(128))
                  gnt, r_gnt = cload(st, "gnt_B", [128, 512], F32, gn_g[l].partition_broadcast(128))
                  wupa = sb(st, "wupa_B", [32, 512], BF16)
                  r_wupa = S.res("wupa")
                  V("pool", "memset", [], [r_wupa], ap=wupa[:], constant=0.0)
                  S.dma("pool", wupa[0:16, :], w_up[l], reads=[], writes=[r_wupa], sres=r_wupa)
                  S.dma("pool", wupa[16:17, :], b_gg[l].unsqueeze(0), reads=[], writes=[r_wupa], sres=r_wupa)

                  stf = sb(st, "stf_B", [128, GH, 128], F32)
                  stb = sb(st, "stb_B", [128, GH, 128], BF16)
                  r_stf = [S.res(f"stf{h}") for h in range(GH)]
                  r_stb = [S.res(f"stb{h}") for h in range(GH)]
                  lgT = sb(st, "lgT_B", [32, G], BF16)
                  r_lgT = S.res("lgT")
                  V("pool", "memset", [], [r_lgT], ap=lgT[:], constant=1.0)

                  NXB = 2
                  xf = [sb(st, f"xf_B{i}", [128, D], F32) for i in range(NXB)]
                  r_xf = [S.res(f"xf{i}") for i in range(NXB)]
                  xb = [sb(st, f"xb_B{i}", [128, D], BF16) for i in range(NXB)]
                  r_xb = [S.res(f"xb{i}") for i in range(NXB)]
                  xT = sb(st, "xT_B", [128, 8, G], BF16)
                  r_xT = S.res("xT")
                  yaT = sb(st, "yaTb_B", [128, 4, G], BF16)
                  r_yaT = S.res("yaTb")
                  ybT = sb(st, "ybT_B", [128, 4, G], BF16)
                  r_ybT = S.res("ybT")
                  spt = sb(st, "spt_B", [128, 4, 512], F32)
                  r_spt = [S.res(f"spt{t}") for t in range(4)]
                  eq = sb(st, "eq_B", [128, GH, G], F32)
                  ek = sb(st, "ek_B", [128, GH, G], F32)
                  r_eq = [S.res(f"eq{h}") for h in range(GH)]
                  r_ek = [S.res(f"ek{h}") for h in range(GH)]
                  qdT = sb(st, "qdT_B", [128, GH, G], BF16)
                  kiT = sb(st, "kiT_B", [128, GH, G], BF16)
                  r_qdT = [S.res(f"qdT{h}") for h in range(GH)]
                  r_kiT = [S.res(f"kiT{h}") for h in range(GH)]
                  kiM = sb(st, "kiM_B", [128, 2, GH, 128], BF16)
                  r_kiM = [S.res("kiM0"), S.res("kiM1")]
                  vg = sb(st, "vg_B", [128, 4, 512], BF16)
                  r_vg = [S.res(f"vg{t}") for t in range(4)]
                  rgs = sb(st, "rgs_B", [128, 4, 512], BF16)
                  r_rgs = [S.res(f"rgs{t}") for t in range(4)]
                  attm = sb(st, "attm_B", [128, 2, GH, 128], BF16)
                  r_attm = [S.res("attm0"), S.res("attm1")]
                  stmp = sb(st, "stmp_B", [128, 2, GH, 128], F32)
                  r_stmp = [S.res("stmp0"), S.res("stmp1")]
                  hn = sb(st, "hn_B", [128, 40], F32)
                  r_hn = S.res("hn")
                  ob = sb(st, "ob_B", [128, 512], F32)
                  r_ob = S.res("ob")
                  yb = sb(st, "yb_B", [128, 512], BF16)
                  r_yb = S.res("yb")
                  sg = sb(st, "sg_B", [128, 2, G], F32)
                  r_sg = [S.res("sg0"), S.res("sg1")]
                  mT = sb(st, "mT_B", [128, 8, G], BF16)
                  r_mT = S.res("mT")
                  tin = [sb(st, f"tin_B{i}", [128, D], F32) for i in range(2)]
                  r_tin = [S.res(f"tin{i}") for i in range(2)]
                  x1o = [sb(st, f"x1o_B{i}", [128, D], F32) for i in range(2)]
                  r_x1o = [S.res(f"x1o{i}") for i in range(2)]
                  lst = sb(st, "lst_B", [128, 16], F32)
                  r_lst = S.res("lst")
                  if router:
                      wrB = sb(st, "wrB", [128, NE, D], F32)
                      r_wrB = S.res("wrB")
                      for e in range(NE):
                          S.dma("pool", wrB[:, e, :], m_rT[e].partition_broadcast(128), writes=[r_wrB], sres=r_wrB)
                  ring = {"big": 0, "x": 0, "ln": 0}

                  pbigB = [0, 1, 2, 4, 5]

                  def nbig():
                      i = pbigB[ring["big"] % len(pbigB)]
                      ring["big"] += 1
                      return i

                  for s in range(NSEQ):
                      V("pool", "memset", [], [r_stf[0]], ap=stf[:], constant=0.0)
                      V("pool", "memset", [], [r_stb[0]], ap=stb[:], constant=0.0)
                      for g in range(NG):
                          tok0 = s * S_ + g * G
                          gi = tok0 // G
                          for t in range(4):
                              i = ring["x"] % NXB
                              ring["x"] += 1
                              r0 = tok0 + t * 128
                              S.dma("pool", xf[i][:], x_src[r0:r0 + 128, :], reads=[r_xsrc[gi]], writes=[r_xf[i]], sres=r_xf[i])
                              ACT(xb[i][:], xf[i][:], AF.Copy, [r_xf[i]], [r_xb[i]])
                              ptr = psb[PTR][:].bitcast(BF16).rearrange("p (c n) -> p c n", c=8)
                              for c in range(8):
                                  TR(ptr[:, c, :], xb[i][:, c * 128:(c + 1) * 128], identb[:], [r_xb[i], r_ident], [r_ps[PTR]])
                              V("dve", "tensor_copy", [r_ps[PTR]], [r_xT], out=xT[:, :, t * 128:(t + 1) * 128], in_=ptr)
                          S.dma("pool", yaT[:], yas[gi], reads=[r_yas[gi]], writes=[r_yaT], sres=r_yaT)
                          ptr4 = psb[PTR][:].bitcast(BF16).rearrange("p (c n) -> p c n", c=8)

                          def proj_tm(key, consume):
                              wv, rw = wload((l, key), "c8")
                              for t in range(4):
                                  pi = nbig()
                                  for c in range(8):
                                      MM(psb[pi][:], xT[:, c, t * 128:(t + 1) * 128], wv[:, c, :], c == 0, c == 7,
                                         [r_xT, rw], [r_ps[pi]])
                                  consume(t, psb[pi], r_ps[pi])

                          wv, rw = wload((l, "lg"), "lg")
                          pi = nbig()
                          for c in range(8):
                              MM(psb[pi][0:16, :], wv[:, c, :], xT[:, c, :], c == 0, c == 7, [r_xT, rw], [r_ps[pi]])
                          V("dve", "tensor_copy", [r_ps[pi]], [r_lgT], out=lgT[0:16, :], in_=psb[pi][0:16, :])
                          for t in range(4):
                              pi = nbig()
                              MM(psb[pi][:], lgT[:, t * 128:(t + 1) * 128], wupa[:], True, True, [r_lgT, r_wupa], [r_ps[pi]])
                              ACT(spt[:, t, :], psb[pi][:], AF.Exp, [r_ps[pi]], [r_spt[t]], scale=-1.0)
                              ACT(spt[:, t, :], spt[:, t, :], AF.Ln, [r_spt[t]], [r_spt[t]], bias=1.0)
                          for h in range(GH):
                              pi = nbig()
                              for t in range(4):
                                  MM(psb[pi][:, t * 128:(t + 1) * 128], spt[:, t, h * 128:(h + 1) * 128], uu[:], True, True,
                                     [r_spt[t], r_uu], [r_ps[pi]])
                              ACT(eq[:, h, :], psb[pi][:], AF.Exp, [r_ps[pi]], [r_eq[h]])
                              ACT(ek[:, h, :], psb[pi][:], AF.Exp, [r_ps[pi]], [r_ek[h]], scale=-1.0)
                          wv, rw = wload((l, "qg"), "c8")
                          for h in range(GH):
                              pi = nbig()
                              for c in range(8):
                                  MM(psb[pi][:], wv[:, c, h * 128:(h + 1) * 128], xT[:, c, :], c == 0, c == 7, [r_xT, rw], [r_ps[pi]])
                              V("dve", "scalar_tensor_tensor", [r_ps[pi], r_eq[h]], [r_qdT[h]], out=qdT[:, h, :], in0=psb[pi][:],
                                scalar=GK ** -0.5, in1=eq[:, h, :], op0=ALU.mult, op1=ALU.mult)
                          wv, rw = wload((l, "kg"), "c8")
                          for h in range(GH):
                              pi = nbig()
                              for c in range(8):
                                  MM(psb[pi][:], wv[:, c, h * 128:(h + 1) * 128], xT[:, c, :], c == 0, c == 7, [r_xT, rw], [r_ps[pi]])
                              V("dve", "tensor_tensor", [r_ps[pi], r_ek[h]], [r_kiT[h]], out=kiT[:, h, :], in0=psb[pi][:],
                                in1=ek[:, h, :], op=ALU.mult)

                          def cons_vg(t, ps, rps):
                              ACT(vg[:, t, :], ps[:], AF.Copy, [rps], [r_vg[t]])
                          proj_tm("vg", cons_vg)

                          def cons_rg(t, ps, rps):
                              ACT(rgs[:, t, :], ps[:], AF.Silu, [rps], [r_rgs[t]])
                          proj_tm("rg", cons_rg)

                          dec4s = {}

                          def gla_stage1(t):
                              j = t % 2
                              cs = slice(t * 128, (t + 1) * 128)
                              for h in range(GH):
                                  TR(ptr4[:, h, :], kiT[:, h, cs], identb[:], [r_kiT[h], r_ident], [r_ps[PTR]])
                              V("dve", "tensor_copy", [r_ps[PTR]], [r_kiM[j]], out=kiM[:, j, :, :], in_=ptr4[:, 0:4, :])
                              pa = nbig()
                              for h in range(GH):
                                  MM(psb[pa][:, h * 128:(h + 1) * 128], kiT[:, h, cs], qdT[:, h, cs], True, True, [r_kiT[h], r_qdT[h]], [r_ps[pa]])
                              V("dve", "tensor_tensor", [r_ps[pa], r_tri], [r_attm[j]], out=attm[:, j, :, :],
                                in0=psb[pa][:].rearrange("p (h n) -> p h n", h=GH), in1=tri[:].unsqueeze(1).broadcast_to([128, GH, 128]), op=ALU.mult)

                          def gla_stage2(t):
                              j = t % 2
                              cs = slice(t * 128, (t + 1) * 128)
                              PO = PMISC if t % 2 == 0 else 3
                              po = psb[PO]
                              for h in range(GH):
                                  MM(po[:, h * 128:(h + 1) * 128], attm[:, j, h, :], vg[:, t, h * 128:(h + 1) * 128], True, False,
                                     [r_attm[j], r_vg[t]], [r_ps[PO]])
                                  MM(po[:, h * 128:(h + 1) * 128], qdT[:, h, cs], stb[:, h, :], False, True,
                                     [r_qdT[h], r_stb[0]], [r_ps[PO]])
                              pd = nbig()
                              for h in range(GH):
                                  MM(psb[pd][:, h * 128:(h + 1) * 128], kiM[:, j, h, :], vg[:, t, h * 128:(h + 1) * 128], True, True,
                                     [r_kiM[j], r_vg[t]], [r_ps[pd]])
                              V("dve", "tensor_tensor", [r_ps[pd], r_stf[0]], [r_stmp[j]], out=stmp[:, j, :, :],
                                in0=psb[pd][:].rearrange("p (h n) -> p h n", h=GH), in1=stf[:], op=ALU.add)
                              dec4 = eq[:, :, t * 128 + 127:t * 128 + 128].broadcast_to([128, GH, 128])
                              V("dve", "tensor_tensor", [r_stmp[j]] + r_eq, [r_stf[0]], out=stf[:], in0=stmp[:, j, :, :], in1=dec4, op=ALU.mult)
                              V("dve", "tensor_tensor", [r_stmp[j]] + r_eq, [r_stb[0]], out=stb[:], in0=stmp[:, j, :, :], in1=dec4, op=ALU.mult)
                              return po, PO

                          gla_stage1(0)
                          for t in range(4):
                              if t + 1 < 4:
                                  gla_stage1(t + 1)
                              po, PO = gla_stage2(t)
                              for h in range(GH):
                                  V("dve", "bn_stats", [r_ps[PO]], [r_hn], out=hn[:, h * 6:(h + 1) * 6], in_=po[:, h * 128:(h + 1) * 128])
                                  V("dve", "bn_aggr", [r_hn], [r_hn], out=hn[:, 24 + 2 * h:26 + 2 * h], in_=hn[:, h * 6:(h + 1) * 6])
                              mv = hn[:, 24:32].rearrange("p (h k) -> p h k", h=GH)
                              V("dve", "tensor_scalar_add", [r_hn], [r_hn], out=hn[:, 32:36], in0=mv[:, :, 1], scalar1=LN_EPS)
                              ACT(hn[:, 32:36], hn[:, 32:36], AF.Sqrt, [r_hn], [r_hn])
                              V("dve", "reciprocal", [r_hn], [r_hn], out=hn[:, 32:36], in_=hn[:, 32:36])
                              for h in range(GH):
                                  V("dve", "tensor_scalar", [r_ps[PO], r_hn], [r_ob], out=ob[:, h * 128:(h + 1) * 128],
                                    in0=po[:, h * 128:(h + 1) * 128], scalar1=hn[:, 24 + 2 * h:25 + 2 * h], scalar2=hn[:, 32 + h:33 + h],
                                    op0=ALU.subtract, op1=ALU.mult)
                              V("pool", "tensor_tensor", [r_ob, r_gnt], [r_ob], out=ob[:], in0=ob[:], in1=gnt[:], op=ALU.mult)
                              V("pool", "tensor_tensor", [r_ob, r_rgs[t]], [r_yb], out=yb[:], in0=ob[:], in1=rgs[:, t, :], op=ALU.mult)
                              for kc in range(4):
                                  TR(ptr4[:, kc, :], yb[:, kc * 128:(kc + 1) * 128], identb[:], [r_yb, r_ident], [r_ps[PTR]])
                              V("dve", "tensor_copy", [r_ps[PTR]], [r_ybT], out=ybT[:, :, t * 128:(t + 1) * 128], in_=ptr4[:, 0:4, :])

                          wa_v, rwa = wload((l, "wa"), "c4")
                          wb_v, rwb = wload((l, "wb"), "c4")
                          for half in range(2):
                              ga_v, rga = wload((l, f"ga{half}"), "c8")
                              gb_v, rgb = wload((l, f"gb{half}"), "c8")
                              for mm_ in range(4):
                                  m = half * 4 + mm_
                                  cs = slice(mm_ * 128, (mm_ + 1) * 128)
                                  ms = slice(m * 128, (m + 1) * 128)
                                  pi = nbig()
                                  for c in range(8):
                                      MM(psb[pi][:], ga_v[:, c, cs], xT[:, c, :], c == 0, c == 7, [r_xT, rga], [r_ps[pi]])
                                  ACT(sg[:, 0, :], psb[pi][:], AF.Sigmoid, [r_ps[pi]], [r_sg[0]])
                                  pi = nbig()
                                  for c in range(4):
                                      MM(psb[pi][:], wa_v[:, c, ms], yaT[:, c, :], c == 0, c == 3, [r_yaT, rwa], [r_ps[pi]])
                                  V("dve", "tensor_tensor", [r_ps[pi], r_sg[0]], [r_sg[0]], out=sg[:, 0, :], in0=psb[pi][:], in1=sg[:, 0, :], op=ALU.mult)
                                  pi = nbig()
                                  for c in range(8):
                                      MM(psb[pi][:], gb_v[:, c, cs], xT[:, c, :], c == 0, c == 7, [r_xT, rgb], [r_ps[pi]])
                                  ACT(sg[:, 1, :], psb[pi][:], AF.Sigmoid, [r_ps[pi]], [r_sg[1]])
                                  pi = nbig()
                                  for c in range(4):
                                      MM(psb[pi][:], wb_v[:, c, ms], ybT[:, c, :], c == 0, c == 3, [r_ybT, rwb], [r_ps[pi]])
                                  V("dve", "tensor_tensor", [r_ps[pi], r_sg[1]], [r_sg[1]], out=sg[:, 1, :], in0=psb[pi][:], in1=sg[:, 1, :], op=ALU.mult)
                                  V("pool", "tensor_tensor", [r_sg[0], r_sg[1]], [r_mT], out=mT[:, m, :], in0=sg[:, 0, :], in1=sg[:, 1, :], op=ALU.add)

                          if cfg.dbg:
                              final_tokens.append(S.dma("pool", dbg_t["ybT"][gi], ybT[:], reads=[r_ybT], writes=[r_dbg], sres=r_dbg))
                              final_tokens.append(S.dma("pool", dbg_t["mT"][gi], mT[:], reads=[r_mT], writes=[r_dbg], sres=r_dbg))
                          wo_v0, rwo0 = wload((l, "wo0"), "c8")
                          wo_v1, rwo1 = wload((l, "wo1"), "c8")
                          for t in range(4):
                              i = ring["ln"] % 2
                              ring["ln"] += 1
                              r0 = tok0 + t * 128
                              S.dma("pool", tin[i][:], x_src[r0:r0 + 128, :], reads=[r_xsrc[gi]], writes=[r_tin[i]], sres=r_tin[i])
                              for half, (wv_, rw_) in enumerate(((wo_v0, rwo0), (wo_v1, rwo1))):
                                  pi = nbig()
                                  for c in range(8):
                                      MM(psb[pi][:], mT[:, c, t * 128:(t + 1) * 128], wv_[:, c, :], c == 0, c == 7, [r_mT, rw_], [r_ps[pi]])
                                  hs = slice(half * 512, (half + 1) * 512)
                                  V("dve", "scalar_tensor_tensor", [r_ps[pi], r_tin[i]], [r_tin[i]], out=tin[i][:, hs], in0=tin[i][:, hs],
                                    scalar=ALPHA, in1=psb[pi][:], op0=ALU.mult, op1=ALU.add)
                              layer_norm_tile((lst, r_lst), tin[i], r_tin[i], lnm_g, lnm_b, [r_lng, r_lnb], x1o[i][:], r_x1o[i], eng="dve")
                              if router:
                                  for e in range(NE):
                                      V("dve", "scalar_tensor_tensor", [r_x1o[i], r_wrB], [r_tin[i], r_lgR], out=tin[i][:], in0=x1o[i][:], scalar=1.0,
                                        in1=wrB[:, e, :], op0=ALU.mult, op1=ALU.mult, accum_out=lgR[:, r0 // 128, e:e + 1])
                              S.dma("pool", x1s[r0:r0 + 128, :], x1o[i][:], reads=[r_x1o[i]], writes=[r_x1s[gi]], sres=r_x1o[i], load=False)
            S.barrier()

        def phase_ffn(l, dst, r_dst, is_final):
            moe = (l % 2 == 1)
            with ExitStack() as st:
                set_ring(st, 6)
                lnf_g = sb(st, "lnf_g", [128, D], F32)
                lnf_b = sb(st, "lnf_b", [128, D], F32)
                r_lnf = S.res("lnf")
                S.dma("pool", lnf_g[:], ln_fg[l].partition_broadcast(128), writes=[r_lnf], sres=r_lnf)
                S.dma("pool", lnf_b[:], ln_fb[l].partition_broadcast(128), writes=[r_lnf], sres=r_lnf)
                xf = [sb(st, f"fxf{i}", [128, D], F32) for i in range(2)]
                r_xf = [S.res(f"fxf{i}") for i in range(2)]
                xb = [sb(st, f"fxb{i}", [128, D], BF16) for i in range(2)]
                r_xb = [S.res(f"fxb{i}") for i in range(2)]
                xT = sb(st, "fxT", [128, 8, G], BF16)
                r_xT = S.res("fxT")
                hT = sb(st, "hT", [128, 28, G], BF16)
                r_hT = [S.res(f"hT{i}") for i in range(7)]
                sgt = [sb(st, f"sgt{i}", [128, G], BF16) for i in range(2)]
                r_sgt = [S.res(f"sgt{i}") for i in range(2)]
                tin = [sb(st, f"ftin{i}", [128, D], F32) for i in range(4)]
                r_tin = [S.res(f"ftin{i}") for i in range(4)]
                yo = [sb(st, f"fyo{i}", [128, D], F32) for i in range(4)]
                r_yo = [S.res(f"fyo{i}") for i in range(4)]
                lst = sb(st, "flst", [128, 16], F32)
                r_lst = S.res("flst")
                if moe:
                    wr = sb(st, "wr", [128, NE, D], F32)
                    r_wr = S.res("wr")
                    for e in range(NE):
                        S.dma("pool", wr[:, e, :], m_rT[e].partition_broadcast(128), writes=[r_wr], sres=r_wr)
                    acc = sb(st, "macc", [128, 4, D], F32)
                    r_acc = [S.res(f"macc{t}") for t in range(4)]
                    lg = sb(st, "mlg", [128, 4, 32], F32)
                    r_lg = [S.res(f"mlg{t}") for t in range(4)]
                    junk = sb(st, "mjunk", [128, D], F32)
                    r_junk = S.res("mjunk")
                ring = {"big": 0, "x": 0, "sg": 0, "ln": 0}
                pbig = [0, 1, 2, 3]
                pdn = [4, 5]
                PTR = 6

                def nbig():
                    i = pbig[ring["big"] % len(pbig)]
                    ring["big"] += 1
                    return i

                for gi in range(T // G):
                    tok0 = gi * G
                    for t in range(4):
                        i = ring["x"] % 2
                        ring["x"] += 1
                        r0 = tok0 + t * 128
                        S.dma("pool", xf[i][:], x1s[r0:r0 + 128, :], reads=[r_x1s[gi]], writes=[r_xf[i]], sres=r_xf[i])
                        ACT(xb[i][:], xf[i][:], AF.Copy, [r_xf[i]], [r_xb[i]])
                        ptr = psb[PTR][:].bitcast(BF16).rearrange("p (c n) -> p c n", c=8)
                        for c in range(8):
                            TR(ptr[:, c, :], xb[i][:, c * 128:(c + 1) * 128], identb[:], [r_xb[i], r_ident], [r_ps[PTR]])
                        V("dve", "tensor_copy", [r_ps[PTR]], [r_xT], out=xT[:, :, t * 128:(t + 1) * 128], in_=ptr)
                        if moe:
                            for e in range(NE):
                                V("dve", "tensor_tensor", [r_xf[i], r_wr], [r_junk], out=junk[:], in0=xf[i][:],
                                  in1=wr[:, e, :], op=ALU.mult)
                                V("dve", "tensor_reduce", [r_junk], [r_lg[t]], out=lg[:, t, e:e + 1], in_=junk[:], axis=AX.X, op=ALU.add)
                            L = lg[:, t, 0:8]
                            m1 = lg[:, t, 8:9]
                            m2 = lg[:, t, 9:10]
                            E1 = lg[:, t, 10:18]
                            L2 = lg[:, t, 18:26]
                            V("dve", "tensor_reduce", [r_lg[t]], [r_lg[t]], out=m1, in_=L, axis=AX.X, op=ALU.max)
                            V("dve", "tensor_scalar", [r_lg[t]], [r_lg[t]], out=E1, in0=L, scalar1=m1, scalar2=None, op0=ALU.is_ge)
                            V("dve", "scalar_tensor_tensor", [r_lg[t]], [r_lg[t]], out=L2, in0=E1, scalar=-BIG, in1=L, op0=ALU.mult, op1=ALU.add)
                            V("dve", "tensor_reduce", [r_lg[t]], [r_lg[t]], out=m2, in_=L2, axis=AX.X, op=ALU.max)
                            V("dve", "tensor_scalar", [r_lg[t]], [r_lg[t]], out=E1, in0=L, scalar1=m2, scalar2=None, op0=ALU.is_ge)
                            V("dve", "tensor_scalar", [r_lg[t]], [r_lg[t]], out=L2, in0=L, scalar1=m1, scalar2=None, op0=ALU.subtract)
                            ACT(L2, L2, AF.Exp, [r_lg[t]], [r_lg[t]])
                            V("dve", "tensor_tensor", [r_lg[t]], [r_lg[t]], out=L2, in0=L2, in1=E1, op=ALU.mult)
                            V("dve", "tensor_reduce", [r_lg[t]], [r_lg[t]], out=lg[:, t, 26:27], in_=L2, axis=AX.X, op=ALU.add)
                            V("dve", "reciprocal", [r_lg[t]], [r_lg[t]], out=lg[:, t, 26:27], in_=lg[:, t, 26:27])
                            V("dve", "tensor_scalar", [r_lg[t]], [r_lg[t]], out=E1, in0=L2, scalar1=lg[:, t, 26:27], scalar2=None, op0=ALU.mult)

                    for e in range(NE if moe else 1):
                        kg = (lambda i: ("m", e, "g", i)) if moe else (lambda i: ("f", "g", i))
                        ku = (lambda i: ("m", e, "u", i)) if moe else (lambda i: ("f", "u", i))
                        kd = (lambda i: ("m", e, "d", i)) if moe else (lambda i: ("f", "d", i))
                        for bi in range(7):
                            wg_v, rwg = wload(kg(bi), "c8", alt=True)
                            wu_v, rwu = wload(ku(bi), "c8", alt=True)
                            for j in range(4):
                                fc = bi * 4 + j
                                js = slice(j * 128, (j + 1) * 128)
                                pg = nbig()
                                for c in range(8):
                                    MM(psb[pg][:], wg_v[:, c, js], xT[:, c, :], c == 0, c == 7, [r_xT, rwg], [r_ps[pg]])
                                k = ring["sg"] % 2
                                ring["sg"] += 1
                                ACT(sgt[k][:], psb[pg][:], AF.Silu, [r_ps[pg]], [r_sgt[k]])
                                pu = nbig()
                                for c in range(8):
                                    MM(psb[pu][:], wu_v[:, c, js], xT[:, c, :], c == 0, c == 7, [r_xT, rwu], [r_ps[pu]])
                                V("dve", "tensor_tensor", [r_ps[pu], r_sgt[k]], [r_hT[bi]], out=hT[:, fc, :], in0=psb[pu][:], in1=sgt[k][:], op=ALU.mult)
                        dblocks = []
                        for half in range(2):
                            accp = [nbig() for _ in range(4)]
                            for bi in range(7):
                                wd_v, rwd = wload(kd(bi), "c4", alt=True)
                                for t in range(4):
                                    for j in range(4):
                                        fc = bi * 4 + j
                                        MM(psb[accp[t]][:], hT[:, fc, t * 128:(t + 1) * 128], wd_v[:, j, half * 512:(half + 1) * 512],
                                           fc == 0, fc == 27, [r_hT[bi], rwd], [r_ps[accp[t]]])
                            hs = slice(half * 512, (half + 1) * 512)
                            for t in range(4):
                                if moe:
                                    cw = lg[:, t, 10 + e:11 + e]
                                    if e == 0:
                                        V("dve", "tensor_scalar", [r_ps[accp[t]], r_lg[t]], [r_acc[t]], out=acc[:, t, hs], in0=psb[accp[t]][:],
                                          scalar1=cw, scalar2=None, op0=ALU.mult)
                                    else:
                                        V("dve", "scalar_tensor_tensor", [r_ps[accp[t]], r_lg[t], r_acc[t]], [r_acc[t]], out=acc[:, t, hs],
                                          in0=psb[accp[t]][:], scalar=cw, in1=acc[:, t, hs], op0=ALU.mult, op1=ALU.add)
                                else:
                                    if half == 0:
                                        i = t
                                        r0 = tok0 + t * 128
                                        S.dma("pool", tin[i][:], x1s[r0:r0 + 128, :], reads=[r_x1s[gi]], writes=[r_tin[i]], sres=r_tin[i])
                                        dblocks.append(i)
                                    i = dblocks[t]
                                    V("dve", "scalar_tensor_tensor", [r_ps[accp[t]], r_tin[i]], [r_tin[i]], out=tin[i][:, hs], in0=tin[i][:, hs],
                                      scalar=ALPHA, in1=psb[accp[t]][:], op0=ALU.mult, op1=ALU.add)
                                    if half == 1:
                                        r0 = tok0 + t * 128
                                        layer_norm_tile((lst, r_lst), tin[i], r_tin[i], lnf_g, lnf_b, [r_lnf], yo[i][:], r_yo[i], eng="pool")
                                        o = S.dma("pool", dst[r0:r0 + 128, :], yo[i][:], reads=[r_yo[i]], writes=[r_dst[gi]], sres=r_yo[i], load=False)
                                        if is_final:
                                            final_tokens.append(o)
                    if moe:
                        for t in range(4):
                            i = t
                            r0 = tok0 + t * 128
                            S.dma("pool", tin[i][:], x1s[r0:r0 + 128, :], reads=[r_x1s[gi]], writes=[r_tin[i]], sres=r_tin[i])
                            V("dve", "scalar_tensor_tensor", [r_acc[t], r_tin[i]], [r_tin[i]], out=tin[i][:], in0=tin[i][:],
                              scalar=ALPHA, in1=acc[:, t, :], op0=ALU.mult, op1=ALU.add)
                            layer_norm_tile((lst, r_lst), tin[i], r_tin[i], lnf_g, lnf_b, [r_lnf], yo[i][:], r_yo[i], eng="pool")
                            o = S.dma("pool", dst[r0:r0 + 128, :], yo[i][:], reads=[r_yo[i]], writes=[r_dst[gi]], sres=r_yo[i], load=False)
                            if is_final:
                                final_tokens.append(o)
            S.barrier()

        def phase_moe(l, dst, r_dst, is_final):
            IOA = bass.IndirectOffsetOnAxis
            NT = T // 128
            wsc_rows = wsm.rearrange("b p n -> (b p) n")
            moe_blk0 = [r_wsc[tab[("m", e, "g", 0)]] for e in range(NE)]
            r_s2t = S.res("s2t")
            r_s2t_parts = []
            r_yg = [S.res(f"yg{j}") for j in range(NGRP)]
            PTR, PMISC = 6, 7
            with ExitStack() as so:
                set_ring(so, 6)
                lg = sb(so, "mlg", [128, NT, 32], F32)
                r_lg = S.res("mlg")
                selm = sb(so, "selm", [128, NT, NE], F32)
                hot1 = sb(so, "hot1", [128, NT, NE], F32)
                hot2 = sb(so, "hot2", [128, NT, NE], F32)
                rankg = sb(so, "rankg", [128, NT, NE], F32)
                r_rt = S.res("route")
                cwk = sb(so, "cwk", [128, NT, 2], F32)
                r_cwk = S.res("cwk")
                run = sb(so, "runc", [128, 48], F32)
                r_run = S.res("runc")
                widx = sb(so, "widx", [128, NGRP, 21], I32)
                r_widx = S.res("widx")
                sl0i = sb(so, "sl0i", [128, NT], I32)
                sl1i = sb(so, "sl1i", [128, NT], I32)
                r_sli = S.res("sli")
                tokc = sb(so, "tokc", [128, NT], I32)
                r_tokc = S.res("tokc")
                S.dma("pool", tokc[:], c_tok, writes=[r_tokc], sres=r_tokc)

                with ExitStack() as st:
                    if not cfg.do_mix:
                        wr = sb(st, "wr", [128, NE, D], F32)
                        r_wr = S.res("wr")
                        for e in range(NE):
                            S.dma("pool", wr[:, e, :], m_rT[e].partition_broadcast(128), writes=[r_wr], sres=r_wr)
                        xf = [sb(st, f"rxf{i}", [128, D], F32) for i in range(2)]
                        r_xf = [S.res(f"rxf{i}") for i in range(2)]
                        junk = sb(st, "rjunk", [128, D], F32)
                        r_junk = S.res("rjunk")
                    tris, r_tris = sb(st, "tris", [128, 128], F32), S.res("tris")
                    S.dma("pool", tris[:], c_tris, writes=[r_tris], sres=r_tris)
                    onesf, r_onesf = sb(st, "onesf", [128, 128], F32), S.res("onesf")
                    V("pool", "memset", [], [r_onesf], ap=onesf[:], constant=1.0)
                    thr, r_thr = sb(st, "thr", [128, 16], F32), S.res("thr")
                    S.dma("pool", thr[:], c_thr, writes=[r_thr], sres=r_thr)
                    jj, r_jj = sb(st, "jj", [128, NGRP], F32), S.res("jj")
                    S.dma("pool", jj[:], c_jj, writes=[r_jj], sres=r_jj)
                    wcst, r_wcst = sb(st, "wcst", [128, 21], F32), S.res("wcst")
                    S.dma("pool", wcst[:], c_wcst, writes=[r_wcst], sres=r_wcst)
                    cmp3 = sb(st, "cmp3", [128, NE, 16], F32)
                    ej = sb(st, "ej", [128, NGRP], F32)
                    widxf = sb(st, "widxf", [128, NGRP, 21], F32)
                    slotf = sb(st, "slotf", [128, NT, NE], F32)
                    stmp = sb(st, "rstmp", [128, NT, NE], F32)
                    s01f = sb(st, "s01f", [128, 2, NT], F32)
                    zi = sb(st, "zi", [128, NSLOT // 128], I32)
                    r_zi = S.res("zi")
                    V("pool", "memset", [], [r_zi], ap=zi[:], constant=0)
                    S.dma("pool", s2t.rearrange("(p n) o -> p (n o)", p=128), zi[:], reads=[r_zi], writes=[r_s2t], sres=r_s2t)
                    r_misc = S.res("rmisc")
                    V("dve", "memset", [], [r_run], ap=run[:], constant=0.0)

                    for i in range(NT):
                        b = i % 2
                        r0 = i * 128
                        if cfg.do_mix:
                            V("dve", "tensor_copy", [r_lgR], [r_lg], out=lg[:, i, 0:8], in_=lgR[:, i, :])
                        else:
                            S.dma("pool", xf[b][:], x1s[r0:r0 + 128, :], reads=[r_x1s[r0 // G]], writes=[r_xf[b]], sres=r_xf[b])
                            for e in range(NE):
                                V("dve", "scalar_tensor_tensor", [r_xf[b], r_wr], [r_junk, r_lg], out=junk[:], in0=xf[b][:], scalar=1.0, in1=wr[:, e, :],
                                  op0=ALU.mult, op1=ALU.mult, accum_out=lg[:, i, e:e + 1])
                        L = lg[:, i, 0:8]
                        m1 = lg[:, i, 8:9]
                        m2 = lg[:, i, 9:10]
                        L2 = lg[:, i, 18:26]
                        dd = lg[:, i, 26:27]
                        H1 = hot1[:, i, :]
                        H2 = hot2[:, i, :]
                        V("dve", "tensor_reduce", [r_lg], [r_lg], out=m1, in_=L, axis=AX.X, op=ALU.max)
                        V("dve", "tensor_scalar", [r_lg], [r_rt], out=H1, in0=L, scalar1=m1, scalar2=None, op0=ALU.is_ge)
                        V("dve", "scalar_tensor_tensor", [r_lg, r_rt], [r_lg], out=L2, in0=H1, scalar=-BIG, in1=L, op0=ALU.mult, op1=ALU.add)
                        V("dve", "tensor_reduce", [r_lg], [r_lg], out=m2, in_=L2, axis=AX.X, op=ALU.max)
                        V("dve", "tensor_scalar", [r_lg], [r_rt], out=H2, in0=L2, scalar1=m2, scalar2=None, op0=ALU.is_ge)
                        V("dve", "tensor_tensor", [r_rt], [r_rt], out=selm[:, i, :], in0=H1, in1=H2, op=ALU.add)
                        V("dve", "tensor_tensor", [r_lg], [r_lg], out=dd, in0=m2, in1=m1, op=ALU.subtract)
                        ACT(dd, dd, AF.Exp, [r_lg], [r_lg])
                        V("dve", "tensor_scalar_add", [r_lg], [r_cwk], out=cwk[:, i, 0:1], in0=dd, scalar1=1.0)
                        V("dve", "reciprocal", [r_cwk], [r_cwk], out=cwk[:, i, 0:1], in_=cwk[:, i, 0:1])
                        V("dve", "tensor_tensor", [r_cwk, r_lg], [r_cwk], out=cwk[:, i, 1:2], in0=cwk[:, i, 0:1], in1=dd, op=ALU.mult)
                        pr = psb[PMISC][:, 0:16]
                        MM(pr[:, 0:8], tris[:], selm[:, i, :], True, True, [r_tris, r_rt], [r_ps[PMISC]])
                        MM(pr[:, 8:16], onesf[:], selm[:, i, :], True, True, [r_onesf, r_rt], [r_ps[PMISC]])
                        V("dve", "tensor_tensor", [r_ps[PMISC], r_run], [r_rt], out=rankg[:, i, :], in0=pr[:, 0:8], in1=run[:, 0:8], op=ALU.add)
                        V("dve", "tensor_tensor", [r_ps[PMISC], r_run], [r_run], out=run[:, 0:8], in0=pr[:, 8:16], in1=run[:, 0:8], op=ALU.add)

                    cnt, ng, gend, base = run[:, 0:8], run[:, 8:16], run[:, 16:24], run[:, 24:32]
                    V("dve", "tensor_tensor", [r_run, r_thr], [r_misc], out=cmp3[:], in0=cnt.unsqueeze(2).broadcast_to([128, NE, 16]),
                      in1=thr[:].unsqueeze(1).broadcast_to([128, NE, 16]), op=ALU.is_gt)
                    V("dve", "tensor_reduce", [r_misc], [r_run], out=ng, in_=cmp3[:], axis=AX.X, op=ALU.add)
                    V("dve", "tensor_copy", [r_run], [r_run], out=gend[:, 0:1], in_=ng[:, 0:1])
                    for e in range(1, NE):
                        V("dve", "tensor_tensor", [r_run], [r_run], out=gend[:, e:e + 1], in0=gend[:, e - 1:e], in1=ng[:, e:e + 1], op=ALU.add)
                    V("dve", "tensor_tensor", [r_run], [r_run], out=base, in0=gend, in1=ng, op=ALU.subtract)
                    V("dve", "tensor_scalar_mul", [r_run], [r_run], out=base, in0=base, scalar1=512.0)
                    V("dve", "tensor_scalar", [r_jj, r_run], [r_misc], out=ej[:], in0=jj[:], scalar1=gend[:, 0:1], scalar2=None, op0=ALU.is_ge)
                    for e in range(1, NE):
                        V("dve", "scalar_tensor_tensor", [r_jj, r_run, r_misc], [r_misc], out=ej[:], in0=jj[:], scalar=gend[:, e:e + 1], in1=ej[:],
                          op0=ALU.is_ge, op1=ALU.add)
                    V("dve", "tensor_scalar_min", [r_misc], [r_misc], out=ej[:], in0=ej[:], scalar1=float(NE - 1))
                    V("dve", "scalar_tensor_tensor", [r_misc, r_wcst], [r_misc], out=widxf[:], in0=ej[:].unsqueeze(2).broadcast_to([128, NGRP, 21]),
                      scalar=float(21 * 128), in1=wcst[:].unsqueeze(1).broadcast_to([128, NGRP, 21]), op0=ALU.mult, op1=ALU.add)
                    V("dve", "tensor_copy", [r_misc], [r_widx], out=widx[:], in_=widxf[:])
                    V("dve", "tensor_tensor", [r_rt, r_run], [r_misc], out=slotf[:], in0=rankg[:], in1=base.unsqueeze(1).broadcast_to([128, NT, NE]), op=ALU.add)
                    for k, hot in enumerate((hot1, hot2)):
                        V("dve", "tensor_tensor", [r_misc, r_rt], [r_misc], out=stmp[:], in0=slotf[:], in1=hot[:], op=ALU.mult)
                        V("dve", "tensor_reduce", [r_misc], [r_misc], out=s01f[:, k, :], in_=stmp[:], axis=AX.X, op=ALU.add)
                    V("dve", "tensor_copy", [r_misc], [r_sli], out=sl0i[:], in_=s01f[:, 0, :])
                    V("dve", "tensor_copy", [r_misc], [r_sli], out=sl1i[:], in_=s01f[:, 1, :])
                    sc_st = S.new_stream("scat")
                    sc_st.q = "pool"
                    sc_ops = []
                    for i in range(NT):
                        for sli in (sl0i, sl1i):
                            rr = S.res("s2tw")
                            r_s2t_parts.append(rr)
                            sc_ops.append(S.dma("pool", None, None, reads=[r_sli, r_tokc, r_s2t], writes=[rr], sres=rr, stream=sc_st,
                                                fn=(lambda e, sli=sli, i=i: e.indirect_dma_start(out=s2t[:, :], out_offset=IOA(ap=sli[:, i:i + 1], axis=0),
                                                                                                  in_=tokc[:, i:i + 1], in_offset=None))))
                    for o_ in sc_ops:
                        o_.dval = sc_st.count * 16
                S.barrier()
                with ExitStack() as st:
                    tix = [sb(st, f"tix{i}", [128, 4], I32) for i in range(2)]
                    r_tix = [S.res(f"tix{i}") for i in range(2)]
                    xg = [[sb(st, f"xg{i}_{q}", [128, D], BF16) for q in range(4)] for i in range(2)]
                    r_xg = [[S.res(f"xg{i}_{q}") for q in range(4)] for i in range(2)]
                    xT = sb(st, "exT", [128, 8, G], BF16)
                    r_xT = S.res("exT")
                    hT = sb(st, "ehT", [128, 28, G], BF16)
                    r_hT = [S.res(f"ehT{i}") for i in range(7)]
                    sgt = [sb(st, f"esgt{i}", [128, G], BF16) for i in range(2)]
                    r_sgt = [S.res(f"esgt{i}") for i in range(2)]
                    yo = [sb(st, f"eyo{i}", [128, D], F32) for i in range(4)]
                    r_yo = [S.res(f"eyo{i}") for i in range(4)]
                    ring = {"big": 0, "sg": 0}
                    pbig = [0, 1, 2, 3]

                    def nbig():
                        i = pbig[ring["big"] % len(pbig)]
                        ring["big"] += 1
                        return i

                    def wload_dyn(j, k, view):
                        i = wstate["i"] % len(wring)
                        wstate["i"] += 1
                        S.dma("pool", None, None, reads=[r_widx] + moe_blk0, writes=[r_wring[i]], sres=r_wring[i],
                              fn=(lambda e, i=i, j=j, k=k: e.indirect_dma_start(out=wring[i][:], out_offset=None, in_=wsc_rows,
                                                                               in_offset=IOA(ap=widx[:, j, k:k + 1], axis=0))))
                        t = wring[i]
                        v = t[:].rearrange("p (c n) -> p c n", c=8 if view == "c8" else 4)
                        return v, r_wring[i]

                    def gather_group(j):
                        jb = j % 2
                        for q in range(4):
                            s0 = j * 512 + q * 128
                            S.dma("sp", tix[jb][:, q:q + 1], s2t[s0:s0 + 128, :], reads=[r_s2t] + r_s2t_parts, writes=[r_tix[jb]], sres=r_tix[jb])
                        for q in range(4):
                            S.dma("pool", None, None, reads=[r_tix[jb]] + r_x1s, writes=[r_xg[jb][q]], sres=r_xg[jb][q],
                                  fn=(lambda e, jb=jb, q=q: e.indirect_dma_start(out=xg[jb][q][:], out_offset=None, in_=x1s[:, :],
                                                                                 in_offset=IOA(ap=tix[jb][:, q:q + 1], axis=0))))

                    gather_group(0)
                    for j in range(NGRP):
                        jb = j % 2
                        if j + 1 < NGRP:
                            gather_group(j + 1)
                        for q in range(4):
                            PB = PTR if q % 2 == 0 else PMISC
                            ptr = psb[PB][:].bitcast(BF16).rearrange("p (c n) -> p c n", c=8)
                            for c in range(8):
                                TR(ptr[:, c, :], xg[jb][q][:, c * 128:(c + 1) * 128], identb[:], [r_xg[jb][q], r_ident], [r_ps[PB]])
                            V("dve", "tensor_copy", [r_ps[PB]], [r_xT], out=xT[:, :, q * 128:(q + 1) * 128], in_=ptr)
                        for bi in range(7):
                            wg_v, rwg = wload_dyn(j, bi, "c8")
                            wu_v, rwu = wload_dyn(j, 7 + bi, "c8")
                            for jn in range(4):
                                fc = bi * 4 + jn
                                js = slice(jn * 128, (jn + 1) * 128)
                                pg = nbig()
                                for c in range(8):
                                    MM(psb[pg][:], wg_v[:, c, js], xT[:, c, :], c == 0, c == 7, [r_xT, rwg], [r_ps[pg]])
                                k = ring["sg"] % 2
                                ring["sg"] += 1
                                ACT(sgt[k][:], psb[pg][:], AF.Silu, [r_ps[pg]], [r_sgt[k]])
                                pu = nbig()
                                for c in range(8):
                                    MM(psb[pu][:], wu_v[:, c, js], xT[:, c, :], c == 0, c == 7, [r_xT, rwu], [r_ps[pu]])
                                V("dve", "tensor_tensor", [r_ps[pu], r_sgt[k]], [r_hT[bi]], out=hT[:, fc, :], in0=psb[pu][:], in1=sgt[k][:], op=ALU.mult)
                        for half in range(2):
                            accp = [nbig() for _ in range(4)]
                            for bi in range(7):
                                wd_v, rwd = wload_dyn(j, 14 + bi, "c4")
                                for t in range(4):
                                    for jn in range(4):
                                        fc = bi * 4 + jn
                                        MM(psb[accp[t]][:], hT[:, fc, t * 128:(t + 1) * 128], wd_v[:, jn, half * 512:(half + 1) * 512],
                                           fc == 0, fc == 27, [r_hT[bi], rwd], [r_ps[accp[t]]])
                            hs = slice(half * 512, (half + 1) * 512)
                            for t in range(4):
                                ACT(yo[t][:, hs], psb[accp[t]][:], AF.Copy, [r_ps[accp[t]]], [r_yo[t]])
                                if half == 1:
                                    s0 = j * 512 + t * 128
                                    S.dma("sp", ygs[s0:s0 + 128, :], yo[t][:], reads=[r_yo[t]], writes=[r_yg[j]], sres=r_yo[t], load=False)
                S.barrier()
                with ExitStack() as st:
                    lnf_g = sb(st, "elnf_g", [128, D], F32)
                    lnf_b = sb(st, "elnf_b", [128, D], F32)
                    r_lnf = S.res("elnf")
                    S.dma("pool", lnf_g[:], ln_fg[l].partition_broadcast(128), writes=[r_lnf], sres=r_lnf)
                    S.dma("pool", lnf_b[:], ln_fb[l].partition_broadcast(128), writes=[r_lnf], sres=r_lnf)
                    y0 = [sb(st, f"oy0_{i}", [128, D], F32) for i in range(2)]
                    y1 = [sb(st, f"oy1_{i}", [128, D], F32) for i in range(2)]
                    r_y0 = [S.res(f"oy0_{i}") for i in range(2)]
                    r_y1 = [S.res(f"oy1_{i}") for i in range(2)]
                    tin = [sb(st, f"otin{i}", [128, D], F32) for i in range(2)]
                    r_tin = [S.res(f"otin{i}") for i in range(2)]
                    yo = [sb(st, f"oyo{i}", [128, D], F32) for i in range(2)]
                    r_yo = [S.res(f"oyo{i}") for i in range(2)]
                    lst = sb(st, "olst", [128, 16], F32)
                    r_lst = S.res("olst")
                    for i in range(NT):
                        b = i % 2
                        r0 = i * 128
                        gi = r0 // G
                        S.dma("sp", tin[b][:], x1s[r0:r0 + 128, :], reads=[r_x1s[gi]], writes=[r_tin[b]], sres=r_tin[b])
                        for yt, ryt, sli in ((y0, r_y0, sl0i), (y1, r_y1, sl1i)):
                            S.dma("pool", None, None, reads=[r_sli] + r_yg, writes=[ryt[b]], sres=ryt[b],
                                  fn=(lambda e, yt=yt, sli=sli, b=b, i=i: e.indirect_dma_start(out=yt[b][:], out_offset=None, in_=ygs[:, :],
                                                                                                in_offset=IOA(ap=sli[:, i:i + 1], axis=0))))
                        ACT(tin[b][:], tin[b][:], AF.Copy, [r_tin[b]], [r_tin[b]], scale=ALPHA)
                        V("dve", "scalar_tensor_tensor", [r_y0[b], r_cwk, r_tin[b]], [r_tin[b]], out=tin[b][:], in0=y0[b][:], scalar=cwk[:, i, 0:1],
                          in1=tin[b][:], op0=ALU.mult, op1=ALU.add)
                        V("dve", "scalar_tensor_tensor", [r_y1[b], r_cwk, r_tin[b]], [r_tin[b]], out=tin[b][:], in0=y1[b][:], scalar=cwk[:, i, 1:2],
                          in1=tin[b][:], op0=ALU.mult, op1=ALU.add)
                        layer_norm_tile((lst, r_lst), tin[b], r_tin[b], lnf_g, lnf_b, [r_lnf], yo[b][:], r_yo[b], eng="dve")
                        o = S.dma("sp", dst[r0:r0 + 128, :], yo[b][:], reads=[r_yo[b]], writes=[r_dst[gi]], sres=r_yo[b], load=False)
                        if is_final:
                            final_tokens.append(o)
            S.barrier()

        r_xin = [S.res(f"xin{i}") for i in range(T // G)]
        r_yout = [S.res(f"yout{i}") for i in range(T // G)]
        pend = []
        for n_, l in enumerate(cfg.layers):
            if n_ == 0 and cfg.do_mix:
                prepass_layer(l)
            else:
                pend.append(lambda l=l: prepass_layer(l))
            if l % 2 == 0:
                pend.append(prepass_ffn)
            else:
                for e in range(NE):
                    pend.append(lambda e=e: prepass_moe([e]))
        if not cfg.do_mix:
            while pend:
                pend.pop(0)()
        last = cfg.layers[-1]
        for l in cfg.layers:
            src, rsrc = (x_in, r_xin) if l == cfg.layers[0] else (xs1, r_xs1)
            if cfg.do_mix:
                hk, pend = pend, []
                phase_mixer(l, src, rsrc, hooks=hk, router=(l % 2 == 1 and cfg.do_ffn and cfg.sparse))
            if (not cfg.do_mix) or ("B" not in cfg.parts):
                for gi in range(T // G):
                    S.dma("pool", x1s[gi * G:(gi + 1) * G, :], src[gi * G:(gi + 1) * G, :], reads=[rsrc[gi]], writes=[r_x1s[gi]],
                          sres=r_x1s[gi])
            if not cfg.do_ffn:
                for gi in range(T // G):
                    o = S.dma("pool", y_out[gi * G:(gi + 1) * G, :], x1s[gi * G:(gi + 1) * G, :], reads=[r_x1s[gi]], writes=[r_yout[gi]],
                              sres=r_yout[gi])
                    final_tokens.append(o)
            if cfg.do_ffn:
                ph = phase_moe if (l % 2 == 1 and cfg.sparse) else phase_ffn
                if l == last:
                    ph(l, y_out, r_yout, True)
                else:
                    ph(l, xs1, r_xs1, False)
        stats = S.emit(final_tokens)
        print("instr/wait per engine:", stats, "sems:", S.nsem, flush=True)
    return nc


def host_consts(S_):
    c = {}
    c["c_ident"] = np.eye(128, dtype=np.float32)
    inv = np.power(np.float32(500000.0), -np.arange(0, 16, 2, dtype=np.float32) / np.float32(16)).astype(np.float32)
    ang = (np.arange(S_, dtype=np.float32)[:, None] * inv[None, :]).astype(np.float32)
    co, si = np.cos(ang).astype(np.float32), np.sin(ang).astype(np.float32)
    c["c_cc"] = np.concatenate([co, co], 1).astype(np.float32)
    c["c_ss"] = np.concatenate([-si, si], 1).astype(np.float32)
    p = np.arange(128)[:, None]
    f = np.arange(512)[None, :]
    c["c_cmb"] = np.stack([np.where(cl * 128 + p <= f, 0.0, NEG) for cl in range(4)]).astype(np.float32)
    tok = np.arange(S_)[None, :]
    c["c_oh"] = (tok // MBLK == np.arange(16)[:, None]).astype(np.float32)
    j = np.tile(np.arange(16), MH)[None, :]
    blk = np.arange(16)[:, None]
    pm = (j < blk).astype(np.float32)
    c["c_pm"] = pm
    c["c_pa"] = ((pm - 1.0) * BIG).astype(np.float32)
    c["c_own"] = (j == blk).astype(np.float32)
    s = np.arange(128)[:, None]
    t = np.arange(128)[None, :]
    c["c_u"] = np.where(s <= t, -1.0 / 16.0, 0.0).astype(np.float32)
    c["c_tri"] = (s <= t).astype(np.float32)
    c["c_tris"] = (s < t).astype(np.float32)
    return c


def host_consts_moe(T):
    c = {}
    ntt = T // 128
    ngrp = (2 * T + NE * 511) // 512
    p = np.arange(128)[:, None]
    c["c_tok"] = (np.arange(ntt)[None, :] * 128 + p).astype(np.int32)
    tab, _ = block_table()
    base_m = tab[("m", 0, "g", 0)]
    c["c_wcst"] = (np.arange(21)[None, :] * 128 + p).astype(np.float32)
    c["c_jj"] = np.broadcast_to(np.arange(ngrp, dtype=np.float32)[None, :], (128, ngrp)).copy()
    c["c_thr"] = np.broadcast_to((np.arange(16, dtype=np.float32) * 512.0)[None, :], (128, 16)).copy()
    return c


_CACHE = {}


def kernel(**inputs):
    ncores = 8
    S_ = 4096
    nseq = 2
    if "prog" not in _CACHE:
        _CACHE["prog"] = build_program(Cfg(S=S_, NSEQ=nseq))
    nc = _CACHE["prog"]
    x = np.ascontiguousarray(np.asarray(inputs["x"], dtype=np.float32))
    consts = host_consts(S_)
    consts.update(host_consts_moe(S_ * nseq))
    shared = {k: np.ascontiguousarray(np.asarray(v, dtype=np.float32)) for k, v in inputs.items() if k != "x"}
    shared["moe_w_router_T"] = np.ascontiguousarray(shared["moe_w_router"][0].T)
    in_maps = []
    for c in range(ncores):
        m = dict(shared)
        m.update(consts)
        m["x"] = x[c * nseq:(c + 1) * nseq].reshape(nseq * S_, D)
        in_maps.append(m)
    res = run_bass_kernel_spmd(nc, in_maps, core_ids=list(range(ncores)))
    out = np.concatenate([np.asarray(r["y"]).reshape(nseq, S_, D) for r in res.results], axis=0)
    return out.astype(np.float32)
```

```python
import math
from contextlib import ExitStack

import numpy as np
import concourse.bass as bass
import concourse.mybir as mybir
from concourse.bass_utils import run_bass_kernel_spmd

F32 = mybir.dt.float32
BF16 = mybir.dt.bfloat16
I32 = mybir.dt.int32
AF = mybir.ActivationFunctionType
ALU = mybir.AluOpType
AX = mybir.AxisListType

D = 1024
DEPTH = 2
MH, MD = 8, 64
MBLK = 256
GH, GK = 4, 128
FF = 3584
NE = 8
IN_COLS = 5648
ALPHA = (2 * DEPTH) ** 0.25
LN_EPS = 1e-5
G = 512
NEG = -240000.0
BIG = 1.0e30

ENGS = ("pe", "act", "dve", "pool", "sp")
EPOCH = 12000
DMA_EPOCH = 1500


class Res:
    __slots__ = ("name", "writer", "readers", "stream_w", "stream_r")

    def __init__(self, name):
        self.name = name
        self.writer = None
        self.readers = []
        self.stream_w = None
        self.stream_r = None


class Stream:
    __slots__ = ("sem", "count", "name", "q")

    def __init__(self, name):
        self.name = name
        self.sem = None
        self.count = 0
        self.q = None


class Op:
    __slots__ = ("eng", "fn", "deps", "needs_inc", "epoch", "val", "is_dma", "stream", "dval")

    def __init__(self, eng, fn):
        self.eng = eng
        self.fn = fn
        self.deps = []
        self.needs_inc = False
        self.epoch = 0
        self.val = 0
        self.is_dma = False
        self.stream = None
        self.dval = 0


class Sched:
    def __init__(self, nc, stack):
        self.nc = nc
        self.stack = stack
        self.ops = {e: [] for e in ENGS}
        self.nsem = 0
        self.all_res = []
        self.live_streams = []
        self.free_streams = {"pool": [], "sp": []}

    def res(self, name):
        r = Res(name)
        self.all_res.append(r)
        return r

    def _newsem(self, name):
        self.nsem += 1
        return self.stack.enter_context(self.nc.semaphore(f"{name[:40]}_{self.nsem}"))

    def _collect(self, op, reads, writes):
        deps = []
        for r in reads:
            if r.writer is not None:
                deps.append((r.writer, "raw"))
        for w in writes:
            if w.writer is not None:
                deps.append((w.writer, "waw"))
            for t in w.readers:
                deps.append((t, "war"))
        seen = set()
        for t, kind in deps:
            if t is op or id(t) in seen:
                continue
            if (not t.is_dma) and (not op.is_dma) and t.eng == op.eng:
                if op.eng == "pe":
                    continue
            seen.add(id(t))
            op.deps.append(t)
            if not t.is_dma:
                t.needs_inc = True
        for r in reads:
            if not op.is_dma:
                r.readers = [t for t in r.readers if t.is_dma or t.eng != op.eng]
            r.readers.append(op)
        for w in writes:
            w.writer = op
            w.readers = []

    def op(self, eng, fn, reads=(), writes=()):
        o = Op(eng, fn)
        self._collect(o, reads, writes)
        self.ops[eng].append(o)
        return o

    def new_stream(self, name):
        st = Stream(name)
        st.sem = self._newsem(name[:48])
        return st

    def dma(self, q, out, in_, reads=(), writes=(), sres=None, load=True, stream=None, fn=None):
        o = Op(q, fn if fn is not None else (lambda e: e.dma_start(out=out, in_=in_)))
        o.is_dma = True
        st = stream if stream is not None else (sres.stream_w if load else sres.stream_r)
        if stream is None and (st is None or st.count >= DMA_EPOCH or st.q != q):
            fl = self.free_streams[q]
            while fl and fl[-1].count >= DMA_EPOCH - 64:
                fl.pop()
            if fl:
                st = fl.pop()
            else:
                st = Stream(sres.name + ("_w" if load else "_r"))
                st.sem = self._newsem(st.name[:48])
                st.q = q
            self.live_streams.append(st)
            if load:
                sres.stream_w = st
            else:
                sres.stream_r = st
        st.count += 1
        o.stream = st
        o.dval = st.count * 16
        self._collect(o, reads, writes)
        self.ops[q].append(o)
        return o

    def barrier(self):
        allr = list(self.all_res)
        for e in ENGS:
            if e == "sp":
                continue
            self.op(e, lambda e_: e_.drain(), reads=(), writes=allr)
        for r in allr:
            r.stream_w = None
            r.stream_r = None
        for st in self.live_streams:
            self.free_streams[st.q].append(st)
        self.live_streams = []

    def emit(self, final_tokens=()):
        nc = self.nc
        esems = {}
        for e in ENGS:
            cnt = 0
            ep = 0
            for o in self.ops[e]:
                if o.is_dma or not o.needs_inc:
                    continue
                if cnt >= EPOCH:
                    ep += 1
                    cnt = 0
                cnt += 1
                o.epoch = ep
                o.val = cnt
                if (e, ep) not in esems:
                    esems[(e, ep)] = self._newsem(f"s_{e}_{ep}")
        stats = {e: [0, 0] for e in ENGS}

        def run(e):
            def body(eng):
                known = {}
                for o in self.ops[e]:
                    need = {}
                    for t in o.deps:
                        if t.is_dma:
                            sem, val = t.stream.sem, t.dval
                        else:
                            sem, val = esems[(t.eng, t.epoch)], t.val
                        k = id(sem)
                        if k not in need or need[k][1] < val:
                            need[k] = (sem, val)
                    for k, (sem, val) in need.items():
                        if known.get(k, 0) >= val:
                            continue
                        known[k] = val
                        eng.wait_ge(sem, val)
                        stats[e][1] += 1
                    ins = o.fn(eng)
                    stats[e][0] += 1
                    if o.is_dma:
                        ins.then_inc(o.stream.sem, 16)
                    elif o.needs_inc:
                        ins.then_inc(esems[(e, o.epoch)], 1)
                if e == "pool":
                    for t in final_tokens:
                        eng.wait_ge(t.stream.sem, t.dval)
            return body

        with nc.Block() as block:
            block.tensor(run("pe"))
            block.scalar(run("act"))
            block.vector(run("dve"))
            block.gpsimd(run("pool"))
            block.sync(run("sp"))
        return stats


INP = [("qm", 0), ("km", 512), ("vm", 1024), ("qg", 1536), ("kg", 2048), ("vg", 2560), ("rg", 3072),
       ("ga0", 3600), ("ga1", 4112), ("gb0", 4624), ("gb1", 5136)]
LG_OFF = 3584


def block_table():
    tab = {}
    n = 0

    def add(key):
        nonlocal n
        tab[key] = n
        n += 1

    for l in range(DEPTH):
        for name, _ in INP:
            add((l, name))
        add((l, "lg"))
        add((l, "wa"))
        add((l, "wb"))
        add((l, "wo0"))
        add((l, "wo1"))
    for i in range(7):
        add(("f", "g", i))
    for i in range(7):
        add(("f", "u", i))
    for i in range(7):
        add(("f", "d", i))
    for e in range(NE):
        for i in range(7):
            add(("m", e, "g", i))
        for i in range(7):
            add(("m", e, "u", i))
        for i in range(7):
            add(("m", e, "d", i))
    return tab, n


class Cfg:
    def __init__(self, S=4096, NSEQ=2, layers=(0, 1), do_mix=True, do_ffn=True, dbg=False, parts="AB", skip=()):
        self.S = S
        self.NSEQ = NSEQ
        self.T = S * NSEQ
        self.layers = layers
        self.do_mix = do_mix
        self.do_ffn = do_ffn
        self.dbg = dbg
        self.parts = parts
        self.sparse = True
        self.skip = set(skip)


def build_program(cfg):
    S_, NSEQ, T = cfg.S, cfg.NSEQ, cfg.T
    NG = S_ // G
    NTS = S_ // 128
    NBK = S_ // MBLK
    nc = bass.Bass("TRN2", target_bir_lowering=False)

    def din(name, shape, dt=F32):
        return nc.dram_tensor(name, list(shape), dt, kind="ExternalInput").ap()

    x_in = din("x", [T, D])
    w_in = din("w_in", [DEPTH, D, IN_COLS])
    w_up = din("w_gla_gate_up", [DEPTH, 16, 512])
    b_gg = din("b_gla_gate", [DEPTH, 512])
    gn_g = din("gla_norm_g", [DEPTH, 512])
    w_ba = din("w_branch_a", [DEPTH, 512, D])
    w_bb = din("w_branch_b", [DEPTH, 512, D])
    w_o = din("w_out", [DEPTH, D, D])
    ln_mg = din("ln_mix_g", [DEPTH, D])
    ln_mb = din("ln_mix_b", [DEPTH, D])
    f_g = din("ffn_w_gate", [1, D, FF])
    f_u = din("ffn_w_up", [1, D, FF])
    f_d = din("ffn_w_down", [1, FF, D])
    m_r = din("moe_w_router", [1, D, NE])
    m_g = din("moe_w_gate", [1, NE, D, FF])
    m_u = din("moe_w_up", [1, NE, D, FF])
    m_d = din("moe_w_down", [1, NE, FF, D])
    ln_fg = din("ln_ffn_g", [DEPTH, D])
    ln_fb = din("ln_ffn_b", [DEPTH, D])
    c_ident = din("c_ident", [128, 128])
    c_cc = din("c_cc", [S_, 16])
    c_ss = din("c_ss", [S_, 16])
    c_cmb = din("c_cmb", [4, 128, 512])
    c_oh = din("c_oh", [16, S_])
    c_pm = din("c_pm", [16, 128])
    c_pa = din("c_pa", [16, 128])
    c_own = din("c_own", [16, 128])
    c_u = din("c_u", [128, 128])
    c_tri = din("c_tri", [128, 128])
    m_rT = din("moe_w_router_T", [NE, D])
    NTT = T // 128
    NGRP = (2 * T + NE * 511) // 512
    NSLOT = NGRP * 512
    c_tok = din("c_tok", [128, NTT], I32)
    c_wcst = din("c_wcst", [128, 21])
    c_jj = din("c_jj", [128, NGRP])
    c_thr = din("c_thr", [128, 16])
    c_tris = din("c_tris", [128, 128])

    tab, NBLKS = block_table()
    BASE_M = tab[("m", 0, "g", 0)]
    wsc_d = nc.dram_tensor("wsc", [BASE_M, 128, 4096], BF16, kind="Internal").ap()
    wsm = nc.dram_tensor("wsm", [NE * 21, 128, 4096], BF16, kind="Internal").ap()

    class _W:
        def __getitem__(self, b):
            return wsc_d[b] if b < BASE_M else wsm[b - BASE_M]
    wsc = _W()
    x1s = nc.dram_tensor("x1s", [T, D], F32, kind="Internal").ap()
    xs1 = nc.dram_tensor("xs1", [T, D], F32, kind="Internal").ap()
    yas = nc.dram_tensor("yas", [T // G, 128, 4, G], BF16, kind="Internal").ap()
    s2t = nc.dram_tensor("s2t", [NSLOT, 1], I32, kind="Internal").ap()
    ygs = nc.dram_tensor("ygs", [NSLOT, D], F32, kind="Internal").ap()
    y_out = nc.dram_tensor("y", [T, D], F32, kind="ExternalOutput").ap()
    dbg_t = {}
    if cfg.dbg:
        NGT = T // G
        for nm, shp in (("ya", [NGT, 128, 4, 512]), ("ybT", [NGT, 128, 4, G]), ("yaT", [NGT, 128, 4, G]), ("QT", [NGT, 128, MH, G]),
                        ("mT", [NGT, 128, 8, G]), ("BT", [NGT, 16, MH, G])):
            dbg_t[nm] = nc.dram_tensor("dbg_" + nm, shp, BF16, kind="ExternalOutput").ap()

    with ExitStack() as top:
        S = Sched(nc, top)
        r_wsc = [S.res(f"wsc{i}") for i in range(NBLKS)]
        r_x1s = [S.res(f"x1s{i}") for i in range(T // G)]
        r_xs1 = [S.res(f"xs1{i}") for i in range(T // G)]
        r_yas = [S.res(f"yas{i}") for i in range(T // G)]
        final_tokens = []
        r_dbg = S.res("dbg")

        pre = {"st": None, "ops": []}

        def pre_begin(name):
            pre["st"] = S.new_stream(name)
            pre["ops"] = []

        def pre_end():
            for o in pre["ops"]:
                o.dval = pre["st"].count * 16

        def conv(key, src, view):
            b = tab[key]
            if view == "c8":
                dst = wsc[b].rearrange("p (c n) -> p c n", c=8)
                s = src.rearrange("(c p) n -> p c n", p=128)
            elif view == "c4":
                dst = wsc[b].rearrange("p (c n) -> p c n", c=4)
                s = src.rearrange("(c p) n -> p c n", p=128)
            elif view == "lg":
                dst = wsc[b][:, 0:128].rearrange("p (c n) -> p c n", c=8)
                s = src.rearrange("(c p) n -> p c n", p=128)
            pre["ops"].append(S.dma("pool", dst, s, writes=[r_wsc[b]], sres=r_wsc[b], load=True, stream=pre["st"]))

        def prepass_layer(l):
            pre_begin(f"pre_l{l}a")
            for name, off in INP[:3]:
                conv((l, name), w_in[l][:, off:off + 512], "c8")
            pre_end()
            pre_begin(f"pre_l{l}")
            for name, off in INP[3:]:
                conv((l, name), w_in[l][:, off:off + 512], "c8")
            conv((l, "lg"), w_in[l][:, LG_OFF:LG_OFF + 16], "lg")
            conv((l, "wa"), w_ba[l], "c4")
            conv((l, "wb"), w_bb[l], "c4")
            conv((l, "wo0"), w_o[l][:, 0:512], "c8")
            conv((l, "wo1"), w_o[l][:, 512:1024], "c8")
            pre_end()

        def prepass_ffn():
            pre_begin("pre_ffn")
            for i in range(7):
                conv(("f", "g", i), f_g[0][:, i * 512:(i + 1) * 512], "c8")
                conv(("f", "u", i), f_u[0][:, i * 512:(i + 1) * 512], "c8")
                conv(("f", "d", i), f_d[0][i * 512:(i + 1) * 512, :], "c4")
            pre_end()

        def prepass_moe(experts=range(NE)):
            for e in experts:
                pre_begin(f"pre_moe{e}")
                for i in range(7):
                    conv(("m", e, "g", i), m_g[0][e][:, i * 512:(i + 1) * 512], "c8")
                    conv(("m", e, "u", i), m_u[0][e][:, i * 512:(i + 1) * 512], "c8")
                    conv(("m", e, "d", i), m_d[0][e][i * 512:(i + 1) * 512, :], "c4")
                pre_end()

        sbn = {"n": 0}

        def sb(st, name, shape, dt):
            sbn["n"] += 1
            return st.enter_context(nc.sbuf_tensor(f"{name}_{sbn['n']}", list(shape), dt))

        psb = [top.enter_context(nc.psum_tensor(f"ps{i}", [128, 512], F32)) for i in range(8)]
        r_ps = [S.res(f"ps{i}") for i in range(8)]

        identb = sb(top, "identb", [128, 128], BF16)
        r_ident = S.res("ident")
        S.dma("pool", identb[:], c_ident, writes=[r_ident], sres=r_ident)
        lgR = sb(top, "lgR", [128, T // 128, NE], F32)
        r_lgR = S.res("lgR")
        identf = sb(top, "identf", [128, 128], F32)
        r_identf = S.res("identf")
        S.dma("pool", identf[:], c_ident, writes=[r_identf], sres=r_identf)

        wring = []
        r_wring = []
        wstate = {"i": 0}

        def set_ring(st, n):
            wring[:] = [sb(st, f"wring{i}", [128, 4096], BF16) for i in range(n)]
            r_wring[:] = [S.res(f"wring{i}") for i in range(n)]
            wstate["i"] = 0

        def wload(key, view):
            i = wstate["i"] % len(wring)
            wstate["i"] += 1
            b = tab[key]
            if view == "lg":
                S.dma("sp", wring[i][:, 0:128], wsc[b][:, 0:128], reads=[r_wsc[b]], writes=[r_wring[i]], sres=r_wring[i])
            else:
                S.dma("sp", wring[i][:], wsc[b], reads=[r_wsc[b]], writes=[r_wring[i]], sres=r_wring[i])
            t = wring[i]
            if view == "c8":
                v = t[:].rearrange("p (c n) -> p c n", c=8)
            elif view == "c4":
                v = t[:].rearrange("p (c n) -> p c n", c=4)
            else:
                v = t[:, 0:128].rearrange("p (c n) -> p c n", c=8)
            return v, r_wring[i]

        def MM(out, lhsT, rhs, start, stop, R, W, **kw):
            S.op("pe", lambda e: e.matmul(out, lhsT=lhsT, rhs=rhs, start=start, stop=stop, **kw), reads=R, writes=W)

        def TR(out, in_, ident, R, W):
            S.op("pe", lambda e: e.transpose(out=out, in_=in_, identity=ident), reads=R, writes=W)

        def ACT(out, in_, func, R, W, **kw):
            S.op("act", lambda e: e.activation(out=out, in_=in_, func=func, **kw), reads=R, writes=W)

        def V(eng, meth, R, W, **kw):
            S.op(eng, lambda e: getattr(e, meth)(**kw), reads=R, writes=W)

        def layer_norm_tile(st_tiles, tin, r_tin, gb, bb, r_gbl, out, r_out, eng="dve"):
            stt, r_st = st_tiles
            V("dve", "bn_stats", [r_tin], [r_st], out=stt[:, 0:6], in_=tin[:, 0:512])
            V("dve", "bn_stats", [r_tin], [r_st], out=stt[:, 6:12], in_=tin[:, 512:1024])
            V("dve", "bn_aggr", [r_st], [r_st], out=stt[:, 12:14], in_=stt[:, 0:12])
            V("dve", "tensor_scalar_add", [r_st], [r_st], out=stt[:, 13:14], in0=stt[:, 13:14], scalar1=LN_EPS)
            ACT(stt[:, 13:14], stt[:, 13:14], AF.Sqrt, [r_st], [r_st])
            V("dve", "reciprocal", [r_st], [r_st], out=stt[:, 13:14], in_=stt[:, 13:14])
            V("dve", "tensor_scalar", [r_tin, r_st], [r_tin], out=tin[:], in0=tin[:], scalar1=stt[:, 12:13],
              scalar2=stt[:, 13:14], op0=ALU.subtract, op1=ALU.mult)
            V(eng, "tensor_tensor", [r_tin] + r_gbl, [r_tin], out=tin[:], in0=tin[:], in1=gb[:], op=ALU.mult)
            V(eng, "tensor_tensor", [r_tin] + r_gbl, [r_out], out=out, in0=tin[:], in1=bb[:], op=ALU.add)

        def phase_mixer(l, x_src, r_xsrc, hooks=(), router=False):
            hooks = list(hooks)
            pbig = [0, 1]
            pst = [3, 4]
            PACCS, PTR, PMISC = (2, 5), 6, 7

            def cload(st, name, shape, dt, src, q="pool"):
                t = sb(st, name, shape, dt)
                r = S.res(name)
                S.dma(q, t[:], src, writes=[r], sres=r)
                return t, r

            with ExitStack() as st:
              if "A" in cfg.parts:
                  set_ring(st, 4)
                  cc, r_cc = cload(st, "cc", [128, NTS, 16], F32, c_cc.rearrange("(t p) k -> p t k", p=128))
                  ss, r_ss = cload(st, "ss", [128, NTS, 16], F32, c_ss.rearrange("(t p) k -> p t k", p=128))
                  cmb, r_cmb = cload(st, "cmb", [128, 4, 512], BF16, c_cmb.rearrange("c p n -> p c n"))
                  oh, r_oh = cload(st, "oh", [16, S_], BF16, c_oh)
                  onesb = sb(st, "onesb", [128, 1], BF16)
                  r_ones = S.res("onesb")
                  V("pool", "memset", [], [r_ones], ap=onesb[:], constant=1.0)
                  zerob = sb(st, "zerob", [128, 512], BF16)
                  r_zero = S.res("zerob")
                  V("pool", "memset", [], [r_zero], ap=zerob[:], constant=0.0)

                  KT = sb(st, "KT", [128, 4, S_], BF16)
                  r_KT = [S.res(f"KT{g}") for g in range(NG)]
                  VA = sb(st, "VA", [128, NTS, MH, MD + 1], BF16)
                  r_VA = [S.res(f"VA{g}") for g in range(NG)]
                  V("pool", "memset", [], r_VA, ap=VA[:, :, :, MD:MD + 1], constant=1.0)
                  kmT = sb(st, "kmT", [128, 4, 16], BF16)
                  r_kmT = S.res("kmT")

                  NXB = 2
                  xf = [sb(st, f"xf{i}", [128, D], F32) for i in range(NXB)]
                  r_xf = [S.res(f"xf{i}") for i in range(NXB)]
                  xb = [sb(st, f"xb{i}", [128, D], BF16) for i in range(NXB)]
                  r_xb = [S.res(f"xb{i}") for i in range(NXB)]
                  xT = sb(st, "xT", [128, 8, G], BF16)
                  r_xT = S.res("xT")
                  QA = sb(st, "QA", [128, 4, 512], BF16)
                  r_QA = [S.res(f"QA{t}") for t in range(4)]
                  KA = sb(st, "KA", [128, 4, 512], BF16)
                  r_KA = [S.res(f"KA{t}") for t in range(4)]
                  QTs = [sb(st, f"QT{i}", [128, MH, G], BF16) for i in range(2)]
                  r_QTs = [S.res(f"QT{i}") for i in range(2)]
                  for i in range(2):
                      V("pool", "memset", [], [r_QTs[i]], ap=QTs[i][:], constant=0.0)
                  BTs = [sb(st, f"BT{i}", [16, MH, G], BF16) for i in range(2)]
                  r_BTs = [S.res(f"BT{i}") for i in range(2)]
                  gsb = sb(st, "gsb", [128, 6, 128], F32)
                  r_gsb = S.res("gsb")
                  gm = sb(st, "gm", [128, 3, 8], F32)
                  ksum = sb(st, "ksum", [128, 16], F32)
                  r_gm = S.res("gm")
                  bias_tm = sb(st, "bias_tm", [128, 128], BF16)
                  r_btm = S.res("bias_tm")
                  pmt = sb(st, "pmt", [128, 2, 3, 128], F32)
                  r_pmt = S.res("pmt")
                  rp = sb(st, "rp", [128, 2, 8, 16], F32)
                  r_rp = S.res("rp")
                  NPT = 4
                  PT = [sb(st, f"PT{i}", [128, G], BF16) for i in range(NPT)]
                  r_PT = [S.res(f"PT{i}") for i in range(NPT)]
                  rcp = sb(st, "rcp", [128, 2, 4], F32)
                  r_rcp = [S.res("rcp0"), S.res("rcp1")]
                  ya = sb(st, "ya", [128, 4, 512], BF16)
                  r_ya = S.res("ya")
                  yaT = [sb(st, f"yaT{i}", [128, 4, G], BF16) for i in range(2)]
                  r_yaT = [S.res(f"yaT{i}") for i in range(2)]
                  ring = {"big": 0, "st": 0, "pt": 0, "x": 0}

                  def nbig():
                      i = pbig[ring["big"] % len(pbig)]
                      ring["big"] += 1
                      return i

                  def prelude(s, g):
                      QTb, r_QTb, BTb, r_BTb = QTs[g % 2], r_QTs[g % 2], BTs[g % 2], r_BTs[g % 2]
                      tok0 = s * S_ + g * G
                      gi = tok0 // G
                      for t in range(4):
                          i = ring["x"] % NXB
                          ring["x"] += 1
                          r0 = tok0 + t * 128
                          S.dma("sp", xf[i][:], x_src[r0:r0 + 128, :], reads=[r_xsrc[gi]], writes=[r_xf[i]], sres=r_xf[i])
                          ACT(xb[i][:], xf[i][:], AF.Copy, [r_xf[i]], [r_xb[i]])
                          ptr = psb[PTR][:].bitcast(BF16).rearrange("p (c n) -> p c n", c=8)
                          for c in range(8):
                              TR(ptr[:, c, :], xb[i][:, c * 128:(c + 1) * 128], identb[:], [r_xb[i], r_ident], [r_ps[PTR]])
                          V("dve", "tensor_copy", [r_ps[PTR]], [r_xT], out=xT[:, :, t * 128:(t + 1) * 128], in_=ptr)

                      def proj_tm(key, consume):
                          wv, rw = wload((l, key), "c8")
                          for t in range(4):
                              pi = nbig()
                              for c in range(8):
                                  MM(psb[pi][:], xT[:, c, t * 128:(t + 1) * 128], wv[:, c, :], c == 0, c == 7,
                                     [r_xT, rw], [r_ps[pi]])
                              consume(t, psb[pi], r_ps[pi])

                      def rope_to(dst, r_dst):
                          def consume(t, ps, rps):
                              tt = g * 4 + t
                              p3 = ps[:].rearrange("p (h d) -> p h d", h=MH)
                              d3 = dst[:, t, :].rearrange("p (h d) -> p h d", h=MH)
                              ccb = cc[:, tt, :].unsqueeze(1).broadcast_to([128, MH, 16])
                              ssb = ss[:, tt, :].unsqueeze(1).broadcast_to([128, MH, 16])
                              V("dve", "tensor_tensor", [rps, r_cc], [r_rp], out=rp[:, 0, :, :], in0=p3[:, :, 0:16], in1=ccb, op=ALU.mult)
                              V("dve", "tensor_tensor", [rps, r_ss], [r_rp], out=rp[:, 1, :, 0:8], in0=p3[:, :, 8:16], in1=ssb[:, :, 0:8], op=ALU.mult)
                              V("dve", "tensor_tensor", [rps, r_ss], [r_rp], out=rp[:, 1, :, 8:16], in0=p3[:, :, 0:8], in1=ssb[:, :, 8:16], op=ALU.mult)
                              V("dve", "tensor_tensor", [r_rp], [r_dst[t]], out=d3[:, :, 0:16], in0=rp[:, 0, :, :], in1=rp[:, 1, :, :], op=ALU.add)
                              ACT(d3[:, :, 16:64], p3[:, :, 16:64], AF.Copy, [rps], [r_dst[t]])
                          return consume

                      proj_tm("qm", rope_to(QA, r_QA))
                      proj_tm("km", rope_to(KA, r_KA))

                      def cons_v(t, ps, rps):
                          tt = g * 4 + t
                          ACT(VA[:, tt, :, 0:MD], ps[:].rearrange("p (h d) -> p h d", h=MH), AF.Copy, [rps], [r_VA[g]])
                      proj_tm("vm", cons_v)

                      yield
                      ptr4 = psb[PTR][:].bitcast(BF16).rearrange("p (c n) -> p c n", c=8)
                      for t in range(4):
                          tt = g * 4 + t
                          for pr in range(4):
                              TR(ptr4[:, pr, :], KA[:, t, pr * 128:(pr + 1) * 128], identb[:], [r_KA[t], r_ident], [r_ps[PTR]])
                          V("dve", "tensor_copy", [r_ps[PTR]], [r_KT[g]], out=KT[:, :, tt * 128:(tt + 1) * 128], in_=ptr4[:, 0:4, :])
                      pm3 = psb[PMISC][:, 0:16].rearrange("p (a b) -> p a b", a=4)
                      for t in range(4):
                          for pr in range(4):
                              if "ksum" in cfg.skip:
                                  continue
                              MM(pm3[:, pr, t:t + 1], KA[:, t, pr * 128:(pr + 1) * 128], onesb[:, 0:1], True, True,
                                 [r_KA[t], r_ones], [r_ps[PMISC]])
                      ks3 = ksum[:].rearrange("p (a b) -> p a b", a=4)
                      if "ksum" in cfg.skip:
                          V("dve", "memset", [], [r_gm], ap=ksum[:], constant=0.0)
                      else:
                          V("dve", "tensor_copy", [r_ps[PMISC]], [r_gm], out=ks3, in_=pm3)
                      for bb_ in range(2):
                          blk = g * 2 + bb_
                          V("dve", "tensor_tensor", [r_gm], [r_gm], out=gm[:, 0, 0:4], in0=ks3[:, :, 2 * bb_],
                            in1=ks3[:, :, 2 * bb_ + 1], op=ALU.add)
                          V("dve", "tensor_scalar_mul", [r_gm], [r_kmT], out=kmT[:, :, blk], in0=gm[:, 0, 0:4], scalar1=1.0 / MBLK)

                      for t in range(4):
                          for pr in range(4):
                              TR(ptr4[:, pr, :], QA[:, t, pr * 128:(pr + 1) * 128], identb[:], [r_QA[t], r_ident], [r_ps[PTR]])
                          QT4 = QTb[:].rearrange("p (a b) n -> p a b n", b=2)
                          V("dve", "tensor_copy", [r_ps[PTR]], [r_QTb], out=QT4[0:64, :, 0, t * 128:(t + 1) * 128], in_=ptr4[0:64, 0:4, :])
                          V("dve", "tensor_copy", [r_ps[PTR]], [r_QTb], out=QT4[64:128, :, 1, t * 128:(t + 1) * 128], in_=ptr4[64:128, 0:4, :])

                      for bb_ in range(2):
                          blk = g * 2 + bb_
                          S.dma("sp", pmt[:, bb_, 0, :], c_pm[blk].partition_broadcast(128), writes=[r_pmt], sres=r_pmt)
                          S.dma("sp", pmt[:, bb_, 1, :], c_pa[blk].partition_broadcast(128), writes=[r_pmt], sres=r_pmt)
                          S.dma("sp", pmt[:, bb_, 2, :], c_own[blk].partition_broadcast(128), writes=[r_pmt], sres=r_pmt)

                      yield
                      if "gate" in cfg.skip:
                          V("dve", "memset", [], [r_BTb], ap=BTb[:], constant=0.0)
                      for t in range(4 if "gate" not in cfg.skip else 0):
                          if t == 2:
                              yield
                          bb_ = t // 2
                          pg = psb[PMISC][:, 128:256]
                          for h in range(MH):
                              pr, hh = h // 2, h % 2
                              MM(pg[:, h * 16:(h + 1) * 16], QTb[:, h, t * 128:(t + 1) * 128],
                                 kmT[:, pr, :], True, True, [r_QTb, r_kmT], [r_ps[PMISC]])
                          g0 = gsb[:, 0, :]
                          g1 = gsb[:, 1, :]
                          e1 = gsb[:, 2, :]
                          V("dve", "tensor_tensor", [r_ps[PMISC], r_pmt], [r_gsb], out=g0, in0=pg, in1=pmt[:, bb_, 0, :], op=ALU.mult)
                          V("dve", "tensor_tensor", [r_gsb, r_pmt], [r_gsb], out=g0, in0=g0, in1=pmt[:, bb_, 1, :], op=ALU.add)
                          g03 = g0.rearrange("p (h j) -> p h j", h=MH)
                          g13 = g1.rearrange("p (h j) -> p h j", h=MH)
                          e13 = e1.rearrange("p (h j) -> p h j", h=MH)
                          src3 = g03
                          for k in range(3):
                              V("dve", "tensor_reduce", [r_gsb], [r_gm], out=gm[:, k, :], in_=src3, axis=AX.X, op=ALU.max)
                              if k == 2:
                                  break
                              mb = gm[:, k, :].unsqueeze(2).broadcast_to([128, MH, 16])
                              V("dve", "tensor_tensor", [r_gsb, r_gm], [r_gsb], out=e13, in0=src3, in1=mb, op=ALU.is_ge)
                              V("dve", "scalar_tensor_tensor", [r_gsb], [r_gsb], out=g13, in0=e13, scalar=-BIG, in1=src3,
                                op0=ALU.mult, op1=ALU.add)
                              src3 = g13
                          mb = gm[:, 2, :].unsqueeze(2).broadcast_to([128, MH, 16])
                          V("dve", "tensor_tensor", [r_gsb, r_gm], [r_gsb], out=e13, in0=g03, in1=mb, op=ALU.is_ge)
                          V("dve", "tensor_tensor", [r_gsb, r_pmt], [r_gsb], out=e1, in0=e1, in1=pmt[:, bb_, 0, :], op=ALU.mult)
                          V("dve", "tensor_tensor", [r_gsb, r_pmt], [r_gsb], out=e1, in0=e1, in1=pmt[:, bb_, 2, :], op=ALU.add)
                          V("dve", "tensor_scalar", [r_gsb], [r_btm], out=bias_tm[:], in0=e1, scalar1=-1.0, scalar2=-NEG,
                            op0=ALU.add, op1=ALU.mult)
                          pb = psb[PTR][:].bitcast(BF16)[0:16, :].rearrange("p (h n) -> p h n", h=MH)
                          for h in range(MH):
                              TR(pb[:, h, :], bias_tm[:, h * 16:(h + 1) * 16], identb[:], [r_btm, r_ident], [r_ps[PTR]])
                          V("dve", "tensor_copy", [r_ps[PTR]], [r_BTb], out=BTb[:, :, t * 128:(t + 1) * 128], in_=pb)


                  def attend(s, g, pre):
                      QTb, r_QTb, BTb, r_BTb = QTs[g % 2], r_QTs[g % 2], BTs[g % 2], r_BTs[g % 2]
                      tok0 = s * S_ + g * G
                      gi = tok0 // G
                      ptr4 = psb[PTR][:].bitcast(BF16).rearrange("p (c n) -> p c n", c=8)
                      nch = 4 * (g + 1)
                      if "attn" in cfg.skip:
                          V("dve", "memset", [], [r_ya], ap=ya[:], constant=0.0)
                      items = [(h, c) for h in range(MH if "attn" not in cfg.skip else 0) for c in range(nch)]
                      PIPE = 2
                      pst4 = [3, 4, 7]
                      slots = {}

                      def emit_st(h, c):
                          pr = h // 2
                          gk = c // 4
                          si = pst4[ring["st"] % 3]
                          ring["st"] += 1
                          own_grp = (gk == g)
                          MM(psb[si][:], KT[:, pr, c * 128:(c + 1) * 128], QTb[:, h, :],
                             True, False, [r_KT[gk], r_QTb], [r_ps[si]])
                          MM(psb[si][:], oh[:, c * 128:(c + 1) * 128], BTb[:, h, :], False, not own_grp,
                             [r_oh, r_BTb], [r_ps[si]])
                          if own_grp:
                              MM(psb[si][:], identb[:], cmb[:, c % 4, :], False, True, [r_ident, r_cmb], [r_ps[si]])
                          pi = ring["pt"] % NPT
                          ring["pt"] += 1
                          ACT(PT[pi][:], psb[si][:], AF.Exp, [r_ps[si]], [r_PT[pi]], scale=1.0 / 8.0)
                          slots[(h, c)] = pi

                      def emit_pv(h, c):
                          gk = c // 4
                          a_i = h % 2
                          PACC = PACCS[a_i]
                          acc = psb[PACC][:, 0:260].rearrange("p (q d) -> p q d", q=4)
                          if c == 0:
                              MM(psb[PACC][:, 0:260], zerob[0:1, 0:128], zerob[0:1, 0:260], True, False,
                                 [r_zero], [r_ps[PACC]], skip_group_check=True)
                          pi = slots.pop((h, c))
                          for qs in range(4):
                              MM(acc[:, qs, :], PT[pi][:, qs * 128:(qs + 1) * 128], VA[:, c, h, :], False, c == nch - 1,
                                 [r_PT[pi], r_VA[gk]], [r_ps[PACC]], skip_group_check=True)
                          if c == nch - 1:
                              V("dve", "reciprocal", [r_ps[PACC]], [r_rcp[a_i]], out=rcp[:, a_i, :], in_=acc[:, :, MD])
                              V("dve", "tensor_tensor", [r_ps[PACC], r_rcp[a_i]], [r_ya], out=ya[:, :, h * MD:(h + 1) * MD],
                                in0=acc[:, :, 0:MD], in1=rcp[:, a_i, :].unsqueeze(2).broadcast_to([128, 4, MD]), op=ALU.mult)

                      pull = {0, len(items) // 4, len(items) // 2, (3 * len(items)) // 4}
                      for idx in range(len(items) + PIPE):
                          if pre is not None and idx in pull:
                              next(pre, None)
                          if idx < len(items):
                              emit_st(*items[idx])
                          if idx >= PIPE:
                              emit_pv(*items[idx - PIPE])
                      yi = gi % 2
                      for t in range(4):
                          for kc in range(4):
                              TR(ptr4[:, kc, :], ya[:, t, kc * 128:(kc + 1) * 128], identb[:], [r_ya, r_ident], [r_ps[PTR]])
                          V("dve", "tensor_copy", [r_ps[PTR]], [r_yaT[yi]], out=yaT[yi][:, :, t * 128:(t + 1) * 128], in_=ptr4[:, 0:4, :])
                      S.dma("sp", yas[gi], yaT[yi][:], reads=[r_yaT[yi]], writes=[r_yas[gi]], sres=r_yaT[yi], load=False)
                      if hooks:
                          hooks.pop(0)()
                      if cfg.dbg:
                          for nm, tl, rr in (("ya", ya, r_ya), ("yaT", yaT[yi], r_yaT[yi]), ("QT", QTb, r_QTb)):
                              final_tokens.append(S.dma("pool", dbg_t[nm][gi], tl[:], reads=[rr], writes=[r_dbg], sres=r_dbg))
                          final_tokens.append(S.dma("pool", dbg_t["BT"][gi], BTb[:], reads=[r_BTb], writes=[r_dbg], sres=r_dbg))
                      if pre is not None:
                          for _ in pre:
                              pass

                  for s in range(NSEQ):
                      V("pool", "memset", [], [r_kmT], ap=kmT[:], constant=0.0)
                      for _ in prelude(s, 0):
                          pass
                      for g in range(NG):
                          attend(s, g, prelude(s, g + 1) if g + 1 < NG else None)
            while hooks:
                hooks.pop(0)()
            S.barrier()

            with ExitStack() as st:
              if "B" in cfg.parts:
                  set_ring(st, 6)
                  uu, r_uu = cload(st, "uu_B", [128, 128], F32, c_u)
                  tri, r_tri = cload(st, "tri_B", [128, 128], F32, c_tri)
                  lnm_g, r_lng = cload(st, "lnm_g_B", [128, D], F32, ln_mg[l].partition_broadcast(128))
                  lnm_b, r_lnb = cload(st, "lnm_b_B", [128, D], F32, ln_mb[l].partition_broadcast(128))
                  gnt, r_gnt = cload(st, "gnt_B", [128, 512], F32, gn_g[l].partition_broadcast(128))
                  wupa = sb(st, "wupa_B", [32, 512], BF16)
                  r_wupa = S.res("wupa")
                  V("pool", "memset", [], [r_wupa], ap=wupa[:], constant=0.0)
                  S.dma("pool", wupa[0:16, :], w_up[l], reads=[], writes=[r_wupa], sres=r_wupa)
                  S.dma("pool", wupa[16:17, :], b_gg[l].unsqueeze(0), reads=[], writes=[r_wupa], sres=r_wupa)

                  stf = sb(st, "stf_B", [128, GH, 128], F32)
                  stb = sb(st, "stb_B", [128, GH, 128], BF16)
                  r_stf = [S.res(f"stf{h}") for h in range(GH)]
                  r_stb = [S.res(f"stb{h}") for h in range(GH)]
                  lgT = sb(st, "lgT_B", [32, G], BF16)
                  r_lgT = S.res("lgT")
                  V("pool", "memset", [], [r_lgT], ap=lgT[:], constant=1.0)

                  NXB = 2
                  xf = [sb(st, f"xf_B{i}", [128, D], F32) for i in range(NXB)]
                  r_xf = [S.res(f"xf{i}") for i in range(NXB)]
                  xb = [sb(st, f"xb_B{i}", [128, D], BF16) for i in range(NXB)]
                  r_xb = [S.res(f"xb{i}") for i in range(NXB)]
                  xT = sb(st, "xT_B", [128, 8, G], BF16)
                  r_xT = S.res("xT")
                  yaT = sb(st, "yaTb_B", [128, 4, G], BF16)
                  r_yaT = S.res("yaTb")
                  ybT = sb(st, "ybT_B", [128, 4, G], BF16)
                  r_ybT = S.res("ybT")
                  spt = sb(st, "spt_B", [128, 4, 512], F32)
                  r_spt = [S.res(f"spt{t}") for t in range(4)]
                  eq = sb(st, "eq_B", [128, GH, G], F32)
                  ek = sb(st, "ek_B", [128, GH, G], F32)
                  r_eq = [S.res(f"eq{h}") for h in range(GH)]
                  r_ek = [S.res(f"ek{h}") for h in range(GH)]
                  qdT = sb(st, "qdT_B", [128, GH, G], BF16)
                  kiT = sb(st, "kiT_B", [128, GH, G], BF16)
                  r_qdT = [S.res(f"qdT{h}") for h in range(GH)]
                  r_kiT = [S.res(f"kiT{h}") for h in range(GH)]
                  kiM = sb(st, "kiM_B", [128, 2, GH, 128], BF16)
                  r_kiM = [S.res("kiM0"), S.res("kiM1")]
                  vg = sb(st, "vg_B", [128, 4, 512], BF16)
                  r_vg = [S.res(f"vg{t}") for t in range(4)]
                  rgs = sb(st, "rgs_B", [128, 4, 512], BF16)
                  r_rgs = [S.res(f"rgs{t}") for t in range(4)]
                  attm = sb(st, "attm_B", [128, 2, GH, 128], BF16)
                  r_attm = [S.res("attm0"), S.res("attm1")]
                  stmp = sb(st, "stmp_B", [128, 2, GH, 128], F32)
                  r_stmp = [S.res("stmp0"), S.res("stmp1")]
                  hn = sb(st, "hn_B", [128, 40], F32)
                  r_hn = S.res("hn")
                  ob = sb(st, "ob_B", [128, 512], F32)
                  r_ob = S.res("ob")
                  yb = sb(st, "yb_B", [128, 512], BF16)
                  r_yb = S.res("yb")
                  sg = sb(st, "sg_B", [128, 2, G], F32)
                  r_sg = [S.res("sg0"), S.res("sg1")]
                  mT = sb(st, "mT_B", [128, 8, G], BF16)
                  r_mT = S.res("mT")
                  tin = [sb(st, f"tin_B{i}", [128, D], F32) for i in range(2)]
                  r_tin = [S.res(f"tin{i}") for i in range(2)]
                  x1o = [sb(st, f"x1o_B{i}", [128, D], F32) for i in range(2)]
                  r_x1o = [S.res(f"x1o{i}") for i in range(2)]
                  lst = sb(st, "lst_B", [128, 16], F32)
                  r_lst = S.res("lst")
                  if router:
                      wrB = sb(st, "wrB", [128, NE, D], F32)
                      r_wrB = S.res("wrB")
                      for e in range(NE):
                          S.dma("pool", wrB[:, e, :], m_rT[e].partition_broadcast(128), writes=[r_wrB], sres=r_wrB)
                  ring = {"big": 0, "x": 0, "ln": 0}

                  pbigB = [0, 1, 2, 4, 5]

                  def nbig():
                      i = pbigB[ring["big"] % len(pbigB)]
                      ring["big"] += 1
                      return i

                  for s in range(NSEQ):
                      V("pool", "memset", [], [r_stf[0]], ap=stf[:], constant=0.0)
                      V("pool", "memset", [], [r_stb[0]], ap=stb[:], constant=0.0)
                      for g in range(NG):
                          tok0 = s * S_ + g * G
                          gi = tok0 // G
                          for t in range(4):
                              i = ring["x"] % NXB
                              ring["x"] += 1
                              r0 = tok0 + t * 128
                              S.dma("pool", xf[i][:], x_src[r0:r0 + 128, :], reads=[r_xsrc[gi]], writes=[r_xf[i]], sres=r_xf[i])
                              ACT(xb[i][:], xf[i][:], AF.Copy, [r_xf[i]], [r_xb[i]])
                              ptr = psb[PTR][:].bitcast(BF16).rearrange("p (c n) -> p c n", c=8)
                              for c in range(8):
                                  TR(ptr[:, c, :], xb[i][:, c * 128:(c + 1) * 128], identb[:], [r_xb[i], r_ident], [r_ps[PTR]])
                              V("dve", "tensor_copy", [r_ps[PTR]], [r_xT], out=xT[:, :, t * 128:(t + 1) * 128], in_=ptr)
                          S.dma("pool", yaT[:], yas[gi], reads=[r_yas[gi]], writes=[r_yaT], sres=r_yaT)
                          ptr4 = psb[PTR][:].bitcast(BF16).rearrange("p (c n) -> p c n", c=8)

                          def proj_tm(key, consume):
                              wv, rw = wload((l, key), "c8")
                              for t in range(4):
                                  pi = nbig()
                                  for c in range(8):
                                      MM(psb[pi][:], xT[:, c, t * 128:(t + 1) * 128], wv[:, c, :], c == 0, c == 7,
                                         [r_xT, rw], [r_ps[pi]])
                                  consume(t, psb[pi], r_ps[pi])

                          wv, rw = wload((l, "lg"), "lg")
                          pi = nbig()
                          for c in range(8):
                              MM(psb[pi][0:16, :], wv[:, c, :], xT[:, c, :], c == 0, c == 7, [r_xT, rw], [r_ps[pi]])
                          V("dve", "tensor_copy", [r_ps[pi]], [r_lgT], out=lgT[0:16, :], in_=psb[pi][0:16, :])
                          for t in range(4):
                              pi = nbig()
                              MM(psb[pi][:], lgT[:, t * 128:(t + 1) * 128], wupa[:], True, True, [r_lgT, r_wupa], [r_ps[pi]])
                              ACT(spt[:, t, :], psb[pi][:], AF.Exp, [r_ps[pi]], [r_spt[t]], scale=-1.0)
                              ACT(spt[:, t, :], spt[:, t, :], AF.Ln, [r_spt[t]], [r_spt[t]], bias=1.0)
                          for h in range(GH):
                              pi = nbig()
                              for t in range(4):
                                  MM(psb[pi][:, t * 128:(t + 1) * 128], spt[:, t, h * 128:(h + 1) * 128], uu[:], True, True,
                                     [r_spt[t], r_uu], [r_ps[pi]])
                              ACT(eq[:, h, :], psb[pi][:], AF.Exp, [r_ps[pi]], [r_eq[h]])
                              ACT(ek[:, h, :], psb[pi][:], AF.Exp, [r_ps[pi]], [r_ek[h]], scale=-1.0)
                          wv, rw = wload((l, "qg"), "c8")
                          for h in range(GH):
                              pi = nbig()
                              for c in range(8):
                                  MM(psb[pi][:], wv[:, c, h * 128:(h + 1) * 128], xT[:, c, :], c == 0, c == 7, [r_xT, rw], [r_ps[pi]])
                              V("dve", "scalar_tensor_tensor", [r_ps[pi], r_eq[h]], [r_qdT[h]], out=qdT[:, h, :], in0=psb[pi][:],
                                scalar=GK ** -0.5, in1=eq[:, h, :], op0=ALU.mult, op1=ALU.mult)
                          wv, rw = wload((l, "kg"), "c8")
                          for h in range(GH):
                              pi = nbig()
                              for c in range(8):
                                  MM(psb[pi][:], wv[:, c, h * 128:(h + 1) * 128], xT[:, c, :], c == 0, c == 7, [r_xT, rw], [r_ps[pi]])
                              V("dve", "tensor_tensor", [r_ps[pi], r_ek[h]], [r_kiT[h]], out=kiT[:, h, :], in0=psb[pi][:],
                                in1=ek[:, h, :], op=ALU.mult)

                          def cons_vg(t, ps, rps):
                              ACT(vg[:, t, :], ps[:], AF.Copy, [rps], [r_vg[t]])
                          proj_tm("vg", cons_vg)

                          def cons_rg(t, ps, rps):
                              ACT(rgs[:, t, :], ps[:], AF.Silu, [rps], [r_rgs[t]])
                          proj_tm("rg", cons_rg)

                          dec4s = {}

                          def gla_stage1(t):
                              j = t % 2
                              cs = slice(t * 128, (t + 1) * 128)
                              for h in range(GH):
                                  TR(ptr4[:, h, :], kiT[:, h, cs], identb[:], [r_kiT[h], r_ident], [r_ps[PTR]])
                              V("dve", "tensor_copy", [r_ps[PTR]], [r_kiM[j]], out=kiM[:, j, :, :], in_=ptr4[:, 0:4, :])
                              pa = nbig()
                              for h in range(GH):
                                  MM(psb[pa][:, h * 128:(h + 1) * 128], kiT[:, h, cs], qdT[:, h, cs], True, True, [r_kiT[h], r_qdT[h]], [r_ps[pa]])
                              V("dve", "tensor_tensor", [r_ps[pa], r_tri], [r_attm[j]], out=attm[:, j, :, :],
                                in0=psb[pa][:].rearrange("p (h n) -> p h n", h=GH), in1=tri[:].unsqueeze(1).broadcast_to([128, GH, 128]), op=ALU.mult)

                          def gla_stage2(t):
                              j = t % 2
                              cs = slice(t * 128, (t + 1) * 128)
                              PO = PMISC if t % 2 == 0 else 3
                              po = psb[PO]
                              for h in range(GH):
                                  MM(po[:, h * 128:(h + 1) * 128], attm[:, j, h, :], vg[:, t, h * 128:(h + 1) * 128], True, False,
                                     [r_attm[j], r_vg[t]], [r_ps[PO]])
                                  MM(po[:, h * 128:(h + 1) * 128], qdT[:, h, cs], stb[:, h, :], False, True,
                                     [r_qdT[h], r_stb[0]], [r_ps[PO]])
                              pd = nbig()
                              for h in range(GH):
                                  MM(psb[pd][:, h * 128:(h + 1) * 128], kiM[:, j, h, :], vg[:, t, h * 128:(h + 1) * 128], True, True,
                                     [r_kiM[j], r_vg[t]], [r_ps[pd]])
                              V("dve", "tensor_tensor", [r_ps[pd], r_stf[0]], [r_stmp[j]], out=stmp[:, j, :, :],
                                in0=psb[pd][:].rearrange("p (h n) -> p h n", h=GH), in1=stf[:], op=ALU.add)
                              dec4 = eq[:, :, t * 128 + 127:t * 128 + 128].broadcast_to([128, GH, 128])
                              V("dve", "tensor_tensor", [r_stmp[j]] + r_eq, [r_stf[0]], out=stf[:], in0=stmp[:, j, :, :], in1=dec4, op=ALU.mult)
                              V("dve", "tensor_tensor", [r_stmp[j]] + r_eq, [r_stb[0]], out=stb[:], in0=stmp[:, j, :, :], in1=dec4, op=ALU.mult)
                              return po, PO

                          gla_stage1(0)
                          for t in range(4):
                              if t + 1 < 4:
                                  gla_stage1(t + 1)
                              po, PO = gla_stage2(t)
                              for h in range(GH):
                                  V("dve", "bn_stats", [r_ps[PO]], [r_hn], out=hn[:, h * 6:(h + 1) * 6], in_=po[:, h * 128:(h + 1) * 128])
                                  V("dve", "bn_aggr", [r_hn], [r_hn], out=hn[:, 24 + 2 * h:26 + 2 * h], in_=hn[:, h * 6:(h + 1) * 6])
                              mv = hn[:, 24:32].rearrange("p (h k) -> p h k", h=GH)
                              V("dve", "tensor_scalar_add", [r_hn], [r_hn], out=hn[:, 32:36], in0=mv[:, :, 1], scalar1=LN_EPS)
                              ACT(hn[:, 32:36], hn[:, 32:36], AF.Sqrt, [r_hn], [r_hn])
                              V("dve", "reciprocal", [r_hn], [r_hn], out=hn[:, 32:36], in_=hn[:, 32:36])
                              for h in range(GH):
                                  V("dve", "tensor_scalar", [r_ps[PO], r_hn], [r_ob], out=ob[:, h * 128:(h + 1) * 128],
                                    in0=po[:, h * 128:(h + 1) * 128], scalar1=hn[:, 24 + 2 * h:25 + 2 * h], scalar2=hn[:, 32 + h:33 + h],
                                    op0=ALU.subtract, op1=ALU.mult)
                              V("dve", "tensor_tensor", [r_ob, r_gnt], [r_ob], out=ob[:], in0=ob[:], in1=gnt[:], op=ALU.mult)
                              V("dve", "tensor_tensor", [r_ob, r_rgs[t]], [r_yb], out=yb[:], in0=ob[:], in1=rgs[:, t, :], op=ALU.mult)
                              for kc in range(4):
                                  TR(ptr4[:, kc, :], yb[:, kc * 128:(kc + 1) * 128], identb[:], [r_yb, r_ident], [r_ps[PTR]])
                              V("dve", "tensor_copy", [r_ps[PTR]], [r_ybT], out=ybT[:, :, t * 128:(t + 1) * 128], in_=ptr4[:, 0:4, :])

                          wa_v, rwa = wload((l, "wa"), "c4")
                          wb_v, rwb = wload((l, "wb"), "c4")
                          for half in range(2):
                              ga_v, rga = wload((l, f"ga{half}"), "c8")
                              gb_v, rgb = wload((l, f"gb{half}"), "c8")
                              for mm_ in range(4):
                                  m = half * 4 + mm_
                                  cs = slice(mm_ * 128, (mm_ + 1) * 128)
                                  ms = slice(m * 128, (m + 1) * 128)
                                  pi = nbig()
                                  for c in range(8):
                                      MM(psb[pi][:], ga_v[:, c, cs], xT[:, c, :], c == 0, c == 7, [r_xT, rga], [r_ps[pi]])
                                  ACT(sg[:, 0, :], psb[pi][:], AF.Sigmoid, [r_ps[pi]], [r_sg[0]])
                                  pi = nbig()
                                  for c in range(4):
                                      MM(psb[pi][:], wa_v[:, c, ms], yaT[:, c, :], c == 0, c == 3, [r_yaT, rwa], [r_ps[pi]])
                                  V("dve", "tensor_tensor", [r_ps[pi], r_sg[0]], [r_sg[0]], out=sg[:, 0, :], in0=psb[pi][:], in1=sg[:, 0, :], op=ALU.mult)
                                  pi = nbig()
                                  for c in range(8):
                                      MM(psb[pi][:], gb_v[:, c, cs], xT[:, c, :], c == 0, c == 7, [r_xT, rgb], [r_ps[pi]])
                                  ACT(sg[:, 1, :], psb[pi][:], AF.Sigmoid, [r_ps[pi]], [r_sg[1]])
                                  pi = nbig()
                                  for c in range(4):
                                      MM(psb[pi][:], wb_v[:, c, ms], ybT[:, c, :], c == 0, c == 3, [r_ybT, rwb], [r_ps[pi]])
                                  V("dve", "tensor_tensor", [r_ps[pi], r_sg[1]], [r_sg[1]], out=sg[:, 1, :], in0=psb[pi][:], in1=sg[:, 1, :], op=ALU.mult)
                                  V("dve", "tensor_tensor", [r_sg[0], r_sg[1]], [r_mT], out=mT[:, m, :], in0=sg[:, 0, :], in1=sg[:, 1, :], op=ALU.add)

                          if cfg.dbg:
                              final_tokens.append(S.dma("pool", dbg_t["ybT"][gi], ybT[:], reads=[r_ybT], writes=[r_dbg], sres=r_dbg))
                              final_tokens.append(S.dma("pool", dbg_t["mT"][gi], mT[:], reads=[r_mT], writes=[r_dbg], sres=r_dbg))
                          wo_v0, rwo0 = wload((l, "wo0"), "c8")
                          wo_v1, rwo1 = wload((l, "wo1"), "c8")
                          for t in range(4):
                              i = ring["ln"] % 2
                              ring["ln"] += 1
                              r0 = tok0 + t * 128
                              S.dma("pool", tin[i][:], x_src[r0:r0 + 128, :], reads=[r_xsrc[gi]], writes=[r_tin[i]], sres=r_tin[i])
                              for half, (wv_, rw_) in enumerate(((wo_v0, rwo0), (wo_v1, rwo1))):
                                  pi = nbig()
                                  for c in range(8):
                                      MM(psb[pi][:], mT[:, c, t * 128:(t + 1) * 128], wv_[:, c, :], c == 0, c == 7, [r_mT, rw_], [r_ps[pi]])
                                  hs = slice(half * 512, (half + 1) * 512)
                                  V("dve", "scalar_tensor_tensor", [r_ps[pi], r_tin[i]], [r_tin[i]], out=tin[i][:, hs], in0=tin[i][:, hs],
                                    scalar=ALPHA, in1=psb[pi][:], op0=ALU.mult, op1=ALU.add)
                              layer_norm_tile((lst, r_lst), tin[i], r_tin[i], lnm_g, lnm_b, [r_lng, r_lnb], x1o[i][:], r_x1o[i], eng="dve")
                              if router:
                                  for e in range(NE):
                                      V("dve", "scalar_tensor_tensor", [r_x1o[i], r_wrB], [r_tin[i], r_lgR], out=tin[i][:], in0=x1o[i][:], scalar=1.0,
                                        in1=wrB[:, e, :], op0=ALU.mult, op1=ALU.mult, accum_out=lgR[:, r0 // 128, e:e + 1])
                              S.dma("pool", x1s[r0:r0 + 128, :], x1o[i][:], reads=[r_x1o[i]], writes=[r_x1s[gi]], sres=r_x1o[i], load=False)
            S.barrier()

        def phase_ffn(l, dst, r_dst, is_final):
            moe = (l % 2 == 1)
            with ExitStack() as st:
                set_ring(st, 6)
                lnf_g = sb(st, "lnf_g", [128, D], F32)
                lnf_b = sb(st, "lnf_b", [128, D], F32)
                r_lnf = S.res("lnf")
                S.dma("pool", lnf_g[:], ln_fg[l].partition_broadcast(128), writes=[r_lnf], sres=r_lnf)
                S.dma("pool", lnf_b[:], ln_fb[l].partition_broadcast(128), writes=[r_lnf], sres=r_lnf)
                xf = [sb(st, f"fxf{i}", [128, D], F32) for i in range(2)]
                r_xf = [S.res(f"fxf{i}") for i in range(2)]
                xb = [sb(st, f"fxb{i}", [128, D], BF16) for i in range(2)]
                r_xb = [S.res(f"fxb{i}") for i in range(2)]
                xT = sb(st, "fxT", [128, 8, G], BF16)
                r_xT = S.res("fxT")
                hT = sb(st, "hT", [128, 28, G], BF16)
                r_hT = [S.res(f"hT{i}") for i in range(7)]
                sgt = [sb(st, f"sgt{i}", [128, G], BF16) for i in range(2)]
                r_sgt = [S.res(f"sgt{i}") for i in range(2)]
                tin = [sb(st, f"ftin{i}", [128, D], F32) for i in range(4)]
                r_tin = [S.res(f"ftin{i}") for i in range(4)]
                yo = [sb(st, f"fyo{i}", [128, D], F32) for i in range(4)]
                r_yo = [S.res(f"fyo{i}") for i in range(4)]
                lst = sb(st, "flst", [128, 16], F32)
                r_lst = S.res("flst")
                if moe:
                    wr = sb(st, "wr", [128, NE, D], F32)
                    r_wr = S.res("wr")
                    for e in range(NE):
                        S.dma("pool", wr[:, e, :], m_rT[e].partition_broadcast(128), writes=[r_wr], sres=r_wr)
                    acc = sb(st, "macc", [128, 4, D], F32)
                    r_acc = [S.res(f"macc{t}") for t in range(4)]
                    lg = sb(st, "mlg", [128, 4, 32], F32)
                    r_lg = [S.res(f"mlg{t}") for t in range(4)]
                    junk = sb(st, "mjunk", [128, D], F32)
                    r_junk = S.res("mjunk")
                ring = {"big": 0, "x": 0, "sg": 0, "ln": 0}
                pbig = [0, 1, 2, 3]
                pdn = [4, 5]
                PTR = 6

                def nbig():
                    i = pbig[ring["big"] % len(pbig)]
                    ring["big"] += 1
                    return i

                for gi in range(T // G):
                    tok0 = gi * G
                    for t in range(4):
                        i = ring["x"] % 2
                        ring["x"] += 1
                        r0 = tok0 + t * 128
                        S.dma("pool", xf[i][:], x1s[r0:r0 + 128, :], reads=[r_x1s[gi]], writes=[r_xf[i]], sres=r_xf[i])
                        ACT(xb[i][:], xf[i][:], AF.Copy, [r_xf[i]], [r_xb[i]])
                        ptr = psb[PTR][:].bitcast(BF16).rearrange("p (c n) -> p c n", c=8)
                        for c in range(8):
                            TR(ptr[:, c, :], xb[i][:, c * 128:(c + 1) * 128], identb[:], [r_xb[i], r_ident], [r_ps[PTR]])
                        V("dve", "tensor_copy", [r_ps[PTR]], [r_xT], out=xT[:, :, t * 128:(t + 1) * 128], in_=ptr)
                        if moe:
                            for e in range(NE):
                                V("dve", "tensor_tensor", [r_xf[i], r_wr], [r_junk], out=junk[:], in0=xf[i][:],
                                  in1=wr[:, e, :], op=ALU.mult)
                                V("dve", "tensor_reduce", [r_junk], [r_lg[t]], out=lg[:, t, e:e + 1], in_=junk[:], axis=AX.X, op=ALU.add)
                            L = lg[:, t, 0:8]
                            m1 = lg[:, t, 8:9]
                            m2 = lg[:, t, 9:10]
                            E1 = lg[:, t, 10:18]
                            L2 = lg[:, t, 18:26]
                            V("dve", "tensor_reduce", [r_lg[t]], [r_lg[t]], out=m1, in_=L, axis=AX.X, op=ALU.max)
                            V("dve", "tensor_scalar", [r_lg[t]], [r_lg[t]], out=E1, in0=L, scalar1=m1, scalar2=None, op0=ALU.is_ge)
                            V("dve", "scalar_tensor_tensor", [r_lg[t]], [r_lg[t]], out=L2, in0=E1, scalar=-BIG, in1=L, op0=ALU.mult, op1=ALU.add)
                            V("dve", "tensor_reduce", [r_lg[t]], [r_lg[t]], out=m2, in_=L2, axis=AX.X, op=ALU.max)
                            V("dve", "tensor_scalar", [r_lg[t]], [r_lg[t]], out=E1, in0=L, scalar1=m2, scalar2=None, op0=ALU.is_ge)
                            V("dve", "tensor_scalar", [r_lg[t]], [r_lg[t]], out=L2, in0=L, scalar1=m1, scalar2=None, op0=ALU.subtract)
                            ACT(L2, L2, AF.Exp, [r_lg[t]], [r_lg[t]])
                            V("dve", "tensor_tensor", [r_lg[t]], [r_lg[t]], out=L2, in0=L2, in1=E1, op=ALU.mult)
                            V("dve", "tensor_reduce", [r_lg[t]], [r_lg[t]], out=lg[:, t, 26:27], in_=L2, axis=AX.X, op=ALU.add)
                            V("dve", "reciprocal", [r_lg[t]], [r_lg[t]], out=lg[:, t, 26:27], in_=lg[:, t, 26:27])
                            V("dve", "tensor_scalar", [r_lg[t]], [r_lg[t]], out=E1, in0=L2, scalar1=lg[:, t, 26:27], scalar2=None, op0=ALU.mult)

                    for e in range(NE if moe else 1):
                        kg = (lambda i: ("m", e, "g", i)) if moe else (lambda i: ("f", "g", i))
                        ku = (lambda i: ("m", e, "u", i)) if moe else (lambda i: ("f", "u", i))
                        kd = (lambda i: ("m", e, "d", i)) if moe else (lambda i: ("f", "d", i))
                        for bi in range(7):
                            wg_v, rwg = wload(kg(bi), "c8")
                            wu_v, rwu = wload(ku(bi), "c8")
                            for j in range(4):
                                fc = bi * 4 + j
                                js = slice(j * 128, (j + 1) * 128)
                                pg = nbig()
                                for c in range(8):
                                    MM(psb[pg][:], wg_v[:, c, js], xT[:, c, :], c == 0, c == 7, [r_xT, rwg], [r_ps[pg]])
                                k = ring["sg"] % 2
                                ring["sg"] += 1
                                ACT(sgt[k][:], psb[pg][:], AF.Silu, [r_ps[pg]], [r_sgt[k]])
                                pu = nbig()
                                for c in range(8):
                                    MM(psb[pu][:], wu_v[:, c, js], xT[:, c, :], c == 0, c == 7, [r_xT, rwu], [r_ps[pu]])
                                V("dve", "tensor_tensor", [r_ps[pu], r_sgt[k]], [r_hT[bi]], out=hT[:, fc, :], in0=psb[pu][:], in1=sgt[k][:], op=ALU.mult)
                        dblocks = []
                        for half in range(2):
                            accp = [nbig() for _ in range(4)]
                            for bi in range(7):
                                wd_v, rwd = wload(kd(bi), "c4")
                                for t in range(4):
                                    for j in range(4):
                                        fc = bi * 4 + j
                                        MM(psb[accp[t]][:], hT[:, fc, t * 128:(t + 1) * 128], wd_v[:, j, half * 512:(half + 1) * 512],
                                           fc == 0, fc == 27, [r_hT[bi], rwd], [r_ps[accp[t]]])
                            hs = slice(half * 512, (half + 1) * 512)
                            for t in range(4):
                                if moe:
                                    cw = lg[:, t, 10 + e:11 + e]
                                    if e == 0:
                                        V("dve", "tensor_scalar", [r_ps[accp[t]], r_lg[t]], [r_acc[t]], out=acc[:, t, hs], in0=psb[accp[t]][:],
                                          scalar1=cw, scalar2=None, op0=ALU.mult)
                                    else:
                                        V("dve", "scalar_tensor_tensor", [r_ps[accp[t]], r_lg[t], r_acc[t]], [r_acc[t]], out=acc[:, t, hs],
                                          in0=psb[accp[t]][:], scalar=cw, in1=acc[:, t, hs], op0=ALU.mult, op1=ALU.add)
                                else:
                                    if half == 0:
                                        i = t
                                        r0 = tok0 + t * 128
                                        S.dma("pool", tin[i][:], x1s[r0:r0 + 128, :], reads=[r_x1s[gi]], writes=[r_tin[i]], sres=r_tin[i])
                                        dblocks.append(i)
                                    i = dblocks[t]
                                    V("dve", "scalar_tensor_tensor", [r_ps[accp[t]], r_tin[i]], [r_tin[i]], out=tin[i][:, hs], in0=tin[i][:, hs],
                                      scalar=ALPHA, in1=psb[accp[t]][:], op0=ALU.mult, op1=ALU.add)
                                    if half == 1:
                                        r0 = tok0 + t * 128
                                        layer_norm_tile((lst, r_lst), tin[i], r_tin[i], lnf_g, lnf_b, [r_lnf], yo[i][:], r_yo[i], eng="pool")
                                        o = S.dma("pool", dst[r0:r0 + 128, :], yo[i][:], reads=[r_yo[i]], writes=[r_dst[gi]], sres=r_yo[i], load=False)
                                        if is_final:
                                            final_tokens.append(o)
                    if moe:
                        for t in range(4):
                            i = t
                            r0 = tok0 + t * 128
                            S.dma("pool", tin[i][:], x1s[r0:r0 + 128, :], reads=[r_x1s[gi]], writes=[r_tin[i]], sres=r_tin[i])
                            V("dve", "scalar_tensor_tensor", [r_acc[t], r_tin[i]], [r_tin[i]], out=tin[i][:], in0=tin[i][:],
                              scalar=ALPHA, in1=acc[:, t, :], op0=ALU.mult, op1=ALU.add)
                            layer_norm_tile((lst, r_lst), tin[i], r_tin[i], lnf_g, lnf_b, [r_lnf], yo[i][:], r_yo[i], eng="pool")
                            o = S.dma("pool", dst[r0:r0 + 128, :], yo[i][:], reads=[r_yo[i]], writes=[r_dst[gi]], sres=r_yo[i], load=False)
                            if is_final:
                                final_tokens.append(o)
            S.barrier()

        def phase_moe(l, dst, r_dst, is_final):
            IOA = bass.IndirectOffsetOnAxis
            NT = T // 128
            wsc_rows = wsm.rearrange("b p n -> (b p) n")
            moe_blk0 = [r_wsc[tab[("m", e, "g", 0)]] for e in range(NE)]
            r_s2t = S.res("s2t")
            r_s2t_parts = []
            r_yg = [S.res(f"yg{j}") for j in range(NGRP)]
            PTR, PMISC = 6, 7
            with ExitStack() as so:
                set_ring(so, 6)
                lg = sb(so, "mlg", [128, NT, 32], F32)
                r_lg = S.res("mlg")
                selm = sb(so, "selm", [128, NT, NE], F32)
                hot1 = sb(so, "hot1", [128, NT, NE], F32)
                hot2 = sb(so, "hot2", [128, NT, NE], F32)
                rankg = sb(so, "rankg", [128, NT, NE], F32)
                r_rt = S.res("route")
                cwk = sb(so, "cwk", [128, NT, 2], F32)
                r_cwk = S.res("cwk")
                run = sb(so, "runc", [128, 48], F32)
                r_run = S.res("runc")
                widx = sb(so, "widx", [128, NGRP, 21], I32)
                r_widx = S.res("widx")
                sl0i = sb(so, "sl0i", [128, NT], I32)
                sl1i = sb(so, "sl1i", [128, NT], I32)
                r_sli = S.res("sli")
                tokc = sb(so, "tokc", [128, NT], I32)
                r_tokc = S.res("tokc")
                S.dma("pool", tokc[:], c_tok, writes=[r_tokc], sres=r_tokc)

                with ExitStack() as st:
                    if not cfg.do_mix:
                        wr = sb(st, "wr", [128, NE, D], F32)
                        r_wr = S.res("wr")
                        for e in range(NE):
                            S.dma("pool", wr[:, e, :], m_rT[e].partition_broadcast(128), writes=[r_wr], sres=r_wr)
                        xf = [sb(st, f"rxf{i}", [128, D], F32) for i in range(2)]
                        r_xf = [S.res(f"rxf{i}") for i in range(2)]
                        junk = sb(st, "rjunk", [128, D], F32)
                        r_junk = S.res("rjunk")
                    tris, r_tris = sb(st, "tris", [128, 128], F32), S.res("tris")
                    S.dma("pool", tris[:], c_tris, writes=[r_tris], sres=r_tris)
                    onesf, r_onesf = sb(st, "onesf", [128, 128], F32), S.res("onesf")
                    V("pool", "memset", [], [r_onesf], ap=onesf[:], constant=1.0)
                    thr, r_thr = sb(st, "thr", [128, 16], F32), S.res("thr")
                    S.dma("pool", thr[:], c_thr, writes=[r_thr], sres=r_thr)
                    jj, r_jj = sb(st, "jj", [128, NGRP], F32), S.res("jj")
                    S.dma("pool", jj[:], c_jj, writes=[r_jj], sres=r_jj)
                    wcst, r_wcst = sb(st, "wcst", [128, 21], F32), S.res("wcst")
                    S.dma("pool", wcst[:], c_wcst, writes=[r_wcst], sres=r_wcst)
                    cmp3 = sb(st, "cmp3", [128, NE, 16], F32)
                    ej = sb(st, "ej", [128, NGRP], F32)
                    widxf = sb(st, "widxf", [128, NGRP, 21], F32)
                    slotf = sb(st, "slotf", [128, NT, NE], F32)
                    stmp = sb(st, "rstmp", [128, NT, NE], F32)
                    s01f = sb(st, "s01f", [128, 2, NT], F32)
                    zi = sb(st, "zi", [128, NSLOT // 128], I32)
                    r_zi = S.res("zi")
                    V("pool", "memset", [], [r_zi], ap=zi[:], constant=0)
                    S.dma("pool", s2t.rearrange("(p n) o -> p (n o)", p=128), zi[:], reads=[r_zi], writes=[r_s2t], sres=r_s2t)
                    r_misc = S.res("rmisc")
                    V("dve", "memset", [], [r_run], ap=run[:], constant=0.0)

                    for i in range(NT):
                        b = i % 2
                        r0 = i * 128
                        if cfg.do_mix:
                            V("dve", "tensor_copy", [r_lgR], [r_lg], out=lg[:, i, 0:8], in_=lgR[:, i, :])
                        else:
                            S.dma("pool", xf[b][:], x1s[r0:r0 + 128, :], reads=[r_x1s[r0 // G]], writes=[r_xf[b]], sres=r_xf[b])
                            for e in range(NE):
                                V("dve", "scalar_tensor_tensor", [r_xf[b], r_wr], [r_junk, r_lg], out=junk[:], in0=xf[b][:], scalar=1.0, in1=wr[:, e, :],
                                  op0=ALU.mult, op1=ALU.mult, accum_out=lg[:, i, e:e + 1])
                        L = lg[:, i, 0:8]
                        m1 = lg[:, i, 8:9]
                        m2 = lg[:, i, 9:10]
                        L2 = lg[:, i, 18:26]
                        dd = lg[:, i, 26:27]
                        H1 = hot1[:, i, :]
                        H2 = hot2[:, i, :]
                        V("dve", "tensor_reduce", [r_lg], [r_lg], out=m1, in_=L, axis=AX.X, op=ALU.max)
                        V("dve", "tensor_scalar", [r_lg], [r_rt], out=H1, in0=L, scalar1=m1, scalar2=None, op0=ALU.is_ge)
                        V("dve", "scalar_tensor_tensor", [r_lg, r_rt], [r_lg], out=L2, in0=H1, scalar=-BIG, in1=L, op0=ALU.mult, op1=ALU.add)
                        V("dve", "tensor_reduce", [r_lg], [r_lg], out=m2, in_=L2, axis=AX.X, op=ALU.max)
                        V("dve", "tensor_scalar", [r_lg], [r_rt], out=H2, in0=L2, scalar1=m2, scalar2=None, op0=ALU.is_ge)
                        V("dve", "tensor_tensor", [r_rt], [r_rt], out=selm[:, i, :], in0=H1, in1=H2, op=ALU.add)
                        V("dve", "tensor_tensor", [r_lg], [r_lg], out=dd, in0=m2, in1=m1, op=ALU.subtract)
                        ACT(dd, dd, AF.Exp, [r_lg], [r_lg])
                        V("dve", "tensor_scalar_add", [r_lg], [r_cwk], out=cwk[:, i, 0:1], in0=dd, scalar1=1.0)
                        V("dve", "reciprocal", [r_cwk], [r_cwk], out=cwk[:, i, 0:1], in_=cwk[:, i, 0:1])
                        V("dve", "tensor_tensor", [r_cwk, r_lg], [r_cwk], out=cwk[:, i, 1:2], in0=cwk[:, i, 0:1], in1=dd, op=ALU.mult)
                        pr = psb[PMISC][:, 0:16]
                        MM(pr[:, 0:8], tris[:], selm[:, i, :], True, True, [r_tris, r_rt], [r_ps[PMISC]])
                        MM(pr[:, 8:16], onesf[:], selm[:, i, :], True, True, [r_onesf, r_rt], [r_ps[PMISC]])
                        V("dve", "tensor_tensor", [r_ps[PMISC], r_run], [r_rt], out=rankg[:, i, :], in0=pr[:, 0:8], in1=run[:, 0:8], op=ALU.add)
                        V("dve", "tensor_tensor", [r_ps[PMISC], r_run], [r_run], out=run[:, 0:8], in0=pr[:, 8:16], in1=run[:, 0:8], op=ALU.add)

                    cnt, ng, gend, base = run[:, 0:8], run[:, 8:16], run[:, 16:24], run[:, 24:32]
                    V("dve", "tensor_tensor", [r_run, r_thr], [r_misc], out=cmp3[:], in0=cnt.unsqueeze(2).broadcast_to([128, NE, 16]),
                      in1=thr[:].unsqueeze(1).broadcast_to([128, NE, 16]), op=ALU.is_gt)
                    V("dve", "tensor_reduce", [r_misc], [r_run], out=ng, in_=cmp3[:], axis=AX.X, op=ALU.add)
                    V("dve", "tensor_copy", [r_run], [r_run], out=gend[:, 0:1], in_=ng[:, 0:1])
                    for e in range(1, NE):
                        V("dve", "tensor_tensor", [r_run], [r_run], out=gend[:, e:e + 1], in0=gend[:, e - 1:e], in1=ng[:, e:e + 1], op=ALU.add)
                    V("dve", "tensor_tensor", [r_run], [r_run], out=base, in0=gend, in1=ng, op=ALU.subtract)
                    V("dve", "tensor_scalar_mul", [r_run], [r_run], out=base, in0=base, scalar1=512.0)
                    V("dve", "tensor_scalar", [r_jj, r_run], [r_misc], out=ej[:], in0=jj[:], scalar1=gend[:, 0:1], scalar2=None, op0=ALU.is_ge)
                    for e in range(1, NE):
                        V("dve", "scalar_tensor_tensor", [r_jj, r_run, r_misc], [r_misc], out=ej[:], in0=jj[:], scalar=gend[:, e:e + 1], in1=ej[:],
                          op0=ALU.is_ge, op1=ALU.add)
                    V("dve", "tensor_scalar_min", [r_misc], [r_misc], out=ej[:], in0=ej[:], scalar1=float(NE - 1))
                    V("dve", "scalar_tensor_tensor", [r_misc, r_wcst], [r_misc], out=widxf[:], in0=ej[:].unsqueeze(2).broadcast_to([128, NGRP, 21]),
                      scalar=float(21 * 128), in1=wcst[:].unsqueeze(1).broadcast_to([128, NGRP, 21]), op0=ALU.mult, op1=ALU.add)
                    V("dve", "tensor_copy", [r_misc], [r_widx], out=widx[:], in_=widxf[:])
                    V("dve", "tensor_tensor", [r_rt, r_run], [r_misc], out=slotf[:], in0=rankg[:], in1=base.unsqueeze(1).broadcast_to([128, NT, NE]), op=ALU.add)
                    for k, hot in enumerate((hot1, hot2)):
                        V("dve", "tensor_tensor", [r_misc, r_rt], [r_misc], out=stmp[:], in0=slotf[:], in1=hot[:], op=ALU.mult)
                        V("dve", "tensor_reduce", [r_misc], [r_misc], out=s01f[:, k, :], in_=stmp[:], axis=AX.X, op=ALU.add)
                    V("dve", "tensor_copy", [r_misc], [r_sli], out=sl0i[:], in_=s01f[:, 0, :])
                    V("dve", "tensor_copy", [r_misc], [r_sli], out=sl1i[:], in_=s01f[:, 1, :])
                    sc_st = S.new_stream("scat")
                    sc_st.q = "pool"
                    sc_ops = []
                    for i in range(NT):
                        for sli in (sl0i, sl1i):
                            rr = S.res("s2tw")
                            r_s2t_parts.append(rr)
                            sc_ops.append(S.dma("pool", None, None, reads=[r_sli, r_tokc, r_s2t], writes=[rr], sres=rr, stream=sc_st,
                                                fn=(lambda e, sli=sli, i=i: e.indirect_dma_start(out=s2t[:, :], out_offset=IOA(ap=sli[:, i:i + 1], axis=0),
                                                                                                  in_=tokc[:, i:i + 1], in_offset=None))))
                    for o_ in sc_ops:
                        o_.dval = sc_st.count * 16
                S.barrier()
                with ExitStack() as st:
                    tix = [sb(st, f"tix{i}", [128, 4], I32) for i in range(2)]
                    r_tix = [S.res(f"tix{i}") for i in range(2)]
                    xg = [[sb(st, f"xg{i}_{q}", [128, D], BF16) for q in range(4)] for i in range(2)]
                    r_xg = [[S.res(f"xg{i}_{q}") for q in range(4)] for i in range(2)]
                    xT = sb(st, "exT", [128, 8, G], BF16)
                    r_xT = S.res("exT")
                    hT = sb(st, "ehT", [128, 28, G], BF16)
                    r_hT = [S.res(f"ehT{i}") for i in range(7)]
                    sgt = [sb(st, f"esgt{i}", [128, G], BF16) for i in range(2)]
                    r_sgt = [S.res(f"esgt{i}") for i in range(2)]
                    yo = [sb(st, f"eyo{i}", [128, D], F32) for i in range(4)]
                    r_yo = [S.res(f"eyo{i}") for i in range(4)]
                    ring = {"big": 0, "sg": 0}
                    pbig = [0, 1, 2, 3]

                    def nbig():
                        i = pbig[ring["big"] % len(pbig)]
                        ring["big"] += 1
                        return i

                    def wload_dyn(j, k, view):
                        i = wstate["i"] % len(wring)
                        wstate["i"] += 1
                        S.dma("pool", None, None, reads=[r_widx] + moe_blk0, writes=[r_wring[i]], sres=r_wring[i],
                              fn=(lambda e, i=i, j=j, k=k: e.indirect_dma_start(out=wring[i][:], out_offset=None, in_=wsc_rows,
                                                                               in_offset=IOA(ap=widx[:, j, k:k + 1], axis=0))))
                        t = wring[i]
                        v = t[:].rearrange("p (c n) -> p c n", c=8 if view == "c8" else 4)
                        return v, r_wring[i]

                    def gather_group(j):
                        jb = j % 2
                        for q in range(4):
                            s0 = j * 512 + q * 128
                            S.dma("sp", tix[jb][:, q:q + 1], s2t[s0:s0 + 128, :], reads=[r_s2t] + r_s2t_parts, writes=[r_tix[jb]], sres=r_tix[jb])
                        for q in range(4):
                            S.dma("pool", None, None, reads=[r_tix[jb]] + r_x1s, writes=[r_xg[jb][q]], sres=r_xg[jb][q],
                                  fn=(lambda e, jb=jb, q=q: e.indirect_dma_start(out=xg[jb][q][:], out_offset=None, in_=x1s[:, :],
                                                                                 in_offset=IOA(ap=tix[jb][:, q:q + 1], axis=0))))

                    gather_group(0)
                    for j in range(NGRP):
                        jb = j % 2
                        if j + 1 < NGRP:
                            gather_group(j + 1)
                        for q in range(4):
                            PB = PTR if q % 2 == 0 else PMISC
                            ptr = psb[PB][:].bitcast(BF16).rearrange("p (c n) -> p c n", c=8)
                            for c in range(8):
                                TR(ptr[:, c, :], xg[jb][q][:, c * 128:(c + 1) * 128], identb[:], [r_xg[jb][q], r_ident], [r_ps[PB]])
                            V("dve", "tensor_copy", [r_ps[PB]], [r_xT], out=xT[:, :, q * 128:(q + 1) * 128], in_=ptr)
                        for bi in range(7):
                            wg_v, rwg = wload_dyn(j, bi, "c8")
                            wu_v, rwu = wload_dyn(j, 7 + bi, "c8")
                            for jn in range(4):
                                fc = bi * 4 + jn
                                js = slice(jn * 128, (jn + 1) * 128)
                                pg = nbig()
                                for c in range(8):
                                    MM(psb[pg][:], wg_v[:, c, js], xT[:, c, :], c == 0, c == 7, [r_xT, rwg], [r_ps[pg]])
                                k = ring["sg"] % 2
                                ring["sg"] += 1
                                ACT(sgt[k][:], psb[pg][:], AF.Silu, [r_ps[pg]], [r_sgt[k]])
                                pu = nbig()
                                for c in range(8):
                                    MM(psb[pu][:], wu_v[:, c, js], xT[:, c, :], c == 0, c == 7, [r_xT, rwu], [r_ps[pu]])
                                V("dve", "tensor_tensor", [r_ps[pu], r_sgt[k]], [r_hT[bi]], out=hT[:, fc, :], in0=psb[pu][:], in1=sgt[k][:], op=ALU.mult)
                        for half in range(2):
                            accp = [nbig() for _ in range(4)]
                            for bi in range(7):
                                wd_v, rwd = wload_dyn(j, 14 + bi, "c4")
                                for t in range(4):
                                    for jn in range(4):
                                        fc = bi * 4 + jn
                                        MM(psb[accp[t]][:], hT[:, fc, t * 128:(t + 1) * 128], wd_v[:, jn, half * 512:(half + 1) * 512],
                                           fc == 0, fc == 27, [r_hT[bi], rwd], [r_ps[accp[t]]])
                            hs = slice(half * 512, (half + 1) * 512)
                            for t in range(4):
                                ACT(yo[t][:, hs], psb[accp[t]][:], AF.Copy, [r_ps[accp[t]]], [r_yo[t]])
                                if half == 1:
                                    s0 = j * 512 + t * 128
                                    S.dma("sp", ygs[s0:s0 + 128, :], yo[t][:], reads=[r_yo[t]], writes=[r_yg[j]], sres=r_yo[t], load=False)
                S.barrier()
                with ExitStack() as st:
                    lnf_g = sb(st, "elnf_g", [128, D], F32)
                    lnf_b = sb(st, "elnf_b", [128, D], F32)
                    r_lnf = S.res("elnf")
                    S.dma("pool", lnf_g[:], ln_fg[l].partition_broadcast(128), writes=[r_lnf], sres=r_lnf)
                    S.dma("pool", lnf_b[:], ln_fb[l].partition_broadcast(128), writes=[r_lnf], sres=r_lnf)
                    y0 = [sb(st, f"oy0_{i}", [128, D], F32) for i in range(2)]
                    y1 = [sb(st, f"oy1_{i}", [128, D], F32) for i in range(2)]
                    r_y0 = [S.res(f"oy0_{i}") for i in range(2)]
                    r_y1 = [S.res(f"oy1_{i}") for i in range(2)]
                    tin = [sb(st, f"otin{i}", [128, D], F32) for i in range(2)]
                    r_tin = [S.res(f"otin{i}") for i in range(2)]
                    yo = [sb(st, f"oyo{i}", [128, D], F32) for i in range(2)]
                    r_yo = [S.res(f"oyo{i}") for i in range(2)]
                    lst = sb(st, "olst", [128, 16], F32)
                    r_lst = S.res("olst")
                    for i in range(NT):
                        b = i % 2
                        r0 = i * 128
                        gi = r0 // G
                        S.dma("sp", tin[b][:], x1s[r0:r0 + 128, :], reads=[r_x1s[gi]], writes=[r_tin[b]], sres=r_tin[b])
                        for yt, ryt, sli in ((y0, r_y0, sl0i), (y1, r_y1, sl1i)):
                            S.dma("pool", None, None, reads=[r_sli] + r_yg, writes=[ryt[b]], sres=ryt[b],
                                  fn=(lambda e, yt=yt, sli=sli, b=b, i=i: e.indirect_dma_start(out=yt[b][:], out_offset=None, in_=ygs[:, :],
                                                                                                in_offset=IOA(ap=sli[:, i:i + 1], axis=0))))
                        ACT(tin[b][:], tin[b][:], AF.Copy, [r_tin[b]], [r_tin[b]], scale=ALPHA)
                        V("dve", "scalar_tensor_tensor", [r_y0[b], r_cwk, r_tin[b]], [r_tin[b]], out=tin[b][:], in0=y0[b][:], scalar=cwk[:, i, 0:1],
                          in1=tin[b][:], op0=ALU.mult, op1=ALU.add)
                        V("dve", "scalar_tensor_tensor", [r_y1[b], r_cwk, r_tin[b]], [r_tin[b]], out=tin[b][:], in0=y1[b][:], scalar=cwk[:, i, 1:2],
                          in1=tin[b][:], op0=ALU.mult, op1=ALU.add)
                        layer_norm_tile((lst, r_lst), tin[b], r_tin[b], lnf_g, lnf_b, [r_lnf], yo[b][:], r_yo[b], eng="dve")
                        o = S.dma("sp", dst[r0:r0 + 128, :], yo[b][:], reads=[r_yo[b]], writes=[r_dst[gi]], sres=r_yo[b], load=False)
                        if is_final:
                            final_tokens.append(o)
            S.barrier()

        r_xin = [S.res(f"xin{i}") for i in range(T // G)]
        r_yout = [S.res(f"yout{i}") for i in range(T // G)]
        pend = []
        for n_, l in enumerate(cfg.layers):
            if n_ == 0 and cfg.do_mix:
                prepass_layer(l)
            else:
                pend.append(lambda l=l: prepass_layer(l))
            if l % 2 == 0:
                pend.append(prepass_ffn)
            else:
                for e in range(NE):
                    pend.append(lambda e=e: prepass_moe([e]))
        if not cfg.do_mix:
            while pend:
                pend.pop(0)()
        last = cfg.layers[-1]
        for l in cfg.layers:
            src, rsrc = (x_in, r_xin) if l == cfg.layers[0] else (xs1, r_xs1)
            if cfg.do_mix:
                hk, pend = pend, []
                phase_mixer(l, src, rsrc, hooks=hk, router=(l % 2 == 1 and cfg.do_ffn and cfg.sparse))
            if (not cfg.do_mix) or ("B" not in cfg.parts):
                for gi in range(T // G):
                    S.dma("pool", x1s[gi * G:(gi + 1) * G, :], src[gi * G:(gi + 1) * G, :], reads=[rsrc[gi]], writes=[r_x1s[gi]],
                          sres=r_x1s[gi])
            if not cfg.do_ffn:
                for gi in range(T // G):
                    o = S.dma("pool", y_out[gi * G:(gi + 1) * G, :], x1s[gi * G:(gi + 1) * G, :], reads=[r_x1s[gi]], writes=[r_yout[gi]],
                              sres=r_yout[gi])
                    final_tokens.append(o)
            if cfg.do_ffn:
                ph = phase_moe if (l % 2 == 1 and cfg.sparse) else phase_ffn
                if l == last:
                    ph(l, y_out, r_yout, True)
                else:
                    ph(l, xs1, r_xs1, False)
        stats = S.emit(final_tokens)
        print("instr/wait per engine:", stats, "sems:", S.nsem, flush=True)
    return nc


def host_consts(S_):
    c = {}
    c["c_ident"] = np.eye(128, dtype=np.float32)
    inv = np.power(np.float32(500000.0), -np.arange(0, 16, 2, dtype=np.float32) / np.float32(16)).astype(np.float32)
    ang = (np.arange(S_, dtype=np.float32)[:, None] * inv[None, :]).astype(np.float32)
    co, si = np.cos(ang).astype(np.float32), np.sin(ang).astype(np.float32)
    c["c_cc"] = np.concatenate([co, co], 1).astype(np.float32)
    c["c_ss"] = np.concatenate([-si, si], 1).astype(np.float32)
    p = np.arange(128)[:, None]
    f = np.arange(512)[None, :]
    c["c_cmb"] = np.stack([np.where(cl * 128 + p <= f, 0.0, NEG) for cl in range(4)]).astype(np.float32)
    tok = np.arange(S_)[None, :]
    c["c_oh"] = (tok // MBLK == np.arange(16)[:, None]).astype(np.float32)
    j = np.tile(np.arange(16), MH)[None, :]
    blk = np.arange(16)[:, None]
    pm = (j < blk).astype(np.float32)
    c["c_pm"] = pm
    c["c_pa"] = ((pm - 1.0) * BIG).astype(np.float32)
    c["c_own"] = (j == blk).astype(np.float32)
    s = np.arange(128)[:, None]
    t = np.arange(128)[None, :]
    c["c_u"] = np.where(s <= t, -1.0 / 16.0, 0.0).astype(np.float32)
    c["c_tri"] = (s <= t).astype(np.float32)
    c["c_tris"] = (s < t).astype(np.float32)
    return c


def host_consts_moe(T):
    c = {}
    ntt = T // 128
    ngrp = (2 * T + NE * 511) // 512
    p = np.arange(128)[:, None]
    c["c_tok"] = (np.arange(ntt)[None, :] * 128 + p).astype(np.int32)
    tab, _ = block_table()
    base_m = tab[("m", 0, "g", 0)]
    c["c_wcst"] = (np.arange(21)[None, :] * 128 + p).astype(np.float32)
    c["c_jj"] = np.broadcast_to(np.arange(ngrp, dtype=np.float32)[None, :], (128, ngrp)).copy()
    c["c_thr"] = np.broadcast_to((np.arange(16, dtype=np.float32) * 512.0)[None, :], (128, 16)).copy()
    return c


_CACHE = {}


def kernel(**inputs):
    ncores = 8
    S_ = 4096
    nseq = 2
    if "prog" not in _CACHE:
        _CACHE["prog"] = build_program(Cfg(S=S_, NSEQ=nseq))
    nc = _CACHE["prog"]
    x = np.ascontiguousarray(np.asarray(inputs["x"], dtype=np.float32))
    consts = host_consts(S_)
    consts.update(host_consts_moe(S_ * nseq))
    shared = {k: np.ascontiguousarray(np.asarray(v, dtype=np.float32)) for k, v in inputs.items() if k != "x"}
    shared["moe_w_router_T"] = np.ascontiguousarray(shared["moe_w_router"][0].T)
    in_maps = []
    for c in range(ncores):
        m = dict(shared)
        m.update(consts)
        m["x"] = x[c * nseq:(c + 1) * nseq].reshape(nseq * S_, D)
        in_maps.append(m)
    res = run_bass_kernel_spmd(nc, in_maps, core_ids=list(range(ncores)))
    out = np.concatenate([np.asarray(r["y"]).reshape(nseq, S_, D) for r in res.results], axis=0)
    return out.astype(np.float32)
```
